# Optimizing a Trainium2 kernel written in Bass

```python
import jax
import jax.numpy as jnp
from jax import lax
import numpy as np

D_MODEL = 1024
BATCH = 4
SEQ = 8192
DEPTH = 1

GRID_W = 64
N_MEM = 256
HEAD_DIM = 64
NA_HEADS = 6
NA_WIDTH = NA_HEADS * HEAD_DIM
NA_WIN_ROWS = 8
NA_WIN_COLS = 16
RW_HEADS = 6
RW_WIDTH = RW_HEADS * HEAD_DIM
DECAY_LORA = 64
AAA_LORA = 64
GATE_LORA = 128
MEM_HEADS = 4
MEM_WIDTH = MEM_HEADS * HEAD_DIM
N_BRANCH = 3
N_EXPERTS = 16
EC_CAPACITY = 2
EXPERT_FF = 2 * D_MODEL
RMS_EPS = 1e-6
GN_EPS = 64e-5
IN_WIDTHS = (NA_WIDTH, NA_WIDTH, NA_WIDTH, RW_WIDTH, RW_WIDTH, RW_WIDTH, DECAY_LORA, AAA_LORA, GATE_LORA, MEM_WIDTH, D_MODEL, D_MODEL, D_MODEL)
D_IN = sum(IN_WIDTHS)

kernel_name = 'hybrid_na_rwkv7_mem_ecmoe_encoder'


def rms_norm(x, g):
    xf = x.astype(jnp.float32)
    y = xf * lax.rsqrt(jnp.mean(xf * xf, axis=-1, keepdims=True) + RMS_EPS)
    return (y * g.astype(jnp.float32)).astype(x.dtype)


def split_columns(p):
    cuts = []
    acc = 0
    for w in IN_WIDTHS[:-1]:
        acc += w
        cuts.append(acc)
    return jnp.split(p, cuts, axis=-1)


def to_heads(t, n_heads):
    b, s, c = t.shape
    return t.reshape(b, s, n_heads, c // n_heads)


def neighborhood_attention(q, k, v, rpb):
    b, s, h, dh = q.shape
    rows = s // GRID_W
    kr = min(NA_WIN_ROWS, rows)
    kc = NA_WIN_COLS
    q = (q * (dh ** -0.5)).reshape(b, rows, GRID_W, h, dh).transpose(1, 0, 2, 3, 4)
    k = k.reshape(b, rows, GRID_W, h, dh)
    v = v.reshape(b, rows, GRID_W, h, dh)
    col = jnp.arange(GRID_W)
    col_start = jnp.clip(col - kc // 2, 0, GRID_W - kc)
    col_idx = col_start[:, None] + jnp.arange(kc)[None, :]
    col_off = col_idx - col[:, None] + (NA_WIN_COLS - 1)

    def row_block(args):
        r, q_r = args
        r_start = jnp.clip(r - kr // 2, 0, rows - kr)
        k_win = lax.dynamic_slice_in_dim(k, r_start, kr, axis=1)[:, :, col_idx]
        v_win = lax.dynamic_slice_in_dim(v, r_start, kr, axis=1)[:, :, col_idx]
        row_off = r_start + jnp.arange(kr) - r + (NA_WIN_ROWS - 1)
        bias = rpb[:, row_off[:, None, None], col_off[None, :, :]].transpose(0, 2, 1, 3)
        scores = jnp.einsum('bwhd,biwjhd->bhwij', q_r, k_win).astype(jnp.float32) + bias.astype(jnp.float32)[None]
        probs = jax.nn.softmax(scores.reshape(b, h, GRID_W, kr * kc), axis=-1).reshape(b, h, GRID_W, kr, kc)
        return jnp.einsum('bhwij,biwjhd->bwhd', probs.astype(v.dtype), v_win)

    out = lax.map(row_block, (jnp.arange(rows), q))
    return out.transpose(1, 0, 2, 3, 4).reshape(b, s, h * dh)


def token_shift(t, direction):
    if direction == 0:
        return jnp.pad(t[:, :-1], ((0, 0), (1, 0), (0, 0)))
    return jnp.pad(t[:, 1:], ((0, 0), (0, 1), (0, 0)))


def wkv7_scan(r, w, k, v, a, b, reverse):
    bsz, _, h, n = r.shape

    def step(state, inp):
        r_t, w_t, k_t, v_t, a_t, b_t = inp
        sa = jnp.einsum('bhvk,bhk->bhv', state, a_t)
        state = state * w_t[:, :, None, :] + sa[..., None] * b_t[:, :, None, :] + v_t[..., None] * k_t[:, :, None, :]
        return state, jnp.einsum('bhvk,bhk->bhv', state, r_t)

    xs = tuple(t.transpose(1, 0, 2, 3) for t in (r, w, k, v, a, b))
    _, y = lax.scan(step, jnp.zeros((bsz, h, n, n), jnp.float32), xs, reverse=reverse)
    return y.transpose(1, 0, 2, 3)


def rwkv7_bidirectional(p_r, p_k, p_v, lat_w, lat_a, lat_g, mu_rkv, mu_w, mu_a, w0, w2, a0, a2, k_k, k_a, r_k, g2, ln_g, ln_b):
    out_dtype = p_r.dtype
    f32 = lambda t: t.astype(jnp.float32)
    p_r, p_k, p_v, lat_w, lat_a, lat_g = (f32(t) for t in (p_r, p_k, p_v, lat_w, lat_a, lat_g))
    mu_rkv, mu_w, mu_a, w0, w2, a0, a2 = (f32(t) for t in (mu_rkv, mu_w, mu_a, w0, w2, a0, a2))
    k_k, k_a, r_k, g2, ln_g, ln_b = (f32(t) for t in (k_k, k_a, r_k, g2, ln_g, ln_b))
    bsz, s, c = p_r.shape
    y_sum = jnp.zeros((bsz, s, RW_HEADS, HEAD_DIM), jnp.float32)
    bonus_sum = jnp.zeros((bsz, s, RW_HEADS, HEAD_DIM), jnp.float32)
    for d in range(2):
        def mix(t, mu):
            return t + (token_shift(t, d) - t) * mu
        r = mix(p_r, mu_rkv[d, 0])
        k = mix(p_k, mu_rkv[d, 1])
        v = mix(p_v, mu_rkv[d, 2])
        w_log = -jax.nn.softplus(-(w0[d] + jnp.tanh(mix(lat_w, mu_w[d])) @ w2[d])) - 0.5
        decay = jnp.exp(-jnp.exp(w_log))
        a = jax.nn.sigmoid(a0[d] + mix(lat_a, mu_a[d]) @ a2[d])
        kk = to_heads(k * k_k, RW_HEADS)
        kk = kk / jnp.maximum(jnp.sqrt(jnp.sum(kk * kk, axis=-1, keepdims=True)), 1e-12)
        k = k * (1.0 + (a - 1.0) * k_a)
        rh, kh, vh, ah = (to_heads(t, RW_HEADS) for t in (r, k, v, a))
        y_sum = y_sum + wkv7_scan(rh, to_heads(decay, RW_HEADS), kh, vh, -kk, kk * ah, reverse=(d == 1))
        bonus_sum = bonus_sum + jnp.sum(rh * kh * r_k, axis=-1, keepdims=True) * vh
    mean = jnp.mean(y_sum, axis=-1, keepdims=True)
    var = jnp.mean(jnp.square(y_sum - mean), axis=-1, keepdims=True)
    y = ((y_sum - mean) * lax.rsqrt(var + GN_EPS)).reshape(bsz, s, c) * ln_g + ln_b
    y = y + bonus_sum.reshape(bsz, s, c)
    g = jax.nn.sigmoid(lat_g) @ g2
    return (y * g).astype(out_dtype)


def memory_attention(q, mem_h, w_mem_kv):
    b, s, _ = q.shape
    mk, mv = jnp.split(mem_h @ w_mem_kv, 2, axis=-1)
    qh = to_heads(q, MEM_HEADS) * (HEAD_DIM ** -0.5)
    kh = to_heads(mk, MEM_HEADS)
    vh = to_heads(mv, MEM_HEADS)
    scores = jnp.einsum('bshd,bmhd->bhsm', qh, kh).astype(jnp.float32)
    probs = jax.nn.softmax(scores, axis=-1)
    return jnp.einsum('bhsm,bmhd->bshd', probs.astype(vh.dtype), vh).reshape(b, s, MEM_WIDTH)


def expert_choice_ffn(h, w_router, w_gate, w_up, w_down):
    b, s, d = h.shape
    cap = EC_CAPACITY * s // N_EXPERTS
    affinity = jax.nn.softmax((h @ w_router).astype(jnp.float32), axis=-1)
    top_val, top_idx = lax.top_k(affinity.transpose(0, 2, 1), cap)
    xe = jax.vmap(lambda hb, ib: hb[ib])(h, top_idx)
    act = jax.nn.silu(jnp.einsum('becd,edf->becf', xe, w_gate)) * jnp.einsum('becd,edf->becf', xe, w_up)
    ye = jnp.einsum('becf,efd->becd', act, w_down) * top_val[..., None].astype(h.dtype)
    return jax.vmap(lambda yb, ib: jnp.zeros((s, d), yb.dtype).at[ib.reshape(-1)].add(yb.reshape(-1, d)))(ye, top_idx)


def setup_inputs(seed: int = 0) -> dict:
    key = jax.random.key(seed)
    ks = iter(jax.random.split(key, 40))
    L = DEPTH

    def nrm(shape, scale):
        return scale * jax.random.normal(next(ks), shape, jnp.float32)

    def uni(shape):
        return jax.random.uniform(next(ks), shape, jnp.float32, 0.0, 1.0)

    decay_ramp = jnp.linspace(-6.5, -1.5, RW_WIDTH, dtype=jnp.float32)
    return {
        'x': nrm((BATCH, SEQ, D_MODEL), 1.0),
        'mem': nrm((BATCH, N_MEM, D_MODEL), 1.0),
        'norm_mix_g': 1.0 + nrm((L, D_MODEL), 0.02),
        'norm_mem_g': 1.0 + nrm((L, D_MODEL), 0.02),
        'w_in': nrm((L, D_MODEL, D_IN), D_MODEL ** -0.5),
        'na_rpb': nrm((L, NA_HEADS, 2 * NA_WIN_ROWS - 1, 2 * NA_WIN_COLS - 1), 0.05),
        'rw_mu_rkv': uni((L, 2, 3, RW_WIDTH)),
        'rw_mu_w': uni((L, 2, DECAY_LORA)),
        'rw_mu_a': uni((L, 2, AAA_LORA)),
        'rw_w0': decay_ramp + nrm((L, 2, RW_WIDTH), 0.1),
        'rw_w2': nrm((L, 2, DECAY_LORA, RW_WIDTH), 0.1),
        'rw_a0': nrm((L, 2, RW_WIDTH), 0.1),
        'rw_a2': nrm((L, 2, AAA_LORA, RW_WIDTH), 0.1),
        'rw_k_k': 0.85 + nrm((L, RW_WIDTH), 0.02),
        'rw_k_a': 1.0 + nrm((L, RW_WIDTH), 0.02),
        'rw_r_k': nrm((L, RW_HEADS, HEAD_DIM), 0.1),
        'rw_g2': nrm((L, GATE_LORA, RW_WIDTH), GATE_LORA ** -0.5),
        'rw_ln_g': 1.0 + nrm((L, RW_WIDTH), 0.02),
        'rw_ln_b': nrm((L, RW_WIDTH), 0.02),
        'w_mem_kv': nrm((L, D_MODEL, 2 * MEM_WIDTH), D_MODEL ** -0.5),
        'w_branch_na': nrm((L, NA_WIDTH, D_MODEL), NA_WIDTH ** -0.5),
        'w_branch_rw': nrm((L, RW_WIDTH, D_MODEL), RW_WIDTH ** -0.5),
        'w_branch_mem': nrm((L, MEM_WIDTH, D_MODEL), MEM_WIDTH ** -0.5),
        'w_out': nrm((L, D_MODEL, D_MODEL), D_MODEL ** -0.5),
        'norm_ffn_g': 1.0 + nrm((L, D_MODEL), 0.02),
        'w_router': nrm((L, D_MODEL, N_EXPERTS), D_MODEL ** -0.5),
        'w_exp_gate': nrm((L, N_EXPERTS, D_MODEL, EXPERT_FF), D_MODEL ** -0.5),
        'w_exp_up': nrm((L, N_EXPERTS, D_MODEL, EXPERT_FF), D_MODEL ** -0.5),
        'w_exp_down': nrm((L, N_EXPERTS, EXPERT_FF, D_MODEL), EXPERT_FF ** -0.5),
        'norm_final_g': 1.0 + nrm((D_MODEL,), 0.02),
    }


def reference(x, mem, norm_mix_g, norm_mem_g, w_in, na_rpb, rw_mu_rkv, rw_mu_w, rw_mu_a, rw_w0, rw_w2, rw_a0, rw_a2, rw_k_k, rw_k_a, rw_r_k, rw_g2, rw_ln_g, rw_ln_b, w_mem_kv, w_branch_na, w_branch_rw, w_branch_mem, w_out, norm_ffn_g, w_router, w_exp_gate, w_exp_up, w_exp_down, norm_final_g):
    for l in range(DEPTH):
        h = rms_norm(x, norm_mix_g[l])
        (na_q, na_k, na_v, p_r, p_k, p_v, lat_w, lat_a, lat_g, mem_q,
         gate_na, gate_rw, gate_mem) = split_columns(h @ w_in[l])
        y_na = neighborhood_attention(to_heads(na_q, NA_HEADS), to_heads(na_k, NA_HEADS), to_heads(na_v, NA_HEADS), na_rpb[l])
        y_rw = rwkv7_bidirectional(p_r, p_k, p_v, lat_w, lat_a, lat_g, rw_mu_rkv[l], rw_mu_w[l], rw_mu_a[l], rw_w0[l], rw_w2[l], rw_a0[l], rw_a2[l], rw_k_k[l], rw_k_a[l], rw_r_k[l], rw_g2[l], rw_ln_g[l], rw_ln_b[l])
        y_mem = memory_attention(mem_q, rms_norm(mem, norm_mem_g[l]), w_mem_kv[l])
        merged = (jax.nn.sigmoid(gate_na) * (y_na @ w_branch_na[l])
                  + jax.nn.sigmoid(gate_rw) * (y_rw @ w_branch_rw[l])
                  + jax.nn.sigmoid(gate_mem) * (y_mem @ w_branch_mem[l]))
        x = x + merged @ w_out[l]
        x = x + expert_choice_ffn(rms_norm(x, norm_ffn_g[l]), w_router[l], w_exp_gate[l], w_exp_up[l], w_exp_down[l])
    return rms_norm(x, norm_final_g)
```

```python
import numpy as np
from contextlib import ExitStack
import concourse.bass as bass
import concourse.mybir as mybir
from concourse.bass_utils import run_bass_kernel_spmd

F32 = mybir.dt.float32
BF16 = mybir.dt.bfloat16
I32 = mybir.dt.int32
AF = mybir.ActivationFunctionType
ALU = mybir.AluOpType
AX = mybir.AxisListType

S = 8192
D = 1024
NT = S // 128
DEBUG = False
PHASES = 99
SEM_ROT = 20000
NA_BARRIER = False
RW_CHUNKS = 128
N_EXP = 16
RAW_OUT = False
D_BARRIER = False
DBG_D = False
ALL_EXT = False
DBG_NAMES = ()


class Prog:
    ENG = ("pe", "dve", "act", "pool", "sp")

    def __init__(self, nc, es):
        self.nc = nc
        self.es = es
        self.streams = {e: [] for e in self.ENG}
        self.cur = {}
        self.res = {}
        self.seen = {e: {} for e in self.ENG}
        self.dmasem = {}
        self.freed = []
        self.retired = []
        self.nsem = 0
        for e in self.ENG:
            self._newsem(e)

    def _mksem(self, name):
        self.nsem += 1
        return self.es.enter_context(self.nc.semaphore(f"{name}_{self.nsem}"))

    def _newsem(self, e):
        self.cur[e] = [self._mksem("s" + e), 0]

    def _need(self, eng, waits, sv):
        if sv is None:
            return
        sem, val = sv
        k = id(sem)
        if self.seen[eng].get(k, (None, 0))[1] >= val:
            return
        if k not in waits or waits[k][1] < val:
            waits[k] = (sem, val)

    def _deps(self, eng, reads, writes):
        waits = {}
        for key in reads:
            r = self.res.get(key)
            if r is not None:
                self._need(eng, waits, r[0])
        for key in writes:
            r = self.res.get(key)
            if r is not None:
                self._need(eng, waits, r[0])
                for sv in r[1].values():
                    self._need(eng, waits, sv)
        for k, sv in waits.items():
            self.seen[eng][k] = sv
        return list(waits.values())

    def _mark(self, tag, sv, reads, writes):
        for key in reads:
            r = self.res.setdefault(key, [None, {}])
            r[1][tag] = sv
        for key in writes:
            self.res[key] = [sv, {}]

    def op(self, eng, fns, reads=(), writes=()):
        if callable(fns):
            fns = [fns]
        waits = self._deps(eng, reads, writes)
        c = self.cur[eng]
        if c[1] >= SEM_ROT:
            self._newsem(eng)
            c = self.cur[eng]
        c[1] += 1
        sv = (c[0], c[1])
        self.streams[eng].append((waits, fns, (c[0], 1)))
        self._mark(eng, sv, reads, writes)
        return sv

    def dma(self, q, fn, key, reads=(), writes=()):
        waits = self._deps(q, reads, writes)
        d = self.dmasem.get(key)
        if d is None or d[1] >= 30000:
            if d is not None:
                self.retired.append(d)
            d = self.dmasem[key] = self._getdsem()
        d[1] += 16
        sv = (d[0], d[1])
        self.streams[q].append((waits, [fn], (d[0], 16)))
        self._mark(("dma", key), sv, reads, writes)
        return sv

    def _getdsem(self):
        while self.freed:
            d = self.freed.pop()
            if d[1] < 20000:
                return d
        return [self._mksem("d"), 0]

    def barrier(self):
        targets = [(d[0], d[1]) for d in self.retired]
        self.retired = []
        for e in self.ENG:
            c = self.cur[e]
            if c[1] > 0:
                targets.append((c[0], c[1]))
        for d in self.dmasem.values():
            targets.append((d[0], d[1]))
        for e in self.ENG:
            ws = [t for t in targets if self.seen[e].get(id(t[0]), (None, 0))[1] < t[1]]
            for t in ws:
                self.seen[e][id(t[0])] = t
            self.streams[e].append((ws, [], None))
        self.res = {}
        self.freed.extend(self.dmasem.values())
        self.dmasem = {}

    def emit(self):
        engmap = {"pe": "tensor", "dve": "vector", "act": "scalar", "pool": "gpsimd", "sp": "sync"}
        with self.nc.Block() as block:
            for e in self.ENG:
                stream = self.streams[e]

                def body(engine, stream=stream):
                    for waits, fns, inc in stream:
                        for sem, val in waits:
                            engine.wait_ge(sem, val)
                        ins = None
                        for f in fns:
                            ins = f(engine)
                        if inc is not None:
                            ins.then_inc(inc[0], inc[1])
                getattr(block, engmap[e])(body)


_BREG = {}


def breg(e, val):
    if val not in _BREG:
        _BREG[val] = e.to_reg(val)
    return _BREG[val]


def mm(P, out, pairs, reads, writes):
    n = len(pairs)
    fns = [(lambda e, l=l, r=r, i=i: e.matmul(out, l, r, start=(i == 0), stop=(i == n - 1)))
           for i, (l, r) in enumerate(pairs)]
    return P.op("pe", fns, reads, writes)


class RR:
    def __init__(self, items):
        self.items = list(items)
        self.i = 0

    def next(self):
        v = self.items[self.i % len(self.items)]
        self.i += 1
        return v


def build():
    _BREG.clear()
    nc = bass.Bass("TRN2", target_bir_lowering=False)
    try:
        nc.allow_low_precision("bf16 matmul operands with fp32 accumulation")
    except Exception:
        pass
    outs = {}

    def din(name, shape, dt=F32):
        return nc.dram_tensor(name, list(shape), dt, kind="ExternalInput").ap()

    def scratch(name, shape, dt):
        kind = "ExternalOutput" if ((DEBUG and name in DBG_NAMES) or ALL_EXT) else "Internal"
        t = nc.dram_tensor(name, list(shape), dt, kind=kind).ap()
        if DEBUG:
            outs[name] = t
        return t

    x_d = din("x", [S, D])
    mem_d = din("mem", [256, D])
    w_in = din("w_in", [D, 5888])
    gmixT = din("gmixT", [128, 8])
    gmemT = din("gmemT", [128, 8])
    w_mem_kv = din("w_mem_kv", [D, 512])
    identb_d = din("identb_in", [128, 128], BF16)
    identf_d = din("identf_in", [128, 128])
    out_d = nc.dram_tensor("out", [S, D], F32, kind="ExternalOutput").ap()

    hT_d = scratch("hT_s", [D, S], BF16)
    qk_d = scratch("qk_s", [12, 64, S], BF16)
    v_d = scratch("v_s", [S, 390], BF16)
    prw_d = scratch("prw_s", [22, 64, S + 2], F32)
    mq_d = scratch("mq_s", [4, 64, S], BF16)
    ynaT_d = scratch("ynaT_s", [384, S], BF16)
    ymemT_d = scratch("ymemT_s", [256, S], BF16)
    biasP_d = din("biasP_in", [128, 6 * 14 * 64])
    maskP_d = din("maskP_in", [128, 64])
    mu_d = din("mu_in", [64, 40])
    w2a_d = din("w2a_in", [65, 768])
    a2a_d = din("a2a_in", [65, 768])
    kkp_d = din("kkp_in", [64, 6])
    kap_d = din("kap_in", [64, 6])
    rkp_d = din("rkp_in", [64, 6])
    g2_d = din("g2_in", [64, 768])
    lng_d = din("lng_in", [64, 384])
    lnb_d = din("lnb_in", [64, 384])
    msk_d = din("msk_in", [64, 192])
    jrev_d = din("jrev_in", [64, 64])
    wbna_d = din("w_branch_na", [384, D])
    wbrw_d = din("w_branch_rw", [384, D])
    wbmem_d = din("w_branch_mem", [256, D])
    wout_d = din("w_out", [D, D])
    wrt_d = din("w_router", [D, 16])
    gffn_d = din("gffn_in", [128, D])
    tail_d = din("tail_in", [128, 256])
    acc_d = scratch("acc_s", [S, D], F32)
    ltm_d = din("ltm_in", [128, 128])
    gfin_d = din("gfin_in", [128, D])
    wgate_d = din("w_exp_gate", [16, D, 2048])
    wup_d = din("w_exp_up", [16, D, 2048])
    wdown_d = din("w_exp_down", [16, 2048, D])
    xe_d = [scratch(f"xe_s{e}", [1024, 1028], BF16) for e in range(16)]
    rank_dbg = scratch("rank_dbg", [128, 1024], F32) if DEBUG else None
    h2_d = scratch("h2_s", [S, 1028], BF16)
    aff_d = scratch("aff_s", [S, 16], F32)
    yb_d = scratch("yb_s", [S, 2, 384], F32)
    yrwT_d = scratch("yrwT_s", [384, S], BF16)

    with ExitStack() as es:
        P = Prog(nc, es)

        def sb(name, shape, dt, stack=es):
            return stack.enter_context(nc.sbuf_tensor("sb_" + name, list(shape), dt))

        def ps(name, shape, dt, stack=es):
            return stack.enter_context(nc.psum_tensor("ps_" + name, list(shape), dt))

        identb = sb("identb", [128, 128], BF16)
        identf = sb("identf", [128, 128], F32)
        gmix = sb("gmix", [128, 8], F32)
        gmem = sb("gmem", [128, 8], F32)
        mkT = sb("mkT", [64, 4, 256], BF16)
        mv = sb("mv", [128, 2, 4, 65], BF16)
        zero = sb("zero", [64, 32], F32)
        P.dma("sp", lambda e: e.dma_start(out=identb[:], in_=identb_d), "c_identb", writes=["identb"])
        P.dma("sp", lambda e: e.dma_start(out=identf[:], in_=identf_d), "c_identf", writes=["identf"])
        P.dma("sp", lambda e: e.dma_start(out=gmix[:], in_=gmixT), "c_gmix", writes=["gmix"])
        P.dma("sp", lambda e: e.dma_start(out=gmem[:], in_=gmemT), "c_gmem", writes=["gmem"])
        P.op("dve", lambda e: e.memset(zero[:], 0.0), writes=["zero"])
        P.op("dve", lambda e: e.memset(mv[:], 1.0), writes=["mv"])

        with ExitStack() as pa:
            NCOL = 2816
            wA = sb("wA", [128, 8, NCOL], BF16, pa)
            wkv = sb("wkv", [128, 8, 512], BF16, pa)
            xt = [sb(f"xt{i}", [128, D], F32, pa) for i in range(3)]
            xn = [sb(f"xn{i}", [128, D], BF16, pa) for i in range(2)]
            sq = sb("sqjunk", [128, D], BF16, pa)
            ss = [sb(f"ss{i}", [128, 1], F32, pa) for i in range(3)]
            rs = [sb(f"rs{i}", [128, 1], F32, pa) for i in range(3)]
            hTb = [sb(f"hTb{i}", [128, 8, 512], BF16, pa) for i in range(2)]
            qk_sb = [sb(f"qk_sb{i}", [64, 12, 512], BF16, pa) for i in range(2)]
            v_sb = [sb(f"v_sb{i}", [128, 4, 390], BF16, pa) for i in range(2)]
            rw_sb = [sb(f"rw_sb{i}", [64, 11, 512], F32, pa) for i in range(2)]
            mq_sb = [sb(f"mq_sb{i}", [64, 4, 512], BF16, pa) for i in range(2)]
            ptr = [ps(f"ptr{i}", [128, 8, 128], BF16, pa) for i in range(2)]
            pacc = [ps(f"pacc{i}", [128, 512], F32, pa) for i in range(5)]

            w_in_v = w_in.rearrange("(c p) n -> p c n", p=128)
            for c in range(8):
                P.dma("pool", lambda e, c=c: e.dma_start(out=wA[:, c, :], in_=w_in_v[:, c, 0:NCOL]),
                      "wA", writes=[f"wA{c}"])
            wkv_v = w_mem_kv.rearrange("(c p) n -> p c n", p=128)
            P.dma("pool", lambda e: e.dma_start(out=wkv[:], in_=wkv_v), "wkv", writes=["wkv"])
            for g0 in (0, 11):
                for col in (0, S + 1):
                    P.dma("sp", lambda e, g0=g0, col=col: e.dma_start(
                        out=prw_d[g0:g0 + 11, :, col:col + 1].rearrange("g p t -> p g t"),
                        in_=zero[:, 0:11].unsqueeze(2), allow_slow_non_contiguous=True), "zpad", reads=["zero"], writes=["prw_pad"])
            for i in range(2):
                P.op("pool", lambda e, i=i: e.memset(v_sb[i][:], 1.0), writes=[f"v_sb{i}"])

            evq = RR(["act", "dve"])
            pq = RR(range(5))
            ldq = RR(["sp", "act"])

            def evac(dst, src, reads, writes, eng=None):
                eng = eng or evq.next()
                if eng == "act":
                    P.op("act", lambda e: e.copy(out=dst, in_=src), reads, writes)
                else:
                    P.op("dve", lambda e: e.tensor_copy(out=dst, in_=src), reads, writes)

            def norm_tile(src_ap, ti, gtile, gkey, dst, dstkey, col0):
                b3 = ti % 3
                b2 = ti % 2
                P.dma(ldq.next(), lambda e: e.dma_start(out=xt[b3][:], in_=src_ap), f"xt{b3}", writes=[f"xt{b3}"])
                P.op("act", lambda e: e.activation(out=sq[:], in_=xt[b3][:], func=AF.Square, accum_out=ss[b3][:]),
                     reads=[f"xt{b3}"], writes=["sq", f"ss{b3}"])
                P.op("act", lambda e: e.activation(out=rs[b3][:], in_=ss[b3][:], func=AF.Sqrt, scale=1.0 / D, bias=eps_t[:]),
                     reads=[f"ss{b3}", "eps"], writes=[f"rs{b3}"])
                P.op("dve", lambda e: e.reciprocal(out=rs[b3][:], in_=rs[b3][:]), reads=[f"rs{b3}"], writes=[f"rs{b3}"])
                P.op("act", lambda e: e.activation(out=xn[b2][:], in_=xt[b3][:], func=AF.Copy, scale=rs[b3][:]),
                     reads=[f"xt{b3}", f"rs{b3}"], writes=[f"xn{b2}"])
                P.op("pe", [(lambda e, c=c: e.transpose(out=ptr[b2][:, c, :], in_=xn[b2][:, c * 128:(c + 1) * 128], identity=identb[:]))
                            for c in range(8)], reads=[f"xn{b2}", "identb"], writes=[f"ptr{b2}"])
                P.op("dve", lambda e: e.tensor_tensor(out=dst[:, :, col0:col0 + 128], in0=ptr[b2][:],
                                                      in1=gtile[:].unsqueeze(2).to_broadcast([128, 8, 128]), op=ALU.mult),
                     reads=[f"ptr{b2}", gkey], writes=[dstkey])

            eps_t = sb("eps_t", [128, 1], F32, pa)
            P.op("dve", lambda e: e.memset(eps_t[:], 1e-6), writes=["eps"])

            mhT = sb("mhT", [128, 8, 256], BF16, pa)
            for t in range(2):
                norm_tile(mem_d[t * 128:(t + 1) * 128, :], t, gmem, "gmem", mhT, "mhT", t * 128)
            for hh in range(4):
                pi = pq.next()
                mm(P, pacc[pi][0:64, 0:256], [(wkv[:, c, hh * 64:(hh + 1) * 64], mhT[:, c, :]) for c in range(8)],
                   reads=["wkv", "mhT"], writes=[f"pacc{pi}"])
                evac(mkT[:, hh, :], pacc[pi][0:64, 0:256], [f"pacc{pi}"], ["mkT"])
            for t in range(2):
                pi = pq.next()
                mm(P, pacc[pi][:, 0:256], [(mhT[:, c, t * 128:(t + 1) * 128], wkv[:, c, 256:512]) for c in range(8)],
                   reads=["wkv", "mhT"], writes=[f"pacc{pi}"])
                evac(mv[:, t, :, 0:64], pacc[pi][:, 0:256].rearrange("p (h d) -> p h d", h=4), [f"pacc{pi}"], ["mv"])

            wAkeys = [f"wA{c}" for c in range(8)]
            for blk in range(S // 512):
                hb = blk % 2
                t0 = blk * 512
                for j in range(4):
                    ti = blk * 4 + j
                    norm_tile(x_d[ti * 128:(ti + 1) * 128, :], ti + 2, gmix, "gmix", hTb[hb], f"hTb{hb}", j * 128)
                P.dma("sp", lambda e, hb=hb, t0=t0: e.dma_start(
                    out=hT_d.rearrange("(c p) t -> p c t", p=128)[:, :, t0:t0 + 512], in_=hTb[hb][:]),
                    f"st_hT{hb}", reads=[f"hTb{hb}"], writes=["hT_d"])
                for g in range(12):
                    pi = pq.next()
                    mm(P, pacc[pi][0:64, :], [(wA[:, c, g * 64:(g + 1) * 64], hTb[hb][:, c, :]) for c in range(8)],
                       reads=wAkeys + [f"hTb{hb}"], writes=[f"pacc{pi}"])
                    evac(qk_sb[hb][:, g, :], pacc[pi][0:64, :], [f"pacc{pi}"], [f"qk_sb{hb}"])
                P.dma("sp", lambda e, hb=hb, t0=t0: e.dma_start(
                    out=qk_d[:, :, t0:t0 + 512].rearrange("g p t -> p g t"), in_=qk_sb[hb][:]),
                    f"st_qk{hb}", reads=[f"qk_sb{hb}"], writes=["qk_d"])
                for j in range(4):
                    pi = pq.next()
                    mm(P, pacc[pi][:, 0:384], [(hTb[hb][:, c, j * 128:(j + 1) * 128], wA[:, c, 768:1152]) for c in range(8)],
                       reads=wAkeys + [f"hTb{hb}"], writes=[f"pacc{pi}"])
                    evac(v_sb[hb][:, j, :].rearrange("p (h d) -> p h d", h=6)[:, :, 0:64],
                         pacc[pi][:, 0:384].rearrange("p (h d) -> p h d", h=6), [f"pacc{pi}"], [f"v_sb{hb}"])
                P.dma("act", lambda e, hb=hb, t0=t0: e.dma_start(
                    out=v_d[t0:t0 + 512, :].rearrange("(n p) c -> p n c", p=128), in_=v_sb[hb][:]),
                    f"st_v{hb}", reads=[f"v_sb{hb}"], writes=["v_d"])
                for half in range(2):
                    for gg in range(11):
                        g = half * 11 + gg
                        pi = pq.next()
                        c0 = 1152 + g * 64
                        mm(P, pacc[pi][0:64, :], [(wA[:, c, c0:c0 + 64], hTb[hb][:, c, :]) for c in range(8)],
                           reads=wAkeys + [f"hTb{hb}"], writes=[f"pacc{pi}"])
                        evac(rw_sb[half][:, gg, :], pacc[pi][0:64, :], [f"pacc{pi}"], [f"rw_sb{half}"])
                    P.dma("sp", lambda e, half=half, t0=t0: e.dma_start(
                        out=prw_d[half * 11:half * 11 + 11, :, 1 + t0:1 + t0 + 512].rearrange("g p t -> p g t"),
                        in_=rw_sb[half][:]), f"st_rw{half}", reads=[f"rw_sb{half}"], writes=["prw_d"])
                for g in range(4):
                    pi = pq.next()
                    c0 = 2560 + g * 64
                    mm(P, pacc[pi][0:64, :], [(wA[:, c, c0:c0 + 64], hTb[hb][:, c, :]) for c in range(8)],
                       reads=wAkeys + [f"hTb{hb}"], writes=[f"pacc{pi}"])
                    evac(mq_sb[hb][:, g, :], pacc[pi][0:64, :], [f"pacc{pi}"], [f"mq_sb{hb}"])
                P.dma("act", lambda e, hb=hb, t0=t0: e.dma_start(
                    out=mq_d[:, :, t0:t0 + 512].rearrange("g p t -> p g t"), in_=mq_sb[hb][:]),
                    f"st_mq{hb}", reads=[f"mq_sb{hb}"], writes=["mq_d"])
            P.barrier()

        if PHASES >= 2:
          with ExitStack() as pb:
            EP = sb("EP", [128, 6, 14, 64], BF16, pb)
            biasP = sb("biasP", [128, 6 * 14 * 64], F32, pb)
            maskP = sb("maskP", [128, 64], F32, pb)
            bank = [ps(f"bank{i}", [128, 512], F32, pb) for i in range(6)]
            pbt = ps("pbt", [128, 1024], BF16, pb)
            P.dma("sp", lambda e: e.dma_start(out=biasP[:], in_=biasP_d), "c_biasP", writes=["biasP"])
            P.dma("sp", lambda e: e.dma_start(out=maskP[:], in_=maskP_d), "c_maskP", writes=["maskP"])
            for h in range(6):
                P.op("act", lambda e, h=h: e.activation(out=biasP[:, h * 896:(h + 1) * 896], in_=biasP[:, h * 896:(h + 1) * 896], func=AF.Exp),
                     reads=["biasP"], writes=["biasP"])
                P.op("dve", lambda e, h=h: e.tensor_tensor(out=EP[:, h, :, :], in0=biasP[:, h * 896:(h + 1) * 896].rearrange("p (a q) -> p a q", q=64),
                                                           in1=maskP[:].unsqueeze(1).to_broadcast([128, 14, 64]), op=ALU.mult),
                     reads=["biasP", "maskP"], writes=["EP"])
            qrow = [sb(f"qrow{i}", [64, 6, 64], BF16, pb) for i in range(2)]
            kwin = [sb(f"kwin{i}", [64, 6, 512], BF16, pb) for i in range(2)]
            vwin = [sb(f"vwin{i}", [128, 4, 390], BF16, pb) for i in range(2)]
            pex = [sb(f"pex{i}", [128, 2, 4, 64], BF16, pb) for i in range(3)]
            rec = [sb(f"rec{i}", [128, 6], F32, pb) for i in range(2)]
            yrow = [sb(f"yrow{i}", [128, 384], BF16, pb) for i in range(2)]
            ynaT_sb = [sb(f"ynaT_sb{i}", [128, 3, 512], BF16, pb) for i in range(2)]
            mq_b = [sb(f"mq_b{i}", [64, 4, 512], BF16, pb) for i in range(2)]
            mpex = [sb(f"mpex{i}", [128, 2, 512], BF16, pb) for i in range(4)]
            ymT_sb = [sb(f"ymT_sb{i}", [128, 2, 512], BF16, pb) for i in range(2)]
            bq = RR(range(6))
            BK = lambda k: [f"bank{k}"] + [f"bank{k}_{h}" for h in range(6)]

            def na_hp(r, b2, ro0, hp, pvb, pv):
                sbk = bq.next()
                sT = bank[sbk][:].rearrange("p (a j q) -> p a j q", a=2, j=4)
                fns = []
                for a in range(2):
                    for j in range(4):
                        fns.append(lambda e, a=a, j=j: e.matmul(sT[:, a, j, :], kwin[b2][:, hp * 2 + a, j * 128:(j + 1) * 128],
                                                                qrow[b2][:, hp * 2 + a, :], start=True, stop=True))
                P.op("pe", fns, reads=[f"kwin{b2}", f"qrow{b2}"], writes=BK(sbk))
                P.op("act", lambda e: e.activation(out=pex[hp][:], in_=sT, func=AF.Exp, scale=0.125),
                     reads=BK(sbk), writes=[f"pex{hp}"])
                a0 = (ro0 % 2) * 7 + ro0 // 2
                P.op("dve", lambda e: e.tensor_tensor(out=pex[hp][:], in0=pex[hp][:], in1=EP[:, hp * 2:hp * 2 + 2, a0:a0 + 4, :], op=ALU.mult),
                     reads=[f"pex{hp}", "EP"], writes=[f"pex{hp}"])
                for a in range(2):
                    h = hp * 2 + a
                    mm(P, pv[:, h, :], [(pex[hp][:, a, j, :], vwin[b2][:, j, h * 65:(h + 1) * 65]) for j in range(4)],
                       reads=[f"pex{hp}", f"vwin{b2}"], writes=[f"bank{pvb}_{h}"])

            def na_row(r):
                b2 = r % 2
                rs_ = min(max(r - 4, 0), 120)
                ro0 = rs_ - r + 7
                blk = r // 8
                P.dma("sp", lambda e: e.dma_start(out=qrow[b2][:], in_=qk_d[0:6, :, r * 64:(r + 1) * 64].rearrange("g p t -> p g t")),
                      f"ld_q{b2}", writes=[f"qrow{b2}"])
                P.dma("sp", lambda e: e.dma_start(out=kwin[b2][:], in_=qk_d[6:12, :, rs_ * 64:rs_ * 64 + 512].rearrange("g p t -> p g t")),
                      f"ld_k{b2}", writes=[f"kwin{b2}"])
                P.dma("act", lambda e: e.dma_start(out=vwin[b2][:], in_=v_d[rs_ * 64:rs_ * 64 + 512, :].rearrange("(n p) c -> p n c", p=128)),
                      f"ld_v{b2}", writes=[f"vwin{b2}"])
                pvb = bq.next()
                pv = bank[pvb][0:64, 0:390].rearrange("p (h d) -> p h d", h=6)
                for hp in range(3):
                    na_hp(r, b2, ro0, hp, pvb, pv)
                pvkeys = [f"bank{pvb}_{h}" for h in range(6)]
                P.op("dve", lambda e: e.reciprocal(out=rec[b2][0:64, :], in_=pv[:, :, 64]), reads=pvkeys, writes=[f"rec{b2}"])
                P.op("dve", lambda e: e.tensor_tensor(out=yrow[b2][0:64, :].rearrange("p (h d) -> p h d", h=6), in0=pv[:, :, 0:64],
                                                      in1=rec[b2][0:64, :].unsqueeze(2).to_broadcast([64, 6, 64]), op=ALU.mult),
                     reads=pvkeys + [f"rec{b2}"], writes=[f"yrow{b2}"] + pvkeys)
                P.op("pe", [(lambda e, c=c: e.transpose(out=pbt[:, c * 64:(c + 1) * 64], in_=yrow[b2][0:64, c * 128:(c + 1) * 128], identity=identb[0:64, 0:64]))
                            for c in range(3)], reads=[f"yrow{b2}", "identb"], writes=["pbt_na"])
                yb = blk % 2
                P.op("act", lambda e: e.copy(out=ynaT_sb[yb][:, :, (r % 8) * 64:(r % 8) * 64 + 64], in_=pbt[:, 0:192].rearrange("p (c q) -> p c q", c=3)),
                     reads=["pbt_na"], writes=[f"ynaT_sb{yb}"])
                if r % 8 == 7:
                    t0 = blk * 512
                    P.dma("sp", lambda e: e.dma_start(out=ynaT_d.rearrange("(c p) t -> p c t", p=128)[:, :, t0:t0 + 512], in_=ynaT_sb[yb][:]),
                          f"st_yna{yb}", reads=[f"ynaT_sb{yb}"], writes=["ynaT_d"])
                    mem_block(blk)

            def mem_s(yb, h, kt):
                sbk = bq.next()
                mm(P, bank[sbk][:], [(mkT[:, h, kt * 128:(kt + 1) * 128], mq_b[yb][:, h, :])], reads=["mkT", f"mq_b{yb}"], writes=BK(sbk))
                P.op("act", lambda e: e.activation(out=mpex[h][:, kt, :], in_=bank[sbk][:], func=AF.Exp, scale=0.125),
                     reads=BK(sbk), writes=[f"mpex{h}"])

            def mem_pv(yb, qt):
                pvb = bq.next()
                pvm = bank[pvb][:, 0:260].rearrange("p (h d) -> p h d", h=4)
                for h in range(4):
                    mm(P, pvm[:, h, :], [(mpex[h][:, kt, qt * 128:(qt + 1) * 128], mv[:, kt, h, :]) for kt in range(2)],
                       reads=[f"mpex{h}", "mv"], writes=[f"bank{pvb}_{h}"])
                pk = [f"bank{pvb}_{h}" for h in range(4)]
                rb = qt % 2
                P.op("dve", lambda e: e.reciprocal(out=rec[rb][:, 0:4], in_=pvm[:, :, 64]), reads=pk, writes=[f"rec{rb}"])
                P.op("dve", lambda e: e.tensor_tensor(out=yrow[rb][:, 0:256].rearrange("p (h d) -> p h d", h=4), in0=pvm[:, :, 0:64],
                                                      in1=rec[rb][:, 0:4].unsqueeze(2).to_broadcast([128, 4, 64]), op=ALU.mult),
                     reads=pk + [f"rec{rb}"], writes=[f"yrow{rb}"] + pk)
                P.op("pe", [(lambda e, c=c: e.transpose(out=pbt[:, 512 + c * 128:512 + (c + 1) * 128], in_=yrow[rb][:, c * 128:(c + 1) * 128], identity=identb[:]))
                            for c in range(2)], reads=[f"yrow{rb}", "identb"], writes=["pbt_m"])
                P.op("act", lambda e: e.copy(out=ymT_sb[yb][:, :, qt * 128:(qt + 1) * 128], in_=pbt[:, 512:768].rearrange("p (c q) -> p c q", c=2)),
                     reads=["pbt_m"], writes=[f"ymT_sb{yb}"])

            def mem_block(blk):
                yb = blk % 2
                t0 = blk * 512
                P.dma("act", lambda e: e.dma_start(out=mq_b[yb][:], in_=mq_d[:, :, t0:t0 + 512].rearrange("g p t -> p g t")),
                      f"ld_mq{yb}", writes=[f"mq_b{yb}"])
                for h in range(4):
                    for kt in range(2):
                        mem_s(yb, h, kt)
                for qt in range(4):
                    mem_pv(yb, qt)
                P.dma("sp", lambda e: e.dma_start(out=ymemT_d.rearrange("(c p) t -> p c t", p=128)[:, :, t0:t0 + 512], in_=ymT_sb[yb][:]),
                      f"st_ym{yb}", reads=[f"ymT_sb{yb}"], writes=["ymemT_d"])

            for r in range(128):
                na_row(r)
            P.barrier()

        if PHASES >= 3:
          with ExitStack() as pc:
            C0 = 0.6065306597126334
            cst = {}
            for nm, shp, src in (("mu", [64, 40], mu_d), ("w2a", [65, 768], w2a_d), ("a2a", [65, 768], a2a_d), ("kkp", [64, 6], kkp_d),
                                 ("kap", [64, 6], kap_d), ("rkp", [64, 6], rkp_d), ("g2", [64, 768], g2_d), ("lng", [64, 384], lng_d),
                                 ("lnb", [64, 384], lnb_d), ("msk", [64, 192], msk_d), ("jrev", [64, 64], jrev_d)):
                cst[nm] = sb("c_" + nm, shp, F32, pc)
                P.dma("sp", (lambda e, t=cst[nm], src=src: e.dma_start(out=t[:], in_=src)), "c_" + nm, writes=[nm])
            oma = sb("oma", [64, 6], F32, pc)
            P.op("dve", lambda e: e.tensor_scalar(out=oma[:], in0=cst["kap"][:], scalar1=-1.0, scalar2=1.0, op0=ALU.mult, op1=ALU.add),
                 reads=["kap"], writes=["oma"])
            ones64 = sb("ones64", [64, 384], F32, pc)
            P.op("dve", lambda e: e.memset(ones64[:], 1.0), writes=["ones64"])
            eps12 = sb("eps12", [64, 1], F32, pc)
            P.op("dve", lambda e: e.memset(eps12[:], 64e-5), writes=["eps12"])
            offs = [sb(f"offs{i}", [64, 6], F32, pc) for i in range(2)]
            twa = [sb(f"twa{i}", [65, 2, 64], F32, pc) for i in range(2)]
            for i in range(2):
                P.op("dve", lambda e, i=i: e.memset(offs[i][:], 0.0), writes=[f"offs{i}"])
                P.op("dve", lambda e, i=i: e.memset(twa[i][:], 1.0), writes=[f"twa{i}"])
            Hs = [sb(f"H{i}", [64, 384], F32, pc) for i in range(2)]
            rbank = [ps(f"rbank{i}", [64, 512], F32, pc) for i in range(7)]
            rbt = ps("rbt", [128, 1024], BF16, pc)
            rq = RR(range(7))
            names3 = ["mx", "df"]
            names = ["sg", "al", "L", "Lx", "Ld", "Pinc", "Pinv", "Pexc", "Pdec", "kkr", "sq", "nrm", "kk", "t1", "k2", "bv",
                     "at", "bt", "kt", "rt", "bh", "kh", "rk", "Vt", "bhT", "khT", "MabT", "Nab", "MakT", "MrbT", "MrkT",
                     "X0", "X1", "Na", "Nb", "Ma", "Mb", "W", "U", "dgP", "yo", "ybl", "cen", "sqc", "sgg", "yn"]
            tl = {}
            for i in range(2):
                tl[("X", i)] = sb(f"rwX{i}", [64, 22, 66], F32, pc)
                tl[("yst", i)] = sb(f"rw_yst{i}", [64, 2, 384], F32, pc)
                tl[("yld", i)] = sb(f"rw_yld{i}", [64, 2, 384], F32, pc)
                tl[("yrwT", i)] = sb(f"rw_yrwT{i}", [128, 3, 512], BF16, pc)
                if i == 1:
                    for nm in ("at", "bt", "kt", "rt", "bh", "kh", "bhT", "khT", "Vt", "dgP", "MabT", "Nab", "MakT", "MrbT", "MrkT"):
                        tl[(nm, i)] = sb(f"rw_{nm}{i}", [64, 6, 64], F32, pc)
                    tl[("bon", i)] = sb(f"rw_bon{i}", [64, 6], F32, pc)
                    tl[("Pend", i)] = sb(f"rw_Pend{i}", [64, 6], F32, pc)
                    continue
                for nm in names3:
                    tl[(nm, i)] = sb(f"rw_{nm}{i}", [64, 20, 64], F32, pc)
                for nm in names:
                    tl[(nm, i)] = sb(f"rw_{nm}{i}", [64, 6, 64], F32, pc)
                tl[("bon", i)] = sb(f"rw_bon{i}", [64, 6], F32, pc)
                tl[("Lend", i)] = sb(f"rw_Lend{i}", [64, 6], F32, pc)
                tl[("Pend", i)] = sb(f"rw_Pend{i}", [64, 6], F32, pc)
                tl[("st", i)] = sb(f"rw_st{i}", [64, 6], F32, pc)
                tl[("st2", i)] = sb(f"rw_st2{i}", [64, 6], F32, pc)
                tl[("yrwb", i)] = sb(f"rw_yrwb{i}", [64, 384], BF16, pc)
            evr = RR(["dve", "pool"])
            mskU = cst["msk"][:, 0:64]
            mskUi = cst["msk"][:, 64:128]
            mskL = cst["msk"][:, 128:192]
            b6 = lambda ap2: ap2.unsqueeze(1).to_broadcast([64, 6, 64])
            c6 = lambda ap2: ap2.unsqueeze(2).to_broadcast([64, 6, 64])

            def rw_chunk(d, i):
                pb_ = i % 2
                DB = ("X", "yst", "yld", "at", "bt", "kt", "rt", "bh", "kh", "bhT", "khT", "Vt", "dgP", "MabT", "Nab", "MakT", "MrbT", "MrkT", "bon", "Pend")
                T = lambda nm: tl[(nm, pb_ if nm in DB else 0)]
                K = lambda nm: f"rw_{nm}{pb_ if nm in DB else 0}"
                ci = i if d == 0 else 127 - i
                t0 = ci * 64
                ng = 22 if d == 0 else 20
                X = T("X")
                P.dma("sp", lambda e: e.dma_start(out=X[:, 0:ng, :], in_=prw_d[0:ng, :, t0:t0 + 66].rearrange("g p t -> p g t")),
                      f"ld_X{pb_}", writes=[K("X")])
                if d == 0:
                    cur, shf = X[:, 0:20, 1:65], X[:, 0:20, 0:64]
                else:
                    cur, shf = X[:, 0:20, 64:0:-1], X[:, 0:20, 65:1:-1]

                def tt(eng, out, in0, in1, op, reads, writes):
                    P.op(eng, lambda e: e.tensor_tensor(out=out, in0=in0, in1=in1, op=op), reads, writes)

                def act(out, in_, func, reads, writes, **kw):
                    P.op("act", lambda e: e.activation(out=out, in_=in_, func=func, **kw), reads, writes)

                def mm6(bank_i, lk, rk_, lhs_f, rhs_f, extra_reads=()):
                    ov = rbank[bank_i][:, 0:384].rearrange("p (h t) -> p h t", h=6)
                    fns = [(lambda e, h=h: e.matmul(ov[:, h, :], lhs_f(h), rhs_f(h), start=True, stop=True)) for h in range(6)]
                    P.op("pe", fns, reads=list(lk) + list(rk_) + list(extra_reads), writes=[f"rbank{bank_i}"])
                    return ov

                def mmacc(bank_i, terms, reads):
                    ov = rbank[bank_i][:, 0:384].rearrange("p (h t) -> p h t", h=6)
                    fns = []
                    n = len(terms)
                    for h in range(6):
                        for k_, (lf, rf) in enumerate(terms):
                            fns.append(lambda e, h=h, k_=k_, lf=lf, rf=rf: e.matmul(ov[:, h, :], lf(h), rf(h), start=(k_ == 0), stop=(k_ == n - 1)))
                    P.op("pe", fns, reads=reads, writes=[f"rbank{bank_i}"])
                    return ov

                mub = cst["mu"][:, d * 20:(d + 1) * 20].unsqueeze(2).to_broadcast([64, 20, 64])
                mub_s = cst["mu"][:, d * 20 + 18:d * 20 + 20].unsqueeze(2).to_broadcast([64, 2, 64])
                mub_b = cst["mu"][:, d * 20:d * 20 + 18].unsqueeze(2).to_broadcast([64, 18, 64])
                Kw, Kdw = K("mx") + "w", K("df") + "w"
                dfs, dfb = T("df")[:, 18:20, :], T("df")[:, 0:18, :]
                mxs_, mxb = T("mx")[:, 18:20, :], T("mx")[:, 0:18, :]
                tt("dve", dfs, shf[:, 18:20, :], cur[:, 18:20, :], ALU.subtract, [K("X")], [Kdw])
                tt("dve", dfs, dfs, mub_s, ALU.mult, [Kdw, "mu"], [Kdw])
                tt("dve", mxs_, dfs, cur[:, 18:20, :], ALU.add, [Kdw, K("X")], [Kw])
                tt("pool", dfb, shf[:, 0:18, :], cur[:, 0:18, :], ALU.subtract, [K("X")], [K("df")])
                tt("pool", dfb, dfb, mub_b, ALU.mult, [K("df"), "mu"], [K("df")])
                tt("pool", mxb, dfb, cur[:, 0:18, :], ALU.add, [K("df"), K("X")], [K("mx")])
                mx = T("mx")
                r_, k_, v_ = mx[:, 0:6, :], mx[:, 6:12, :], mx[:, 12:18, :]
                tw = twa[pb_]
                act(tw[0:64, 0, :], mx[:, 18, :], AF.Tanh, [Kw], [f"twa{pb_}"])
                P.op("dve", lambda e: e.tensor_copy(out=tw[0:64, 1, :], in_=mx[:, 19, :]), [Kw], [f"twa{pb_}"])
                zb = rq.next()
                zp = mm6(zb, [f"twa{pb_}"], ["w2a"], lambda h: cst["w2a"][:, d * 384 + h * 64:d * 384 + (h + 1) * 64], lambda h: tw[:, 0, :])
                act(T("sg")[:], zp, AF.Sigmoid, [f"rbank{zb}"], [K("sg")])
                ab = rq.next()
                ap_ = mm6(ab, [f"twa{pb_}"], ["a2a"], lambda h: cst["a2a"][:, d * 384 + h * 64:d * 384 + (h + 1) * 64], lambda h: tw[:, 1, :])
                act(T("al")[:], ap_, AF.Sigmoid, [f"rbank{ab}"], [K("al")])
                Lf = T("L")[:].rearrange("p h t -> p (h t)")
                P.op("dve", lambda e: e.tensor_tensor_scan(out=Lf, data0=ones64[:], data1=T("sg")[:].rearrange("p h t -> p (h t)"),
                                                           initial=0.0, op0=ALU.mult, op1=ALU.add), [K("sg"), "ones64"], [K("L")])
                of = offs[pb_]
                P.op("dve", lambda e: e.tensor_copy(out=of[:, 1:6], in_=T("L")[:, 0:5, 63]), [K("L")], [f"offs{pb_}"])
                tt("dve", T("L")[:], T("L")[:], c6(of[:]), ALU.subtract, [K("L"), f"offs{pb_}"], [K("L")])
                tt("dve", T("Lx")[:], T("L")[:], T("sg")[:], ALU.subtract, [K("L"), K("sg")], [K("Lx")])
                P.op("dve", lambda e: e.tensor_copy(out=T("Lend")[:], in_=T("L")[:, :, 63]), [K("L")], [K("Lend")])
                tt("dve", T("Ld")[:], T("L")[:], c6(T("Lend")[:]), ALU.subtract, [K("L"), K("Lend")], [K("Ld")])
                act(T("Pinc")[:], T("L")[:], AF.Exp, [K("L")], [K("Pinc")], scale=-C0)
                act(T("Pinv")[:], T("L")[:], AF.Exp, [K("L")], [K("Pinv")], scale=C0)
                act(T("Pexc")[:], T("Lx")[:], AF.Exp, [K("Lx")], [K("Pexc")], scale=-C0)
                act(T("Pdec")[:], T("Ld")[:], AF.Exp, [K("Ld")], [K("Pdec")], scale=C0)
                act(T("Pend")[:], T("Lend")[:], AF.Exp, [K("Lend")], [K("Pend")], scale=-C0)
                tt("dve", T("kkr")[:], k_, c6(cst["kkp"][:]), ALU.mult, [K("mx"), "kkp"], [K("kkr")])
                tt("dve", T("sq")[:], T("kkr")[:], T("kkr")[:], ALU.mult, [K("kkr")], [K("sq")])
                sb_ = rq.next()
                ssp = mm6(sb_, [K("sq")], ["ones64"], lambda h: ones64[:, 0:64], lambda h: T("sq")[:, h, :])
                act(T("nrm")[:], ssp, AF.Sqrt, [f"rbank{sb_}"], [K("nrm")])
                P.op("dve", lambda e: e.tensor_scalar_max(out=T("nrm")[:], in0=T("nrm")[:], scalar1=1e-12), [K("nrm")], [K("nrm")])
                P.op("dve", lambda e: e.reciprocal(out=T("nrm")[:], in_=T("nrm")[:]), [K("nrm")], [K("nrm")])
                tt("dve", T("kk")[:], T("kkr")[:], T("nrm")[:], ALU.mult, [K("kkr"), K("nrm")], [K("kk")])
                tt("dve", T("t1")[:], T("al")[:], c6(cst["kap"][:]), ALU.mult, [K("al"), "kap"], [K("t1")])
                tt("dve", T("t1")[:], T("t1")[:], c6(oma[:]), ALU.add, [K("t1"), "oma"], [K("t1")])
                tt("dve", T("k2")[:], k_, T("t1")[:], ALU.mult, [K("mx"), K("t1")], [K("k2")])
                tt("dve", T("bv")[:], T("kk")[:], T("al")[:], ALU.mult, [K("kk"), K("al")], [K("bv")])
                P.op("dve", lambda e: e.scalar_tensor_tensor(out=T("at")[:], in0=T("kk")[:], scalar=-1.0, in1=T("Pexc")[:], op0=ALU.mult, op1=ALU.mult),
                     [K("kk"), K("Pexc")], [K("at")])
                tt("dve", T("bt")[:], T("bv")[:], T("Pinv")[:], ALU.mult, [K("bv"), K("Pinv")], [K("bt")])
                tt("dve", T("kt")[:], T("k2")[:], T("Pinv")[:], ALU.mult, [K("k2"), K("Pinv")], [K("kt")])
                tt("dve", T("rt")[:], r_, T("Pinc")[:], ALU.mult, [K("mx"), K("Pinc")], [K("rt")])
                tt("dve", T("bh")[:], T("bv")[:], T("Pdec")[:], ALU.mult, [K("bv"), K("Pdec")], [K("bh")])
                tt("dve", T("kh")[:], T("k2")[:], T("Pdec")[:], ALU.mult, [K("k2"), K("Pdec")], [K("kh")])
                tt("dve", T("rk")[:], r_, T("k2")[:], ALU.mult, [K("mx"), K("k2")], [K("rk")])
                tt("dve", T("rk")[:], T("rk")[:], c6(cst["rkp"][:]), ALU.mult, [K("rk"), "rkp"], [K("rk")])
                bb = rq.next()
                bfn = [(lambda e, h=h: e.matmul(rbank[bb][:, h:h + 1], T("rk")[:, h, :], ones64[:, 0:1], start=True, stop=True)) for h in range(6)]
                P.op("pe", bfn, reads=[K("rk"), "ones64"], writes=[f"rbank{bb}"])
                P.op("act", lambda e: e.copy(out=T("bon")[:], in_=rbank[bb][:, 0:6]), [f"rbank{bb}"], [K("bon")])
                idf = identf[0:64, 0:64]
                for src_nm, dst_nm, srcap, srckey in (("v", "Vt", v_, K("mx")), ("bh", "bhT", T("bh")[:], K("bh")), ("kh", "khT", T("kh")[:], K("kh"))):
                    tb = rq.next()
                    tv = rbank[tb][:, 0:384].rearrange("p (h t) -> p h t", h=6)
                    P.op("pe", [(lambda e, h=h, tv=tv, srcap=srcap: e.transpose(out=tv[:, h, :], in_=srcap[:, h, :], identity=idf)) for h in range(6)],
                         reads=[srckey, "identf"], writes=[f"rbank{tb}"])
                    P.op("act", lambda e, tv=tv, dst_nm=dst_nm: e.copy(out=T(dst_nm)[:], in_=tv), [f"rbank{tb}"], [K(dst_nm)])
                for nm, ln, rn, msk in (("MabT", "bt", "at", mskU), ("Nab", "at", "bt", mskL), ("MakT", "kt", "at", mskU),
                                        ("MrbT", "bt", "rt", mskUi), ("MrkT", "kt", "rt", mskUi)):
                    mb = rq.next()
                    ov = mm6(mb, [K(ln)], [K(rn)], (lambda h, ln=ln: T(ln)[:, h, :]), (lambda h, rn=rn: T(rn)[:, h, :]))
                    tt("dve", T(nm)[:], ov, b6(msk), ALU.mult, [f"rbank{mb}", "msk"], [K(nm)])
                tt("dve", T("X0")[:], T("MabT")[:], b6(idf), ALU.add, [K("MabT"), "identf"], [K("X0")])
                Mc, Nc, Xc = "MabT", "Nab", "X0"
                for j in range(1, 6):
                    Nn = "Na" if j % 2 else "Nb"
                    Mn = "Ma" if j % 2 else "Mb"
                    Xn = "X1" if j % 2 else "X0"
                    nb = rq.next()
                    ov = mm6(nb, [K(Mc)], [K(Nc)], (lambda h, Mc=Mc: T(Mc)[:, h, :]), (lambda h, Nc=Nc: T(Nc)[:, h, :]))
                    P.op("act", lambda e, ov=ov, Nn=Nn: e.copy(out=T(Nn)[:], in_=ov), [f"rbank{nb}"], [K(Nn)])
                    if j < 5:
                        mb = rq.next()
                        ov2 = mm6(mb, [K(Nc)], [K(Mc)], (lambda h, Nc=Nc: T(Nc)[:, h, :]), (lambda h, Mc=Mc: T(Mc)[:, h, :]))
                        P.op("dve", lambda e, ov2=ov2, Mn=Mn: e.tensor_copy(out=T(Mn)[:], in_=ov2), [f"rbank{mb}"], [K(Mn)])
                    xb = rq.next()
                    ov3 = mm6(xb, [K(Nn)], [K(Xc)], (lambda h, Nn=Nn: T(Nn)[:, h, :]), (lambda h, Xc=Xc: T(Xc)[:, h, :]))
                    tt("dve", T(Xn)[:], ov3, T(Xc)[:], ALU.add, [f"rbank{xb}", K(Xc)], [K(Xn)])
                    Mc, Nc, Xc = Mn, Nn, Xn
                XT = Xc
                Hc, Hn = Hs[i % 2], Hs[(i + 1) % 2]
                Hck, Hnk = f"H{i % 2}", f"H{(i + 1) % 2}"
                Hv = lambda Ht: (lambda h: Ht[:, h * 64:(h + 1) * 64])
                tt("dve", T("dgP")[:], b6(idf), c6(T("Pend")[:]), ALU.mult, ["identf", K("Pend")], [K("dgP")])
                wb = rq.next()
                ov = mmacc(wb, [((lambda h: T("at")[:, h, :]), Hv(Hc)), ((lambda h: T("MakT")[:, h, :]), (lambda h: T("Vt")[:, h, :]))],
                           reads=[K("at"), Hck, K("MakT"), K("Vt")])
                P.op("act", lambda e: e.copy(out=T("W")[:], in_=ov), [f"rbank{wb}"], [K("W")])
                ub = rq.next()
                ovu = mm6(ub, [K(XT)], [K("W")], (lambda h: T(XT)[:, h, :]), (lambda h: T("W")[:, h, :]))
                P.op("dve", lambda e: e.tensor_copy(out=T("U")[:], in_=ovu), [f"rbank{ub}"], [K("U")])
                hb_ = rq.next()
                ovh = mmacc(hb_, [((lambda h: T("dgP")[:, h, :]), Hv(Hc)), ((lambda h: T("bhT")[:, h, :]), (lambda h: T("U")[:, h, :])),
                                  ((lambda h: T("khT")[:, h, :]), (lambda h: T("Vt")[:, h, :]))],
                            reads=[K("dgP"), Hck, K("bhT"), K("U"), K("khT"), K("Vt")])
                P.op("act", lambda e: e.copy(out=Hn[:].rearrange("p (h t) -> p h t", h=6), in_=ovh), [f"rbank{hb_}"], [Hnk])
                yb_ = rq.next()
                ovy = mmacc(yb_, [((lambda h: T("rt")[:, h, :]), Hv(Hc)), ((lambda h: T("MrbT")[:, h, :]), (lambda h: T("U")[:, h, :])),
                                  ((lambda h: T("MrkT")[:, h, :]), (lambda h: T("Vt")[:, h, :]))],
                            reads=[K("rt"), Hck, K("MrbT"), K("U"), K("MrkT"), K("Vt")])
                if d == 1:
                    yst = T("yst")
                    P.op("act", lambda e: e.copy(out=yst[:, 0, :].rearrange("p (h t) -> p h t", h=6), in_=ovy), [f"rbank{yb_}"], [K("yst")])
                    tt("dve", yst[:, 1, :].rearrange("p (h t) -> p h t", h=6), T("Vt")[:], c6(T("bon")[:]), ALU.mult, [K("Vt"), K("bon")], [K("yst")])
                    yst2 = T("yld")
                    for c in range(2):
                        jb = rq.next()
                        P.op("pe", lambda e, c=c, jb=jb: e.matmul(rbank[jb][:, 0:384], cst["jrev"][:], yst[:, c, :], start=True, stop=True),
                             reads=[K("yst"), "jrev"], writes=[f"rbank{jb}"])
                        P.op("act" if c == 0 else "dve", (lambda e, c=c, jb=jb: e.copy(out=yst2[:, c, :], in_=rbank[jb][:, 0:384])) if c == 0 else
                             (lambda e, c=c, jb=jb: e.tensor_copy(out=yst2[:, c, :], in_=rbank[jb][:, 0:384])), [f"rbank{jb}"], [K("yld")])
                    P.dma("sp", lambda e: e.dma_start(out=yb_d[t0:t0 + 64], in_=yst2[:]), f"st_y{pb_}", reads=[K("yld")], writes=["yb_d"])
                else:
                    yld = T("yld")
                    P.dma("act", lambda e: e.dma_start(out=yld[:], in_=yb_d[t0:t0 + 64]), f"ld_y{pb_}", writes=[K("yld")])
                    y3 = lambda ap2: ap2.rearrange("p (h t) -> p h t", h=6)
                    tt("dve", T("yo")[:], ovy, y3(yld[:, 0, :]), ALU.add, [f"rbank{yb_}", K("yld")], [K("yo")])
                    tt("dve", T("ybl")[:], T("Vt")[:], c6(T("bon")[:]), ALU.mult, [K("Vt"), K("bon")], [K("ybl")])
                    tt("dve", T("ybl")[:], T("ybl")[:], y3(yld[:, 1, :]), ALU.add, [K("ybl"), K("yld")], [K("ybl")])
                    P.op("dve", lambda e: e.tensor_reduce(out=T("st")[:], in_=T("yo")[:], axis=AX.X, op=ALU.add), [K("yo")], [K("st")])
                    P.op("dve", lambda e: e.tensor_scalar(out=T("st")[:], in0=T("st")[:], scalar1=1.0 / 64, scalar2=None, op0=ALU.mult), [K("st")], [K("st")])
                    tt("dve", T("cen")[:], T("yo")[:], c6(T("st")[:]), ALU.subtract, [K("yo"), K("st")], [K("cen")])
                    tt("dve", T("sqc")[:], T("cen")[:], T("cen")[:], ALU.mult, [K("cen")], [K("sqc")])
                    P.op("dve", lambda e: e.tensor_reduce(out=T("st2")[:], in_=T("sqc")[:], axis=AX.X, op=ALU.add), [K("sqc")], [K("st2")])
                    act(T("st2")[:], T("st2")[:], AF.Sqrt, [K("st2"), "eps12"], [K("st2")], scale=1.0 / 64, bias=eps12[:])
                    P.op("dve", lambda e: e.reciprocal(out=T("st2")[:], in_=T("st2")[:]), [K("st2")], [K("st2")])
                    tt("dve", T("yn")[:], T("cen")[:], c6(T("st2")[:]), ALU.mult, [K("cen"), K("st2")], [K("yn")])
                    tt("dve", T("yn")[:], T("yn")[:], y3(cst["lng"][:]), ALU.mult, [K("yn"), "lng"], [K("yn")])
                    tt("dve", T("yn")[:], T("yn")[:], y3(cst["lnb"][:]), ALU.add, [K("yn"), "lnb"], [K("yn")])
                    tt("dve", T("yn")[:], T("yn")[:], T("ybl")[:], ALU.add, [K("yn"), K("ybl")], [K("yn")])
                    act(T("sgg")[:, 0:2, :], X[:, 20:22, 1:65], AF.Sigmoid, [K("X")], [K("sgg")])
                    gb = rq.next()
                    gfn = [(lambda e, c=c: e.matmul(rbank[gb][:, 0:384], T("sgg")[:, c, :], cst["g2"][:, c * 384:(c + 1) * 384], start=(c == 0), stop=(c == 1)))
                           for c in range(2)]
                    P.op("pe", gfn, reads=[K("sgg"), "g2"], writes=[f"rbank{gb}"])
                    yrwb = T("yrwb")
                    tt("dve", yrwb[:], T("yn")[:].rearrange("p h t -> p (h t)"), rbank[gb][:, 0:384], ALU.mult, [K("yn"), f"rbank{gb}"], [K("yrwb")])
                    P.op("pe", [(lambda e, c=c: e.transpose(out=rbt[:, c * 64:(c + 1) * 64], in_=yrwb[:, c * 128:(c + 1) * 128], identity=identb[0:64, 0:64]))
                                for c in range(3)], reads=[K("yrwb"), "identb"], writes=["rbt"])
                    ob = (i // 8) % 2
                    yT = tl[("yrwT", ob)]
                    P.op("act", lambda e: e.copy(out=yT[:, :, (i % 8) * 64:(i % 8) * 64 + 64], in_=rbt[:, 0:192].rearrange("p (c q) -> p c q", c=3)),
                         reads=["rbt"], writes=[f"yrwT{ob}"])
                    if i % 8 == 7:
                        tb0 = (i // 8) * 512
                        P.dma("sp", lambda e: e.dma_start(out=yrwT_d.rearrange("(c p) t -> p c t", p=128)[:, :, tb0:tb0 + 512], in_=yT[:]),
                              f"st_yrw{ob}", reads=[f"yrwT{ob}"], writes=["yrwT_d"])

            for d in (1, 0):
                P.op("dve", lambda e: e.memset(Hs[0][:], 0.0), writes=["H0"])
                for i in range(RW_CHUNKS):
                    rw_chunk(d, i)
            P.barrier()

        if PHASES >= 4:
          affall = sb("affall", [128, 64, 16], F32)
          with ExitStack() as pd:
            Wg = sb("Wg", [128, 8, 3072], BF16, pd)
            Wb = [sb("Wna", [128, 3, 1024], BF16, pd), sb("Wrw", [128, 3, 1024], BF16, pd), sb("Wmem", [128, 2, 1024], BF16, pd)]
            Wo = sb("Wo", [128, 8, 1024], BF16, pd)
            wr = sb("wr", [128, 8, 16], F32, pd)
            gffn = sb("gffn", [128, 1024], F32, pd)
            tailc = sb("tailc", [128, 64, 4], F32, pd)
            for c in range(8):
                P.dma("pool", lambda e, c=c: e.dma_start(out=Wg[:, c, :], in_=w_in.rearrange("(c p) n -> p c n", p=128)[:, c, 2816:5888]), "Wg", writes=["Wg"])
            P.dma("pool", lambda e: e.dma_start(out=Wb[0][:], in_=wbna_d.rearrange("(c p) n -> p c n", p=128)), "Wb0", writes=["Wb0"])
            P.dma("pool", lambda e: e.dma_start(out=Wb[1][:], in_=wbrw_d.rearrange("(c p) n -> p c n", p=128)), "Wb1", writes=["Wb1"])
            P.dma("pool", lambda e: e.dma_start(out=Wb[2][:], in_=wbmem_d.rearrange("(c p) n -> p c n", p=128)), "Wb2", writes=["Wb2"])
            P.dma("pool", lambda e: e.dma_start(out=Wo[:], in_=wout_d.rearrange("(c p) n -> p c n", p=128)), "Wo", writes=["Wo"])
            P.dma("sp", lambda e: e.dma_start(out=wr[:], in_=wrt_d.rearrange("(c p) n -> p c n", p=128)), "wr", writes=["wr"])
            P.dma("sp", lambda e: e.dma_start(out=gffn[:], in_=gffn_d), "gffn", writes=["gffn"])
            P.dma("sp", lambda e: e.dma_start(out=tailc[:].rearrange("p t k -> p (t k)"), in_=tail_d), "tailc", writes=["tailc"])
            dhT = [sb(f"d_hTb{i}", [128, 8, 512], BF16, pd) for i in range(2)]
            yT = [[sb(f"d_y{b}T{i}", [128, 3 if b < 2 else 2, 512], BF16, pd) for i in range(2)] for b in range(3)]
            sg_t = [sb(f"d_sg{i}", [128, 512], F32, pd) for i in range(2)]
            mg = sb("d_mg", [128, 512], F32, pd)
            tmpm = sb("d_tmpm", [128, 512], F32, pd)
            mT = sb("d_mT", [128, 8, 512], BF16, pd)
            xt_ = [sb(f"d_xt{i}", [128, D], F32, pd) for i in range(2)]
            x1_ = [sb(f"d_x1{i}", [128, D], F32, pd) for i in range(2)]
            h2_ = [sb(f"d_h2{i}", [128, D], F32, pd) for i in range(2)]
            h2row = [sb(f"d_h2row{i}", [128, 1028], BF16, pd) for i in range(2)]
            h2T = sb("d_h2T", [128, 8, 128], F32, pd)
            sqj = sb("d_sqj", [128, D], BF16, pd)
            sst = [sb(f"d_ss{i}", [128, 1], F32, pd) for i in range(2)]
            rst = [sb(f"d_rs{i}", [128, 1], F32, pd) for i in range(2)]
            mxs = [sb(f"d_mx{i}", [128, 1], F32, pd) for i in range(2)]
            sms = [sb(f"d_sm{i}", [128, 1], F32, pd) for i in range(2)]
            lex = [sb(f"d_lex{i}", [128, 16], F32, pd) for i in range(2)]
            epsd = sb("d_eps", [128, 1], F32, pd)
            P.op("dve", lambda e: e.memset(epsd[:], 1e-6), writes=["d_eps"])
            dbank = [ps(f"dbank{i}", [128, 512], F32, pd) for i in range(8)]
            dq = RR(range(8))
            srcs = (ynaT_d, yrwT_d, ymemT_d)
            nck = (3, 3, 2)

            def d_tile(blk, j):
                ti = blk * 4 + j
                b2 = ti % 2
                P.dma("act", lambda e: e.dma_start(out=xt_[b2][:], in_=x_d[ti * 128:(ti + 1) * 128, :]), f"d_ldx{b2}", writes=[f"d_xt{b2}"])
                for half in range(2):
                    ob = dq.next()
                    mm(P, dbank[ob][:], [(mT[:, c, j * 128:(j + 1) * 128], Wo[:, c, half * 512:(half + 1) * 512]) for c in range(8)],
                       reads=["d_mT", "Wo"], writes=[f"dbank{ob}"])
                    P.op("dve", lambda e, ob=ob, half=half: e.tensor_tensor(out=x1_[b2][:, half * 512:(half + 1) * 512], in0=dbank[ob][:],
                                                                          in1=xt_[b2][:, half * 512:(half + 1) * 512], op=ALU.add),
                         reads=[f"dbank{ob}", f"d_xt{b2}"], writes=[f"d_x1{b2}"])
                P.dma("sp", lambda e: e.dma_start(out=acc_d[ti * 128:(ti + 1) * 128, :], in_=x1_[b2][:]), f"d_stx1{b2}", reads=[f"d_x1{b2}"], writes=["acc_d"])
                P.op("act", lambda e: e.activation(out=sqj[:], in_=x1_[b2][:], func=AF.Square, accum_out=sst[b2][:]),
                     reads=[f"d_x1{b2}"], writes=["d_sqj", f"d_ss{b2}"])
                P.op("act", lambda e: e.activation(out=rst[b2][:], in_=sst[b2][:], func=AF.Sqrt, scale=1.0 / D, bias=epsd[:]),
                     reads=[f"d_ss{b2}", "d_eps"], writes=[f"d_rs{b2}"])
                P.op("dve", lambda e: e.reciprocal(out=rst[b2][:], in_=rst[b2][:]), reads=[f"d_rs{b2}"], writes=[f"d_rs{b2}"])
                P.op("act", lambda e: e.activation(out=h2_[b2][:], in_=x1_[b2][:], func=AF.Copy, scale=rst[b2][:]),
                     reads=[f"d_x1{b2}", f"d_rs{b2}"], writes=[f"d_h2{b2}"])
                P.op("dve", lambda e: e.tensor_tensor(out=h2_[b2][:], in0=h2_[b2][:], in1=gffn[:], op=ALU.mult), reads=[f"d_h2{b2}", "gffn"], writes=[f"d_h2{b2}"])
                P.op("act", lambda e: e.copy(out=h2row[b2][:, 0:1024], in_=h2_[b2][:]), reads=[f"d_h2{b2}"], writes=[f"d_h2row{b2}"])
                P.op("dve", lambda e: e.tensor_copy(out=h2row[b2][:, 1024:1028], in_=tailc[:, ti, :]), reads=["tailc"], writes=[f"d_h2row{b2}"])
                P.dma("sp", lambda e: e.dma_start(out=h2_d[ti * 128:(ti + 1) * 128, :], in_=h2row[b2][:]), f"d_sth2{b2}", reads=[f"d_h2row{b2}"], writes=["h2_d"])
                for half in range(2):
                    tb = dq.next()
                    tv = dbank[tb][:].rearrange("p (c t) -> p c t", c=4)
                    P.op("pe", [(lambda e, c=c, tv=tv, half=half: e.transpose(out=tv[:, c, :], in_=h2_[b2][:, (half * 4 + c) * 128:(half * 4 + c + 1) * 128], identity=identf[:]))
                                for c in range(4)], reads=[f"d_h2{b2}", "identf"], writes=[f"dbank{tb}"])
                    if half == 0:
                        P.op("act", lambda e, tv=tv: e.copy(out=h2T[:, 0:4, :], in_=tv), [f"dbank{tb}"], ["d_h2T"])
                    else:
                        P.op("dve", lambda e, tv=tv: e.tensor_copy(out=h2T[:, 4:8, :], in_=tv), [f"dbank{tb}"], ["d_h2T"])
                lb = dq.next()
                mm(P, dbank[lb][:, 0:16], [(h2T[:, c, :], wr[:, c, :]) for c in range(8)], reads=["d_h2T", "wr"], writes=[f"dbank{lb}"])
                P.op("dve", lambda e: e.tensor_reduce(out=mxs[b2][:], in_=dbank[lb][:, 0:16], axis=AX.X, op=ALU.max), [f"dbank{lb}"], [f"d_mx{b2}"])
                P.op("dve", lambda e: e.tensor_scalar(out=mxs[b2][:], in0=mxs[b2][:], scalar1=-1.0, scalar2=None, op0=ALU.mult), [f"d_mx{b2}"], [f"d_mx{b2}"])
                P.op("act", lambda e: e.activation(out=lex[b2][:], in_=dbank[lb][:, 0:16], func=AF.Exp, bias=mxs[b2][:], accum_out=sms[b2][:]),
                     [f"dbank{lb}", f"d_mx{b2}"], [f"d_lex{b2}", f"d_sm{b2}"])
                P.op("dve", lambda e: e.reciprocal(out=sms[b2][:], in_=sms[b2][:]), [f"d_sm{b2}"], [f"d_sm{b2}"])
                P.op("dve", lambda e: e.tensor_scalar(out=affall[:, ti, :], in0=lex[b2][:], scalar1=sms[b2][:], scalar2=None, op0=ALU.mult),
                     [f"d_lex{b2}", f"d_sm{b2}"], ["affall"])

            def d_chunk(blk, hb, dmc):
                for b in range(3):
                    gb = dq.next()
                    c0 = b * 1024 + dmc * 128
                    mm(P, dbank[gb][:], [(Wg[:, c, c0:c0 + 128], dhT[hb][:, c, :]) for c in range(8)], reads=["Wg", f"d_hTb{hb}"], writes=[f"dbank{gb}"])
                    s2 = b % 2
                    P.op("act", lambda e, gb=gb, s2=s2: e.activation(out=sg_t[s2][:], in_=dbank[gb][:], func=AF.Sigmoid), [f"dbank{gb}"], [f"d_sg{s2}"])
                    bb = dq.next()
                    mm(P, dbank[bb][:], [(Wb[b][:, c, dmc * 128:(dmc + 1) * 128], yT[b][hb][:, c, :]) for c in range(nck[b])],
                       reads=[f"Wb{b}", f"d_y{b}T{hb}"], writes=[f"dbank{bb}"])
                    if b == 0:
                        P.op("dve", lambda e, bb=bb, s2=s2: e.tensor_tensor(out=mg[:], in0=dbank[bb][:], in1=sg_t[s2][:], op=ALU.mult),
                             [f"dbank{bb}", f"d_sg{s2}"], ["d_mg"])
                    else:
                        P.op("dve", lambda e, bb=bb, s2=s2: e.tensor_tensor(out=tmpm[:], in0=dbank[bb][:], in1=sg_t[s2][:], op=ALU.mult),
                             [f"dbank{bb}", f"d_sg{s2}"], ["d_tmpm"])
                        if b == 1:
                            P.op("dve", lambda e: e.tensor_tensor(out=mg[:], in0=mg[:], in1=tmpm[:], op=ALU.add), ["d_mg", "d_tmpm"], ["d_mg"])
                        else:
                            P.op("dve", lambda e: e.tensor_tensor(out=mT[:, dmc, :], in0=mg[:], in1=tmpm[:], op=ALU.add), ["d_mg", "d_tmpm"], ["d_mT"])

            def d_block(blk):
                hb = blk % 2
                t0 = blk * 512
                P.dma("sp", lambda e: e.dma_start(out=dhT[hb][:], in_=hT_d.rearrange("(c p) t -> p c t", p=128)[:, :, t0:t0 + 512]), f"d_ldh{hb}", writes=[f"d_hTb{hb}"])
                for b in range(3):
                    P.dma("act", lambda e, b=b: e.dma_start(out=yT[b][hb][:], in_=srcs[b].rearrange("(c p) t -> p c t", p=128)[:, :, t0:t0 + 512]),
                          f"d_ldy{b}{hb}", writes=[f"d_y{b}T{hb}"])
                for dmc in range(8):
                    d_chunk(blk, hb, dmc)
                    if D_BARRIER:
                        P.barrier()
                for j in range(4):
                    d_tile(blk, j)
                    if D_BARRIER:
                        P.barrier()
                    if DBG_D and blk == 0 and j == 0:
                        P.barrier()
                        dv = lambda r0, r1: out_d[r0:r1, :].rearrange("(p a) n -> p (a n)", p=128)
                        P.dma("sp", lambda e: e.dma_start(out=dv(0, 256).bitcast(BF16), in_=dhT[0][:].rearrange("p c t -> p (c t)")), "dbgd", writes=["out"])
                        P.dma("sp", lambda e: e.dma_start(out=dv(256, 512).bitcast(BF16), in_=mT[:].rearrange("p c t -> p (c t)")), "dbgd", writes=["out"])
                        P.dma("sp", lambda e: e.dma_start(out=out_d[512:640, 0:768].bitcast(BF16), in_=yT[1][0][:].rearrange("p c t -> p (c t)")), "dbgd", writes=["out"])
                        P.dma("sp", lambda e: e.dma_start(out=out_d[640:768, :], in_=x1_[0][:]), "dbgd", writes=["out"])
                        P.dma("sp", lambda e: e.dma_start(out=out_d[768:896, 0:512], in_=sg_t[0][:]), "dbgd", writes=["out"])
                        P.dma("sp", lambda e: e.dma_start(out=out_d[768:896, 512:1024], in_=mg[:]), "dbgd", writes=["out"])
                        P.dma("sp", lambda e: e.dma_start(out=out_d[896:1024, :], in_=xt_[0][:]), "dbgd", writes=["out"])
                        P.barrier()

            for blk in range(S // 512):
                d_block(blk)
            P.dma("sp", lambda e: e.dma_start(out=aff_d.rearrange("(t p) e -> p t e", p=128), in_=affall[:]), "st_aff", reads=["affall"], writes=["aff_d"])
            P.barrier()

        if PHASES >= 5:
          with ExitStack() as pe_:
            ones128 = sb("e_ones", [128, 128], F32, pe_)
            ltm = sb("e_ltm", [128, 128], F32, pe_)
            lo = sb("e_lo", [128, 16], F32, pe_)
            hi = sb("e_hi", [128, 16], F32, pe_)
            mid = sb("e_mid", [128, 16], F32, pe_)
            cnt = sb("e_cnt", [128, 16], F32, pe_)
            ge = sb("e_ge", [128, 16], F32, pe_)
            dlt = sb("e_dlt", [128, 16], F32, pe_)
            cmpt = sb("e_cmp", [128, 64, 16], F32, pe_)
            sel = sb("e_sel", [128, 64, 16], F32, pe_)
            cntb = sb("e_cntb", [128, 64, 16], F32, pe_)
            cum = sb("e_cum", [128, 16, 64], F32, pe_)
            rank = sb("e_rank", [128, 64, 16], F32, pe_)
            ranki = sb("e_ranki", [128, 1024], I32, pe_)
            onesr = sb("e_onesr", [128, 64], F32, pe_)
            ebank = [ps(f"ebank{i}", [128, 512], F32, pe_) for i in range(4)]
            P.op("dve", lambda e: e.memset(ones128[:], 1.0), writes=["e_ones"])
            P.op("dve", lambda e: e.memset(onesr[:], 1.0), writes=["e_onesr"])
            P.dma("sp", lambda e: e.dma_start(out=ltm[:], in_=ltm_d), "e_ltm", writes=["e_ltm"])
            P.op("dve", lambda e: e.memset(lo[:], 0.0), writes=["e_lo"])
            P.op("dve", lambda e: e.memset(hi[:], 2.0), writes=["e_hi"])

            def bis_iter(it):
                P.op("dve", lambda e: e.tensor_tensor(out=mid[:], in0=lo[:], in1=hi[:], op=ALU.add), ["e_lo", "e_hi"], ["e_mid"])
                P.op("dve", lambda e: e.tensor_scalar(out=mid[:], in0=mid[:], scalar1=0.5, scalar2=None, op0=ALU.mult), ["e_mid"], ["e_mid"])
                P.op("dve", lambda e: e.tensor_tensor(out=cmpt[:], in0=affall[:], in1=mid[:].unsqueeze(1).to_broadcast([128, 64, 16]), op=ALU.is_ge),
                     ["affall", "e_mid"], ["e_cmp"])
                P.op("dve", lambda e: e.tensor_reduce(out=cnt[:], in_=cmpt[:].rearrange("p t e -> p e t"), axis=AX.X, op=ALU.add), ["e_cmp"], ["e_cnt"])
                b = it % 2
                mm(P, ebank[b][:, 0:16], [(ones128[:], cnt[:])], reads=["e_ones", "e_cnt"], writes=[f"ebank{b}"])
                P.op("dve", lambda e: e.tensor_scalar(out=ge[:], in0=ebank[b][:, 0:16], scalar1=1023.5, scalar2=None, op0=ALU.is_ge), [f"ebank{b}"], ["e_ge"])
                P.op("dve", lambda e: e.tensor_tensor(out=dlt[:], in0=mid[:], in1=lo[:], op=ALU.subtract), ["e_mid", "e_lo"], ["e_dlt"])
                P.op("dve", lambda e: e.tensor_tensor(out=dlt[:], in0=dlt[:], in1=ge[:], op=ALU.mult), ["e_dlt", "e_ge"], ["e_dlt"])
                P.op("dve", lambda e: e.tensor_tensor(out=lo[:], in0=lo[:], in1=dlt[:], op=ALU.add), ["e_lo", "e_dlt"], ["e_lo"])
                P.op("dve", lambda e: e.tensor_tensor(out=dlt[:], in0=hi[:], in1=mid[:], op=ALU.subtract), ["e_hi", "e_mid"], ["e_dlt"])
                P.op("dve", lambda e: e.tensor_tensor(out=dlt[:], in0=dlt[:], in1=ge[:], op=ALU.mult), ["e_dlt", "e_ge"], ["e_dlt"])
                P.op("dve", lambda e: e.tensor_tensor(out=hi[:], in0=mid[:], in1=dlt[:], op=ALU.add), ["e_mid", "e_dlt"], ["e_hi"])

            for it in range(36):
                bis_iter(it)
            P.op("dve", lambda e: e.tensor_tensor(out=sel[:], in0=affall[:], in1=lo[:].unsqueeze(1).to_broadcast([128, 64, 16]), op=ALU.is_ge),
                 ["affall", "e_lo"], ["e_sel"])
            self2 = sel[:].rearrange("p t e -> p (t e)")
            for half in range(2):
                mm(P, ebank[half][:], [(ones128[:], self2[:, half * 512:(half + 1) * 512])], reads=["e_ones", "e_sel"], writes=[f"ebank{half}"])
                P.op("act", lambda e, half=half: e.copy(out=cntb[:].rearrange("p t e -> p (t e)")[:, half * 512:(half + 1) * 512], in_=ebank[half][:]),
                     [f"ebank{half}"], ["e_cntb"])
            for ex in range(16):
                P.op("dve", lambda e, ex=ex: e.tensor_tensor_scan(out=cum[:, ex, :], data0=onesr[:], data1=cntb[:, :, ex], initial=0.0, op0=ALU.mult, op1=ALU.add),
                     ["e_cntb", "e_onesr"], ["e_cum"])
            P.op("dve", lambda e: e.tensor_tensor(out=rank[:], in0=cum[:].rearrange("p e t -> p t e"), in1=cntb[:], op=ALU.subtract), ["e_cum", "e_cntb"], ["e_rank"])
            for half in range(2):
                mm(P, ebank[2 + half][:], [(ltm[:], self2[:, half * 512:(half + 1) * 512])], reads=["e_ltm", "e_sel"], writes=[f"ebank{2 + half}"])
                P.op("dve", lambda e, half=half: e.tensor_tensor(out=rank[:].rearrange("p t e -> p (t e)")[:, half * 512:(half + 1) * 512],
                                                                 in0=rank[:].rearrange("p t e -> p (t e)")[:, half * 512:(half + 1) * 512], in1=ebank[2 + half][:], op=ALU.add),
                     [f"ebank{2 + half}", "e_rank"], ["e_rank"])
            P.op("dve", lambda e: e.tensor_scalar(out=rank[:], in0=rank[:], scalar1=-60000.0, scalar2=None, op0=ALU.add), ["e_rank"], ["e_rank"])
            P.op("dve", lambda e: e.tensor_tensor(out=rank[:], in0=rank[:], in1=sel[:], op=ALU.mult), ["e_rank", "e_sel"], ["e_rank"])
            P.op("dve", lambda e: e.tensor_scalar(out=rank[:], in0=rank[:], scalar1=60000.0, scalar2=None, op0=ALU.add), ["e_rank"], ["e_rank"])
            P.op("dve", lambda e: e.tensor_copy(out=ranki[:], in_=rank[:].rearrange("p t e -> p (t e)")), ["e_rank"], ["e_ranki"])
            if DEBUG:
                P.dma("sp", lambda e: e.dma_start(out=rank_dbg, in_=rank[:].rearrange("p t e -> p (t e)")), "dbg_rank", reads=["e_rank"], writes=["rank_dbg"])
            h2r = [sb(f"e_h2r{i}", [128, 1028], BF16, pe_) for i in range(6)]

            def disp_tile(ti):
                b3 = ti % 6
                P.dma("sp", lambda e: e.dma_start(out=h2r[b3][:], in_=h2_d[ti * 128:(ti + 1) * 128, :]), f"e_ld{b3}", writes=[f"e_h2r{b3}"])
                for ex in range(16):
                    P.dma("pool", lambda e, ex=ex: e.indirect_dma_start(out=xe_d[ex], out_offset=bass.IndirectOffsetOnAxis(ap=ranki[:, ti * 16 + ex:ti * 16 + ex + 1], axis=0),
                                                                        in_=h2r[b3][:], in_offset=None, bounds_check=breg(e, 1023), oob_is_err=False),
                          f"e_sc{b3}", reads=[f"e_h2r{b3}", "e_ranki"], writes=())

            for ti in range(64):
                disp_tile(ti)
            P.barrier()

        if PHASES >= 6:
          with ExitStack() as pf:
            xe = [sb(f"f_xe{i}", [128, 1028], BF16, pf) for i in range(2)]
            xeT = sb("f_xeT", [128, 8, 1024], BF16, pf)
            tailf = sb("f_tailf", [128, 8, 2], F32, pf)
            idf_ = sb("f_idf", [128, 8], F32, pf)
            idi = sb("f_idi", [128, 8], I32, pf)
            affg = sb("f_affg", [128, 8, 16], F32, pf)
            wgt = [sb(f"f_wg{i}", [128, 8, 512], BF16, pf) for i in range(2)]
            wut = [sb(f"f_wu{i}", [128, 8, 512], BF16, pf) for i in range(2)]
            wdt = [sb(f"f_wd{i}", [128, 4, 1024], BF16, pf) for i in range(2)]
            actT = sb("f_actT", [128, 4, 1024], BF16, pf)
            sil = [sb(f"f_sil{i}", [128, 512], F32, pf) for i in range(2)]
            Y = sb("f_Y", [128, 8, 1024], F32, pf)
            fbank = [ps(f"fbank{i}", [128, 512], F32, pf) for i in range(6)]
            fbt = [ps(f"fbt{i}", [128, 1024], BF16, pf) for i in range(2)]
            fq = RR(range(6))
            wctr = [0]

            def load_w(ex, fg):
                wb = wctr[0] % 2
                wctr[0] += 1
                f0 = fg * 512
                P.dma("pool", lambda e: e.dma_start(out=wgt[wb][:], in_=wgate_d[ex].rearrange("(c p) f -> p c f", p=128)[:, :, f0:f0 + 512]), f"f_ldg{wb}", writes=[f"f_wg{wb}"])
                P.dma("pool", lambda e: e.dma_start(out=wut[wb][:], in_=wup_d[ex].rearrange("(c p) f -> p c f", p=128)[:, :, f0:f0 + 512]), f"f_ldu{wb}", writes=[f"f_wu{wb}"])
                P.dma("pool", lambda e: e.dma_start(out=wdt[wb][:], in_=wdown_d[ex, f0:f0 + 512, :].rearrange("(c p) n -> p c n", p=128)), f"f_ldd{wb}", writes=[f"f_wd{wb}"])
                return wb

            def xe_tile(ex, st):
                b2 = st % 2
                P.dma("sp", lambda e: e.dma_start(out=xe[b2][:], in_=xe_d[ex][st * 128:(st + 1) * 128, :]), f"f_ldxe{b2}", writes=[f"f_xe{b2}"])
                P.op("pe", [(lambda e, c=c: e.transpose(out=fbt[b2][:, c * 128:(c + 1) * 128], in_=xe[b2][:, c * 128:(c + 1) * 128], identity=identb[:])) for c in range(8)],
                     reads=[f"f_xe{b2}", "identb"], writes=[f"fbt{b2}"])
                ev = "act" if b2 == 0 else "dve"
                if ev == "act":
                    P.op("act", lambda e: e.copy(out=xeT[:, :, st * 128:(st + 1) * 128], in_=fbt[b2][:].rearrange("p (c t) -> p c t", c=8)), [f"fbt{b2}"], ["f_xeT"])
                else:
                    P.op("dve", lambda e: e.tensor_copy(out=xeT[:, :, st * 128:(st + 1) * 128], in_=fbt[b2][:].rearrange("p (c t) -> p c t", c=8)), [f"fbt{b2}"], ["f_xeT"])
                P.op("dve", lambda e: e.tensor_copy(out=tailf[:, st, :], in_=xe[b2][:, 1024:1026]), [f"f_xe{b2}"], ["f_tailf"])

            def gu(ex, wb, fc, sh):
                gb, ub = fq.next(), fq.next()
                mm(P, fbank[gb][:], [(wgt[wb][:, c, fc * 128:(fc + 1) * 128], xeT[:, c, sh * 512:(sh + 1) * 512]) for c in range(8)],
                   reads=[f"f_wg{wb}", "f_xeT"], writes=[f"fbank{gb}"])
                mm(P, fbank[ub][:], [(wut[wb][:, c, fc * 128:(fc + 1) * 128], xeT[:, c, sh * 512:(sh + 1) * 512]) for c in range(8)],
                   reads=[f"f_wu{wb}", "f_xeT"], writes=[f"fbank{ub}"])
                s2 = (fc * 2 + sh) % 2
                P.op("act", lambda e: e.activation(out=sil[s2][:], in_=fbank[gb][:], func=AF.Silu), [f"fbank{gb}"], [f"f_sil{s2}"])
                P.op("dve", lambda e: e.tensor_tensor(out=actT[:, fc, sh * 512:(sh + 1) * 512], in0=fbank[ub][:], in1=sil[s2][:], op=ALU.mult),
                     [f"fbank{ub}", f"f_sil{s2}"], ["f_actT"])

            def down(ex, wb, fg, st, dh):
                yb = fq.next()
                mm(P, fbank[yb][:], [(actT[:, fc, st * 128:(st + 1) * 128], wdt[wb][:, fc, dh * 512:(dh + 1) * 512]) for fc in range(4)],
                   reads=["f_actT", f"f_wd{wb}"], writes=[f"fbank{yb}"])
                dst = Y[:, st, dh * 512:(dh + 1) * 512]
                if fg == 0:
                    P.op("act", lambda e: e.copy(out=dst, in_=fbank[yb][:]), [f"fbank{yb}"], [f"f_Y{st}"])
                else:
                    P.op("dve", lambda e: e.tensor_tensor(out=dst, in0=fbank[yb][:], in1=dst, op=ALU.add), [f"fbank{yb}", f"f_Y{st}"], [f"f_Y{st}"])

            def expert(ex):
                for st in range(8):
                    xe_tile(ex, st)
                P.op("dve", lambda e: e.scalar_tensor_tensor(out=idf_[:], in0=tailf[:, :, 1], scalar=128.0, in1=tailf[:, :, 0], op0=ALU.mult, op1=ALU.add),
                     ["f_tailf"], ["f_idf"])
                P.op("dve", lambda e: e.tensor_copy(out=idi[:], in_=idf_[:]), ["f_idf"], ["f_idi"])
                for st in range(8):
                    P.dma("pool", lambda e, st=st: e.indirect_dma_start(out=affg[:, st, :], out_offset=None, in_=aff_d,
                                                                        in_offset=bass.IndirectOffsetOnAxis(ap=idi[:, st:st + 1], axis=0)),
                          "f_affg", reads=["f_idi"], writes=["f_affg"])
                for fg in range(4):
                    wb = load_w(ex, fg)
                    for fc in range(4):
                        for sh in range(2):
                            gu(ex, wb, fc, sh)
                    for st in range(8):
                        for dh in range(2):
                            down(ex, wb, fg, st, dh)
                for st in range(8):
                    P.op("dve", lambda e, st=st: e.tensor_scalar(out=Y[:, st, :], in0=Y[:, st, :], scalar1=affg[:, st, ex:ex + 1], scalar2=None, op0=ALU.mult),
                         [f"f_Y{st}", "f_affg"], [f"f_Y{st}"])
                    P.dma("pool", lambda e, st=st: e.indirect_dma_start(out=acc_d, out_offset=bass.IndirectOffsetOnAxis(ap=idi[:, st:st + 1], axis=0),
                                                                        in_=Y[:, st, :], in_offset=None, bounds_check=breg(e, S - 1), oob_is_err=False, compute_op=ALU.add),
                          "f_sca", reads=[f"f_Y{st}", "f_idi"], writes=["acc_d"])

            for ex in range(N_EXP):
                expert(ex)
            P.barrier()

        if PHASES >= 7:
          with ExitStack() as pg:
            gfin = sb("g_gfin", [128, D], F32, pg)
            P.dma("sp", lambda e: e.dma_start(out=gfin[:], in_=gfin_d), "g_gfin", writes=["g_gfin"])
            at_ = [sb(f"g_a{i}", [128, D], F32, pg) for i in range(3)]
            ot_ = [sb(f"g_o{i}", [128, D], F32, pg) for i in range(2)]
            gsq = sb("g_sq", [128, D], BF16, pg)
            gss = [sb(f"g_ss{i}", [128, 1], F32, pg) for i in range(2)]
            geps = sb("g_eps", [128, 1], F32, pg)
            P.op("dve", lambda e: e.memset(geps[:], 1e-6), writes=["g_eps"])

            def fin_tile(ti):
                b3, b2 = ti % 3, ti % 2
                P.dma("sp" if ti % 2 else "act", lambda e: e.dma_start(out=at_[b3][:], in_=acc_d[ti * 128:(ti + 1) * 128, :]), f"g_ld{b3}", writes=[f"g_a{b3}"])
                P.op("act", lambda e: e.activation(out=gsq[:], in_=at_[b3][:], func=AF.Square, accum_out=gss[b2][:]), [f"g_a{b3}"], ["g_sq", f"g_ss{b2}"])
                P.op("act", lambda e: e.activation(out=gss[b2][:], in_=gss[b2][:], func=AF.Sqrt, scale=1.0 / D, bias=geps[:]), [f"g_ss{b2}", "g_eps"], [f"g_ss{b2}"])
                P.op("dve", lambda e: e.reciprocal(out=gss[b2][:], in_=gss[b2][:]), [f"g_ss{b2}"], [f"g_ss{b2}"])
                P.op("dve", lambda e: e.scalar_tensor_tensor(out=ot_[b2][:], in0=at_[b3][:], scalar=gss[b2][:], in1=gfin[:], op0=ALU.mult, op1=ALU.mult),
                     [f"g_a{b3}", f"g_ss{b2}", "g_gfin"], [f"g_o{b2}"])
                if RAW_OUT:
                    P.dma("sp", lambda e: e.dma_start(out=out_d[ti * 128:(ti + 1) * 128, :], in_=at_[b3][:]), f"g_st{b2}", reads=[f"g_a{b3}"], writes=["out"])
                else:
                    P.dma("sp", lambda e: e.dma_start(out=out_d[ti * 128:(ti + 1) * 128, :], in_=ot_[b2][:]), f"g_st{b2}", reads=[f"g_o{b2}"], writes=["out"])

            for ti in range(8 if DBG_D else 0, 64):
                fin_tile(ti)
            P.barrier()

        if PHASES <= 6:
            tmp = sb("tmpo", [128, D], F32)
            P.op("dve", lambda e: e.memset(tmp[:], 0.0), writes=["tmpo"])
            P.dma("sp", lambda e: e.dma_start(out=out_d[0:128, :], in_=tmp[:]), "o", reads=["tmpo"], writes=["out"])
            P.barrier()
        P.emit()
    return nc, outs


def _bf16_eye():
    import ml_dtypes
    return np.eye(128, dtype=np.float32).astype(ml_dtypes.bfloat16)


def make_inputs(inp, b):
    f = lambda a: np.ascontiguousarray(a, dtype=np.float32)
    m = {
        "x": f(inp["x"][b]),
        "mem": f(inp["mem"][b]),
        "w_in": f(inp["w_in"][0]),
        "gmixT": f(inp["norm_mix_g"][0].reshape(8, 128).T),
        "gmemT": f(inp["norm_mem_g"][0].reshape(8, 128).T),
        "w_mem_kv": f(inp["w_mem_kv"][0]),
        "identb_in": _bf16_eye(),
        "identf_in": np.eye(128, dtype=np.float32),
    }
    rpb = f(inp["na_rpb"][0])
    p = np.arange(128)
    q = np.arange(64)
    coff = (p[:, None] % 64) - q[None, :] + 15
    valid = (coff >= 0) & (coff < 31)
    coffc = np.clip(coff, 0, 30)
    aa = np.arange(14)
    ro = (2 * (aa % 7) + aa // 7)[None, :, None] + (p[:, None, None] // 64)
    bias = rpb[:, ro, coffc[:, None, :]]
    bias = np.where(valid[None, :, None, :], bias, 0.0).transpose(1, 0, 2, 3)
    m["biasP_in"] = f(bias.reshape(128, 6 * 14 * 64))
    hp_ = lambda a: f(a.reshape(6, 64).T)
    mu = np.zeros((64, 2, 20), np.float32)
    for d in range(2):
        for j in range(3):
            mu[:, d, j * 6:(j + 1) * 6] = inp["rw_mu_rkv"][0, d, j].reshape(6, 64).T
        mu[:, d, 18] = inp["rw_mu_w"][0, d]
        mu[:, d, 19] = inp["rw_mu_a"][0, d]
    m["mu_in"] = f(mu.reshape(64, 40))
    w2a = np.zeros((65, 2, 384), np.float32)
    a2a = np.zeros((65, 2, 384), np.float32)
    for d in range(2):
        w2a[0:64, d] = inp["rw_w2"][0, d]
        w2a[64, d] = inp["rw_w0"][0, d]
        a2a[0:64, d] = inp["rw_a2"][0, d]
        a2a[64, d] = inp["rw_a0"][0, d]
    m["w2a_in"] = f(w2a.reshape(65, 768))
    m["a2a_in"] = f(a2a.reshape(65, 768))
    m["kkp_in"] = hp_(inp["rw_k_k"][0])
    m["kap_in"] = hp_(inp["rw_k_a"][0])
    m["rkp_in"] = hp_(inp["rw_r_k"][0].reshape(384))
    m["g2_in"] = f(inp["rw_g2"][0].reshape(2, 64, 384).transpose(1, 0, 2).reshape(64, 768))
    m["lng_in"] = f(np.broadcast_to(inp["rw_ln_g"][0][None, :], (64, 384)))
    m["lnb_in"] = f(np.broadcast_to(inp["rw_ln_b"][0][None, :], (64, 384)))
    ii = np.arange(64)
    mU = (ii[:, None] < ii[None, :]).astype(np.float32)
    mUi = (ii[:, None] <= ii[None, :]).astype(np.float32)
    mL = (ii[:, None] > ii[None, :]).astype(np.float32)
    m["msk_in"] = f(np.concatenate([mU, mUi, mL], axis=1))
    m["jrev_in"] = f(np.eye(64)[::-1])
    m["w_branch_na"] = f(inp["w_branch_na"][0])
    m["w_branch_rw"] = f(inp["w_branch_rw"][0])
    m["w_branch_mem"] = f(inp["w_branch_mem"][0])
    m["w_out"] = f(inp["w_out"][0])
    m["w_router"] = f(inp["w_router"][0])
    m["gffn_in"] = f(np.broadcast_to(inp["norm_ffn_g"][0][None, :], (128, D)))
    tail = np.zeros((128, 64, 4), np.float32)
    tail[:, :, 0] = np.arange(128)[:, None]
    tail[:, :, 1] = np.arange(64)[None, :]
    m["tail_in"] = f(tail.reshape(128, 256))
    pp = np.arange(128)
    m["ltm_in"] = f((pp[:, None] < pp[None, :]).astype(np.float32))
    m["gfin_in"] = f(np.broadcast_to(inp["norm_final_g"][None, :], (128, D)))
    m["w_exp_gate"] = f(inp["w_exp_gate"][0])
    m["w_exp_up"] = f(inp["w_exp_up"][0])
    m["w_exp_down"] = f(inp["w_exp_down"][0])
    cs = np.clip(q - 8, 0, 48)
    kc = p % 64
    m["maskP_in"] = f(((kc[:, None] >= cs[None, :]) & (kc[:, None] < cs[None, :] + 16)).astype(np.float32))
    return m


def kernel(**inputs):
    nc, outs = build()
    in_maps = [make_inputs(inputs, c % 4) for c in range(8)]
    res = run_bass_kernel_spmd(nc, in_maps, core_ids=list(range(8)))
    if DEBUG:
        kernel.last = res
    out = np.stack([np.asarray(res.results[b]["out"]) for b in range(4)], 0)
    return out.astype(np.float32)
```

```python
import numpy as np
from contextlib import ExitStack
import concourse.bass as bass
import concourse.mybir as mybir
from concourse.bass_utils import run_bass_kernel_spmd

F32 = mybir.dt.float32
BF16 = mybir.dt.bfloat16
I32 = mybir.dt.int32
AF = mybir.ActivationFunctionType
ALU = mybir.AluOpType
AX = mybir.AxisListType

S = 8192
D = 1024
NT = S // 128
DEBUG = False
PHASES = 99
SEM_ROT = 20000
NA_BARRIER = False
RW_CHUNKS = 128
N_EXP = 16
RAW_OUT = False
D_BARRIER = False
DBG_D = False
ALL_EXT = False
DBG_NAMES = ()


class Prog:
    ENG = ("pe", "dve", "act", "pool", "sp")

    def __init__(self, nc, es):
        self.nc = nc
        self.es = es
        self.streams = {e: [] for e in self.ENG}
        self.cur = {}
        self.res = {}
        self.seen = {e: {} for e in self.ENG}
        self.dmasem = {}
        self.freed = []
        self.retired = []
        self.nsem = 0
        for e in self.ENG:
            self._newsem(e)

    def _mksem(self, name):
        self.nsem += 1
        return self.es.enter_context(self.nc.semaphore(f"{name}_{self.nsem}"))

    def _newsem(self, e):
        self.cur[e] = [self._mksem("s" + e), 0]

    def _need(self, eng, waits, sv):
        if sv is None:
            return
        sem, val = sv
        k = id(sem)
        if self.seen[eng].get(k, (None, 0))[1] >= val:
            return
        if k not in waits or waits[k][1] < val:
            waits[k] = (sem, val)

    def _deps(self, eng, reads, writes):
        waits = {}
        for key in reads:
            r = self.res.get(key)
            if r is not None:
                self._need(eng, waits, r[0])
        for key in writes:
            r = self.res.get(key)
            if r is not None:
                self._need(eng, waits, r[0])
                for sv in r[1].values():
                    self._need(eng, waits, sv)
        for k, sv in waits.items():
            self.seen[eng][k] = sv
        return list(waits.values())

    def _mark(self, tag, sv, reads, writes):
        for key in reads:
            r = self.res.setdefault(key, [None, {}])
            r[1][tag] = sv
        for key in writes:
            self.res[key] = [sv, {}]

    def op(self, eng, fns, reads=(), writes=()):
        if callable(fns):
            fns = [fns]
        waits = self._deps(eng, reads, writes)
        c = self.cur[eng]
        if c[1] >= SEM_ROT:
            self._newsem(eng)
            c = self.cur[eng]
        c[1] += 1
        sv = (c[0], c[1])
        self.streams[eng].append((waits, fns, (c[0], 1)))
        self._mark(eng, sv, reads, writes)
        return sv

    def dma(self, q, fn, key, reads=(), writes=()):
        waits = self._deps(q, reads, writes)
        d = self.dmasem.get(key)
        if d is None or d[1] >= 30000:
            if d is not None:
                self.retired.append(d)
            d = self.dmasem[key] = self._getdsem()
        d[1] += 16
        sv = (d[0], d[1])
        self.streams[q].append((waits, [fn], (d[0], 16)))
        self._mark(("dma", key), sv, reads, writes)
        return sv

    def _getdsem(self):
        while self.freed:
            d = self.freed.pop()
            if d[1] < 20000:
                return d
        return [self._mksem("d"), 0]

    def barrier(self):
        targets = [(d[0], d[1]) for d in self.retired]
        self.retired = []
        for e in self.ENG:
            c = self.cur[e]
            if c[1] > 0:
                targets.append((c[0], c[1]))
        for d in self.dmasem.values():
            targets.append((d[0], d[1]))
        for e in self.ENG:
            ws = [t for t in targets if self.seen[e].get(id(t[0]), (None, 0))[1] < t[1]]
            for t in ws:
                self.seen[e][id(t[0])] = t
            self.streams[e].append((ws, [], None))
        self.res = {}
        self.freed.extend(self.dmasem.values())
        self.dmasem = {}

    def emit(self):
        engmap = {"pe": "tensor", "dve": "vector", "act": "scalar", "pool": "gpsimd", "sp": "sync"}
        with self.nc.Block() as block:
            for e in self.ENG:
                stream = self.streams[e]

                def body(engine, stream=stream):
                    for waits, fns, inc in stream:
                        for sem, val in waits:
                            engine.wait_ge(sem, val)
                        ins = None
                        for f in fns:
                            ins = f(engine)
                        if inc is not None:
                            ins.then_inc(inc[0], inc[1])
                getattr(block, engmap[e])(body)


_BREG = {}


def breg(e, val):
    if val not in _BREG:
        _BREG[val] = e.to_reg(val)
    return _BREG[val]


def mm(P, out, pairs, reads, writes):
    n = len(pairs)
    fns = [(lambda e, l=l, r=r, i=i: e.matmul(out, l, r, start=(i == 0), stop=(i == n - 1)))
           for i, (l, r) in enumerate(pairs)]
    return P.op("pe", fns, reads, writes)


class RR:
    def __init__(self, items):
        self.items = list(items)
        self.i = 0

    def next(self):
        v = self.items[self.i % len(self.items)]
        self.i += 1
        return v


def build():
    _BREG.clear()
    nc = bass.Bass("TRN2", target_bir_lowering=False)
    try:
        nc.allow_low_precision("bf16 matmul operands with fp32 accumulation")
    except Exception:
        pass
    outs = {}

    def din(name, shape, dt=F32):
        return nc.dram_tensor(name, list(shape), dt, kind="ExternalInput").ap()

    def scratch(name, shape, dt):
        kind = "ExternalOutput" if ((DEBUG and name in DBG_NAMES) or ALL_EXT) else "Internal"
        t = nc.dram_tensor(name, list(shape), dt, kind=kind).ap()
        if DEBUG:
            outs[name] = t
        return t

    x_d = din("x", [S, D])
    mem_d = din("mem", [256, D])
    w_in = din("w_in", [D, 5888])
    gmixT = din("gmixT", [128, 8])
    gmemT = din("gmemT", [128, 8])
    w_mem_kv = din("w_mem_kv", [D, 512])
    identb_d = din("identb_in", [128, 128], BF16)
    identf_d = din("identf_in", [128, 128])
    out_d = nc.dram_tensor("out", [S, D], F32, kind="ExternalOutput").ap()

    hT_d = scratch("hT_s", [D, S], BF16)
    qk_d = scratch("qk_s", [12, 64, S], BF16)
    v_d = scratch("v_s", [S, 390], BF16)
    prw_d = scratch("prw_s", [22, 64, S + 2], F32)
    mq_d = scratch("mq_s", [4, 64, S], BF16)
    ynaT_d = scratch("ynaT_s", [384, S], BF16)
    ymemT_d = scratch("ymemT_s", [256, S], BF16)
    biasP_d = din("biasP_in", [128, 6 * 14 * 64])
    maskP_d = din("maskP_in", [128, 64])
    mu_d = din("mu_in", [64, 40])
    w2a_d = din("w2a_in", [65, 768])
    a2a_d = din("a2a_in", [65, 768])
    kkp_d = din("kkp_in", [64, 6])
    kap_d = din("kap_in", [64, 6])
    rkp_d = din("rkp_in", [64, 6])
    g2_d = din("g2_in", [64, 768])
    lng_d = din("lng_in", [64, 384])
    lnb_d = din("lnb_in", [64, 384])
    msk_d = din("msk_in", [64, 192])
    jrev_d = din("jrev_in", [64, 64])
    wbna_d = din("w_branch_na", [384, D])
    wbrw_d = din("w_branch_rw", [384, D])
    wbmem_d = din("w_branch_mem", [256, D])
    wout_d = din("w_out", [D, D])
    wrt_d = din("w_router", [D, 16])
    gffn_d = din("gffn_in", [128, D])
    tail_d = din("tail_in", [128, 256])
    acc_d = scratch("acc_s", [S, D], F32)
    ltm_d = din("ltm_in", [128, 128])
    gfin_d = din("gfin_in", [128, D])
    wgate_d = din("w_exp_gate", [16, D, 2048])
    wup_d = din("w_exp_up", [16, D, 2048])
    wdown_d = din("w_exp_down", [16, 2048, D])
    xe_d = [scratch(f"xe_s{e}", [1024, 1028], BF16) for e in range(16)]
    rank_dbg = scratch("rank_dbg", [128, 1024], F32) if DEBUG else None
    h2_d = scratch("h2_s", [S, 1028], BF16)
    aff_d = scratch("aff_s", [S, 16], F32)
    yb_d = scratch("yb_s", [S, 2, 384], F32)
    yrwT_d = scratch("yrwT_s", [384, S], BF16)

    with ExitStack() as es:
        P = Prog(nc, es)

        def sb(name, shape, dt, stack=es):
            return stack.enter_context(nc.sbuf_tensor("sb_" + name, list(shape), dt))

        def ps(name, shape, dt, stack=es):
            return stack.enter_context(nc.psum_tensor("ps_" + name, list(shape), dt))

        identb = sb("identb", [128, 128], BF16)
        identf = sb("identf", [128, 128], F32)
        gmix = sb("gmix", [128, 8], F32)
        gmem = sb("gmem", [128, 8], F32)
        mkT = sb("mkT", [64, 4, 256], BF16)
        mv = sb("mv", [128, 2, 4, 65], BF16)
        zero = sb("zero", [64, 32], F32)
        P.dma("sp", lambda e: e.dma_start(out=identb[:], in_=identb_d), "c_identb", writes=["identb"])
        P.dma("sp", lambda e: e.dma_start(out=identf[:], in_=identf_d), "c_identf", writes=["identf"])
        P.dma("sp", lambda e: e.dma_start(out=gmix[:], in_=gmixT), "c_gmix", writes=["gmix"])
        P.dma("sp", lambda e: e.dma_start(out=gmem[:], in_=gmemT), "c_gmem", writes=["gmem"])
        P.op("dve", lambda e: e.memset(zero[:], 0.0), writes=["zero"])
        P.op("dve", lambda e: e.memset(mv[:], 1.0), writes=["mv"])

        with ExitStack() as pa:
            NCOL = 2816
            wA = sb("wA", [128, 8, NCOL], BF16, pa)
            wkv = sb("wkv", [128, 8, 512], BF16, pa)
            xt = [sb(f"xt{i}", [128, D], F32, pa) for i in range(3)]
            xn = [sb(f"xn{i}", [128, D], BF16, pa) for i in range(2)]
            sq = sb("sqjunk", [128, D], BF16, pa)
            ss = [sb(f"ss{i}", [128, 1], F32, pa) for i in range(3)]
            rs = [sb(f"rs{i}", [128, 1], F32, pa) for i in range(3)]
            hTb = [sb(f"hTb{i}", [128, 8, 512], BF16, pa) for i in range(2)]
            qk_sb = [sb(f"qk_sb{i}", [64, 12, 512], BF16, pa) for i in range(2)]
            v_sb = [sb(f"v_sb{i}", [128, 4, 390], BF16, pa) for i in range(2)]
            rw_sb = [sb(f"rw_sb{i}", [64, 11, 512], F32, pa) for i in range(2)]
            mq_sb = [sb(f"mq_sb{i}", [64, 4, 512], BF16, pa) for i in range(2)]
            ptr = [ps(f"ptr{i}", [128, 8, 128], BF16, pa) for i in range(2)]
            pacc = [ps(f"pacc{i}", [128, 512], F32, pa) for i in range(5)]

            w_in_v = w_in.rearrange("(c p) n -> p c n", p=128)
            for c in range(8):
                P.dma("pool", lambda e, c=c: e.dma_start(out=wA[:, c, :], in_=w_in_v[:, c, 0:NCOL]),
                      "wA", writes=[f"wA{c}"])
            wkv_v = w_mem_kv.rearrange("(c p) n -> p c n", p=128)
            P.dma("pool", lambda e: e.dma_start(out=wkv[:], in_=wkv_v), "wkv", writes=["wkv"])
            for g0 in (0, 11):
                for col in (0, S + 1):
                    P.dma("sp", lambda e, g0=g0, col=col: e.dma_start(
                        out=prw_d[g0:g0 + 11, :, col:col + 1].rearrange("g p t -> p g t"),
                        in_=zero[:, 0:11].unsqueeze(2), allow_slow_non_contiguous=True), "zpad", reads=["zero"], writes=["prw_pad"])
            for i in range(2):
                P.op("pool", lambda e, i=i: e.memset(v_sb[i][:], 1.0), writes=[f"v_sb{i}"])

            evq = RR(["act", "dve"])
            pq = RR(range(5))
            ldq = RR(["sp", "act"])

            def evac(dst, src, reads, writes, eng=None):
                eng = eng or evq.next()
                if eng == "act":
                    P.op("act", lambda e: e.copy(out=dst, in_=src), reads, writes)
                else:
                    P.op("dve", lambda e: e.tensor_copy(out=dst, in_=src), reads, writes)

            def norm_tile(src_ap, ti, gtile, gkey, dst, dstkey, col0):
                b3 = ti % 3
                b2 = ti % 2
                P.dma(ldq.next(), lambda e: e.dma_start(out=xt[b3][:], in_=src_ap), f"xt{b3}", writes=[f"xt{b3}"])
                P.op("act", lambda e: e.activation(out=sq[:], in_=xt[b3][:], func=AF.Square, accum_out=ss[b3][:]),
                     reads=[f"xt{b3}"], writes=["sq", f"ss{b3}"])
                P.op("act", lambda e: e.activation(out=rs[b3][:], in_=ss[b3][:], func=AF.Sqrt, scale=1.0 / D, bias=eps_t[:]),
                     reads=[f"ss{b3}", "eps"], writes=[f"rs{b3}"])
                P.op("dve", lambda e: e.reciprocal(out=rs[b3][:], in_=rs[b3][:]), reads=[f"rs{b3}"], writes=[f"rs{b3}"])
                P.op("act", lambda e: e.activation(out=xn[b2][:], in_=xt[b3][:], func=AF.Copy, scale=rs[b3][:]),
                     reads=[f"xt{b3}", f"rs{b3}"], writes=[f"xn{b2}"])
                P.op("pe", [(lambda e, c=c: e.transpose(out=ptr[b2][:, c, :], in_=xn[b2][:, c * 128:(c + 1) * 128], identity=identb[:]))
                            for c in range(8)], reads=[f"xn{b2}", "identb"], writes=[f"ptr{b2}"])
                P.op("dve", lambda e: e.tensor_tensor(out=dst[:, :, col0:col0 + 128], in0=ptr[b2][:],
                                                      in1=gtile[:].unsqueeze(2).to_broadcast([128, 8, 128]), op=ALU.mult),
                     reads=[f"ptr{b2}", gkey], writes=[dstkey])

            eps_t = sb("eps_t", [128, 1], F32, pa)
            P.op("dve", lambda e: e.memset(eps_t[:], 1e-6), writes=["eps"])

            mhT = sb("mhT", [128, 8, 256], BF16, pa)
            for t in range(2):
                norm_tile(mem_d[t * 128:(t + 1) * 128, :], t, gmem, "gmem", mhT, "mhT", t * 128)
            for hh in range(4):
                pi = pq.next()
                mm(P, pacc[pi][0:64, 0:256], [(wkv[:, c, hh * 64:(hh + 1) * 64], mhT[:, c, :]) for c in range(8)],
                   reads=["wkv", "mhT"], writes=[f"pacc{pi}"])
                evac(mkT[:, hh, :], pacc[pi][0:64, 0:256], [f"pacc{pi}"], ["mkT"])
            for t in range(2):
                pi = pq.next()
                mm(P, pacc[pi][:, 0:256], [(mhT[:, c, t * 128:(t + 1) * 128], wkv[:, c, 256:512]) for c in range(8)],
                   reads=["wkv", "mhT"], writes=[f"pacc{pi}"])
                evac(mv[:, t, :, 0:64], pacc[pi][:, 0:256].rearrange("p (h d) -> p h d", h=4), [f"pacc{pi}"], ["mv"])

            wAkeys = [f"wA{c}" for c in range(8)]
            for blk in range(S // 512):
                hb = blk % 2
                t0 = blk * 512
                for j in range(4):
                    ti = blk * 4 + j
                    norm_tile(x_d[ti * 128:(ti + 1) * 128, :], ti + 2, gmix, "gmix", hTb[hb], f"hTb{hb}", j * 128)
                P.dma("sp", lambda e, hb=hb, t0=t0: e.dma_start(
                    out=hT_d.rearrange("(c p) t -> p c t", p=128)[:, :, t0:t0 + 512], in_=hTb[hb][:]),
                    f"st_hT{hb}", reads=[f"hTb{hb}"], writes=["hT_d"])
                for g in range(12):
                    pi = pq.next()
                    mm(P, pacc[pi][0:64, :], [(wA[:, c, g * 64:(g + 1) * 64], hTb[hb][:, c, :]) for c in range(8)],
                       reads=wAkeys + [f"hTb{hb}"], writes=[f"pacc{pi}"])
                    evac(qk_sb[hb][:, g, :], pacc[pi][0:64, :], [f"pacc{pi}"], [f"qk_sb{hb}"])
                P.dma("sp", lambda e, hb=hb, t0=t0: e.dma_start(
                    out=qk_d[:, :, t0:t0 + 512].rearrange("g p t -> p g t"), in_=qk_sb[hb][:]),
                    f"st_qk{hb}", reads=[f"qk_sb{hb}"], writes=["qk_d"])
                for j in range(4):
                    pi = pq.next()
                    mm(P, pacc[pi][:, 0:384], [(hTb[hb][:, c, j * 128:(j + 1) * 128], wA[:, c, 768:1152]) for c in range(8)],
                       reads=wAkeys + [f"hTb{hb}"], writes=[f"pacc{pi}"])
                    evac(v_sb[hb][:, j, :].rearrange("p (h d) -> p h d", h=6)[:, :, 0:64],
                         pacc[pi][:, 0:384].rearrange("p (h d) -> p h d", h=6), [f"pacc{pi}"], [f"v_sb{hb}"])
                P.dma("act", lambda e, hb=hb, t0=t0: e.dma_start(
                    out=v_d[t0:t0 + 512, :].rearrange("(n p) c -> p n c", p=128), in_=v_sb[hb][:]),
                    f"st_v{hb}", reads=[f"v_sb{hb}"], writes=["v_d"])
                for half in range(2):
                    for gg in range(11):
                        g = half * 11 + gg
                        pi = pq.next()
                        c0 = 1152 + g * 64
                        mm(P, pacc[pi][0:64, :], [(wA[:, c, c0:c0 + 64], hTb[hb][:, c, :]) for c in range(8)],
                           reads=wAkeys + [f"hTb{hb}"], writes=[f"pacc{pi}"])
                        evac(rw_sb[half][:, gg, :], pacc[pi][0:64, :], [f"pacc{pi}"], [f"rw_sb{half}"])
                    P.dma("sp", lambda e, half=half, t0=t0: e.dma_start(
                        out=prw_d[half * 11:half * 11 + 11, :, 1 + t0:1 + t0 + 512].rearrange("g p t -> p g t"),
                        in_=rw_sb[half][:]), f"st_rw{half}", reads=[f"rw_sb{half}"], writes=["prw_d"])
                for g in range(4):
                    pi = pq.next()
                    c0 = 2560 + g * 64
                    mm(P, pacc[pi][0:64, :], [(wA[:, c, c0:c0 + 64], hTb[hb][:, c, :]) for c in range(8)],
                       reads=wAkeys + [f"hTb{hb}"], writes=[f"pacc{pi}"])
                    evac(mq_sb[hb][:, g, :], pacc[pi][0:64, :], [f"pacc{pi}"], [f"mq_sb{hb}"])
                P.dma("act", lambda e, hb=hb, t0=t0: e.dma_start(
                    out=mq_d[:, :, t0:t0 + 512].rearrange("g p t -> p g t"), in_=mq_sb[hb][:]),
                    f"st_mq{hb}", reads=[f"mq_sb{hb}"], writes=["mq_d"])
            P.barrier()

        if PHASES >= 2:
          with ExitStack() as pb:
            EP = sb("EP", [128, 6, 14, 64], BF16, pb)
            biasP = sb("biasP", [128, 6 * 14 * 64], F32, pb)
            maskP = sb("maskP", [128, 64], F32, pb)
            bank = [ps(f"bank{i}", [128, 512], F32, pb) for i in range(6)]
            pbt = ps("pbt", [128, 1024], BF16, pb)
            P.dma("sp", lambda e: e.dma_start(out=biasP[:], in_=biasP_d), "c_biasP", writes=["biasP"])
            P.dma("sp", lambda e: e.dma_start(out=maskP[:], in_=maskP_d), "c_maskP", writes=["maskP"])
            for h in range(6):
                P.op("act", lambda e, h=h: e.activation(out=biasP[:, h * 896:(h + 1) * 896], in_=biasP[:, h * 896:(h + 1) * 896], func=AF.Exp),
                     reads=["biasP"], writes=["biasP"])
                P.op("dve", lambda e, h=h: e.tensor_tensor(out=EP[:, h, :, :], in0=biasP[:, h * 896:(h + 1) * 896].rearrange("p (a q) -> p a q", q=64),
                                                           in1=maskP[:].unsqueeze(1).to_broadcast([128, 14, 64]), op=ALU.mult),
                     reads=["biasP", "maskP"], writes=["EP"])
            qrow = [sb(f"qrow{i}", [64, 6, 64], BF16, pb) for i in range(2)]
            kwin = [sb(f"kwin{i}", [64, 6, 512], BF16, pb) for i in range(2)]
            vwin = [sb(f"vwin{i}", [128, 4, 390], BF16, pb) for i in range(2)]
            pex = [sb(f"pex{i}", [128, 2, 4, 64], BF16, pb) for i in range(3)]
            rec = [sb(f"rec{i}", [128, 6], F32, pb) for i in range(2)]
            yrow = [sb(f"yrow{i}", [128, 384], BF16, pb) for i in range(2)]
            ynaT_sb = [sb(f"ynaT_sb{i}", [128, 3, 512], BF16, pb) for i in range(2)]
            mq_b = [sb(f"mq_b{i}", [64, 4, 512], BF16, pb) for i in range(2)]
            mpex = [sb(f"mpex{i}", [128, 2, 512], BF16, pb) for i in range(4)]
            ymT_sb = [sb(f"ymT_sb{i}", [128, 2, 512], BF16, pb) for i in range(2)]
            bq = RR(range(6))
            BK = lambda k: [f"bank{k}"] + [f"bank{k}_{h}" for h in range(6)]

            def na_hp(r, b2, ro0, hp, pvb, pv):
                sbk = bq.next()
                sT = bank[sbk][:].rearrange("p (a j q) -> p a j q", a=2, j=4)
                fns = []
                for a in range(2):
                    for j in range(4):
                        fns.append(lambda e, a=a, j=j: e.matmul(sT[:, a, j, :], kwin[b2][:, hp * 2 + a, j * 128:(j + 1) * 128],
                                                                qrow[b2][:, hp * 2 + a, :], start=True, stop=True))
                P.op("pe", fns, reads=[f"kwin{b2}", f"qrow{b2}"], writes=BK(sbk))
                P.op("act", lambda e: e.activation(out=pex[hp][:], in_=sT, func=AF.Exp, scale=0.125),
                     reads=BK(sbk), writes=[f"pex{hp}"])
                a0 = (ro0 % 2) * 7 + ro0 // 2
                P.op("dve", lambda e: e.tensor_tensor(out=pex[hp][:], in0=pex[hp][:], in1=EP[:, hp * 2:hp * 2 + 2, a0:a0 + 4, :], op=ALU.mult),
                     reads=[f"pex{hp}", "EP"], writes=[f"pex{hp}"])
                for a in range(2):
                    h = hp * 2 + a
                    mm(P, pv[:, h, :], [(pex[hp][:, a, j, :], vwin[b2][:, j, h * 65:(h + 1) * 65]) for j in range(4)],
                       reads=[f"pex{hp}", f"vwin{b2}"], writes=[f"bank{pvb}_{h}"])

            def na_row(r):
                b2 = r % 2
                rs_ = min(max(r - 4, 0), 120)
                ro0 = rs_ - r + 7
                blk = r // 8
                P.dma("sp", lambda e: e.dma_start(out=qrow[b2][:], in_=qk_d[0:6, :, r * 64:(r + 1) * 64].rearrange("g p t -> p g t")),
                      f"ld_q{b2}", writes=[f"qrow{b2}"])
                P.dma("sp", lambda e: e.dma_start(out=kwin[b2][:], in_=qk_d[6:12, :, rs_ * 64:rs_ * 64 + 512].rearrange("g p t -> p g t")),
                      f"ld_k{b2}", writes=[f"kwin{b2}"])
                P.dma("act", lambda e: e.dma_start(out=vwin[b2][:], in_=v_d[rs_ * 64:rs_ * 64 + 512, :].rearrange("(n p) c -> p n c", p=128)),
                      f"ld_v{b2}", writes=[f"vwin{b2}"])
                pvb = bq.next()
                pv = bank[pvb][0:64, 0:390].rearrange("p (h d) -> p h d", h=6)
                for hp in range(3):
                    na_hp(r, b2, ro0, hp, pvb, pv)
                pvkeys = [f"bank{pvb}_{h}" for h in range(6)]
                P.op("dve", lambda e: e.reciprocal(out=rec[b2][0:64, :], in_=pv[:, :, 64]), reads=pvkeys, writes=[f"rec{b2}"])
                P.op("dve", lambda e: e.tensor_tensor(out=yrow[b2][0:64, :].rearrange("p (h d) -> p h d", h=6), in0=pv[:, :, 0:64],
                                                      in1=rec[b2][0:64, :].unsqueeze(2).to_broadcast([64, 6, 64]), op=ALU.mult),
                     reads=pvkeys + [f"rec{b2}"], writes=[f"yrow{b2}"] + pvkeys)
                P.op("pe", [(lambda e, c=c: e.transpose(out=pbt[:, c * 64:(c + 1) * 64], in_=yrow[b2][0:64, c * 128:(c + 1) * 128], identity=identb[0:64, 0:64]))
                            for c in range(3)], reads=[f"yrow{b2}", "identb"], writes=["pbt_na"])
                yb = blk % 2
                P.op("act", lambda e: e.copy(out=ynaT_sb[yb][:, :, (r % 8) * 64:(r % 8) * 64 + 64], in_=pbt[:, 0:192].rearrange("p (c q) -> p c q", c=3)),
                     reads=["pbt_na"], writes=[f"ynaT_sb{yb}"])
                if r % 8 == 7:
                    t0 = blk * 512
                    P.dma("sp", lambda e: e.dma_start(out=ynaT_d.rearrange("(c p) t -> p c t", p=128)[:, :, t0:t0 + 512], in_=ynaT_sb[yb][:]),
                          f"st_yna{yb}", reads=[f"ynaT_sb{yb}"], writes=["ynaT_d"])
                    mem_block(blk)

            def mem_s(yb, h, kt):
                sbk = bq.next()
                mm(P, bank[sbk][:], [(mkT[:, h, kt * 128:(kt + 1) * 128], mq_b[yb][:, h, :])], reads=["mkT", f"mq_b{yb}"], writes=BK(sbk))
                P.op("act", lambda e: e.activation(out=mpex[h][:, kt, :], in_=bank[sbk][:], func=AF.Exp, scale=0.125),
                     reads=BK(sbk), writes=[f"mpex{h}"])

            def mem_pv(yb, qt):
                pvb = bq.next()
                pvm = bank[pvb][:, 0:260].rearrange("p (h d) -> p h d", h=4)
                for h in range(4):
                    mm(P, pvm[:, h, :], [(mpex[h][:, kt, qt * 128:(qt + 1) * 128], mv[:, kt, h, :]) for kt in range(2)],
                       reads=[f"mpex{h}", "mv"], writes=[f"bank{pvb}_{h}"])
                pk = [f"bank{pvb}_{h}" for h in range(4)]
                rb = qt % 2
                P.op("dve", lambda e: e.reciprocal(out=rec[rb][:, 0:4], in_=pvm[:, :, 64]), reads=pk, writes=[f"rec{rb}"])
                P.op("dve", lambda e: e.tensor_tensor(out=yrow[rb][:, 0:256].rearrange("p (h d) -> p h d", h=4), in0=pvm[:, :, 0:64],
                                                      in1=rec[rb][:, 0:4].unsqueeze(2).to_broadcast([128, 4, 64]), op=ALU.mult),
                     reads=pk + [f"rec{rb}"], writes=[f"yrow{rb}"] + pk)
                P.op("pe", [(lambda e, c=c: e.transpose(out=pbt[:, 512 + c * 128:512 + (c + 1) * 128], in_=yrow[rb][:, c * 128:(c + 1) * 128], identity=identb[:]))
                            for c in range(2)], reads=[f"yrow{rb}", "identb"], writes=["pbt_m"])
                P.op("act", lambda e: e.copy(out=ymT_sb[yb][:, :, qt * 128:(qt + 1) * 128], in_=pbt[:, 512:768].rearrange("p (c q) -> p c q", c=2)),
                     reads=["pbt_m"], writes=[f"ymT_sb{yb}"])

            def mem_block(blk):
                yb = blk % 2
                t0 = blk * 512
                P.dma("act", lambda e: e.dma_start(out=mq_b[yb][:], in_=mq_d[:, :, t0:t0 + 512].rearrange("g p t -> p g t")),
                      f"ld_mq{yb}", writes=[f"mq_b{yb}"])
                for h in range(4):
                    for kt in range(2):
                        mem_s(yb, h, kt)
                for qt in range(4):
                    mem_pv(yb, qt)
                P.dma("sp", lambda e: e.dma_start(out=ymemT_d.rearrange("(c p) t -> p c t", p=128)[:, :, t0:t0 + 512], in_=ymT_sb[yb][:]),
                      f"st_ym{yb}", reads=[f"ymT_sb{yb}"], writes=["ymemT_d"])

            for r in range(128):
                na_row(r)
            P.barrier()

        if PHASES >= 3:
          with ExitStack() as pc:
            C0 = 0.6065306597126334
            cst = {}
            for nm, shp, src in (("mu", [64, 40], mu_d), ("w2a", [65, 768], w2a_d), ("a2a", [65, 768], a2a_d), ("kkp", [64, 6], kkp_d),
                                 ("kap", [64, 6], kap_d), ("rkp", [64, 6], rkp_d), ("g2", [64, 768], g2_d), ("lng", [64, 384], lng_d),
                                 ("lnb", [64, 384], lnb_d), ("msk", [64, 192], msk_d), ("jrev", [64, 64], jrev_d)):
                cst[nm] = sb("c_" + nm, shp, F32, pc)
                P.dma("sp", (lambda e, t=cst[nm], src=src: e.dma_start(out=t[:], in_=src)), "c_" + nm, writes=[nm])
            oma = sb("oma", [64, 6], F32, pc)
            P.op("dve", lambda e: e.tensor_scalar(out=oma[:], in0=cst["kap"][:], scalar1=-1.0, scalar2=1.0, op0=ALU.mult, op1=ALU.add),
                 reads=["kap"], writes=["oma"])
            ones64 = sb("ones64", [64, 384], F32, pc)
            P.op("dve", lambda e: e.memset(ones64[:], 1.0), writes=["ones64"])
            eps12 = sb("eps12", [64, 1], F32, pc)
            P.op("dve", lambda e: e.memset(eps12[:], 64e-5), writes=["eps12"])
            offs = [sb(f"offs{i}", [64, 6], F32, pc) for i in range(2)]
            twa = [sb(f"twa{i}", [65, 2, 64], F32, pc) for i in range(2)]
            for i in range(2):
                P.op("dve", lambda e, i=i: e.memset(offs[i][:], 0.0), writes=[f"offs{i}"])
                P.op("dve", lambda e, i=i: e.memset(twa[i][:], 1.0), writes=[f"twa{i}"])
            Hs = [sb(f"H{i}", [64, 384], F32, pc) for i in range(2)]
            rbank = [ps(f"rbank{i}", [64, 512], F32, pc) for i in range(7)]
            rbt = ps("rbt", [128, 1024], BF16, pc)
            rq = RR(range(7))
            names3 = ["mx", "df"]
            names = ["sg", "al", "L", "Lx", "Ld", "Pinc", "Pinv", "Pexc", "Pdec", "kkr", "sq", "nrm", "kk", "t1", "k2", "bv",
                     "at", "bt", "kt", "rt", "bh", "kh", "rk", "Vt", "bhT", "khT", "MabT", "Nab", "MakT", "MrbT", "MrkT",
                     "X0", "X1", "Na", "Nb", "Ma", "Mb", "W", "U", "dgP", "yo", "ybl", "cen", "sqc", "sgg", "yn"]
            tl = {}
            for i in range(2):
                tl[("X", i)] = sb(f"rwX{i}", [64, 22, 66], F32, pc)
                tl[("yst", i)] = sb(f"rw_yst{i}", [64, 2, 384], F32, pc)
                tl[("yld", i)] = sb(f"rw_yld{i}", [64, 2, 384], F32, pc)
                tl[("yrwT", i)] = sb(f"rw_yrwT{i}", [128, 3, 512], BF16, pc)
                if i == 1:
                    for nm in ("at", "bt", "kt", "rt", "bh", "kh", "bhT", "khT", "Vt", "dgP", "MabT", "Nab", "MakT", "MrbT", "MrkT"):
                        tl[(nm, i)] = sb(f"rw_{nm}{i}", [64, 6, 64], F32, pc)
                    tl[("bon", i)] = sb(f"rw_bon{i}", [64, 6], F32, pc)
                    tl[("Pend", i)] = sb(f"rw_Pend{i}", [64, 6], F32, pc)
                    continue
                for nm in names3:
                    tl[(nm, i)] = sb(f"rw_{nm}{i}", [64, 20, 64], F32, pc)
                for nm in names:
                    tl[(nm, i)] = sb(f"rw_{nm}{i}", [64, 6, 64], F32, pc)
                tl[("bon", i)] = sb(f"rw_bon{i}", [64, 6], F32, pc)
                tl[("Lend", i)] = sb(f"rw_Lend{i}", [64, 6], F32, pc)
                tl[("Pend", i)] = sb(f"rw_Pend{i}", [64, 6], F32, pc)
                tl[("st", i)] = sb(f"rw_st{i}", [64, 6], F32, pc)
                tl[("st2", i)] = sb(f"rw_st2{i}", [64, 6], F32, pc)
                tl[("yrwb", i)] = sb(f"rw_yrwb{i}", [64, 384], BF16, pc)
            evr = RR(["dve", "pool"])
            mskU = cst["msk"][:, 0:64]
            mskUi = cst["msk"][:, 64:128]
            mskL = cst["msk"][:, 128:192]
            b6 = lambda ap2: ap2.unsqueeze(1).to_broadcast([64, 6, 64])
            c6 = lambda ap2: ap2.unsqueeze(2).to_broadcast([64, 6, 64])

            def rw_chunk(d, i):
                pb_ = i % 2
                DB = ("X", "yst", "yld", "at", "bt", "kt", "rt", "bh", "kh", "bhT", "khT", "Vt", "dgP", "MabT", "Nab", "MakT", "MrbT", "MrkT", "bon", "Pend")
                T = lambda nm: tl[(nm, pb_ if nm in DB else 0)]
                K = lambda nm: f"rw_{nm}{pb_ if nm in DB else 0}"
                ci = i if d == 0 else 127 - i
                t0 = ci * 64
                ng = 22 if d == 0 else 20
                X = T("X")
                P.dma("sp", lambda e: e.dma_start(out=X[:, 0:ng, :], in_=prw_d[0:ng, :, t0:t0 + 66].rearrange("g p t -> p g t")),
                      f"ld_X{pb_}", writes=[K("X")])
                if d == 0:
                    cur, shf = X[:, 0:20, 1:65], X[:, 0:20, 0:64]
                else:
                    cur, shf = X[:, 0:20, 64:0:-1], X[:, 0:20, 65:1:-1]

                def tt(eng, out, in0, in1, op, reads, writes):
                    P.op(eng, lambda e: e.tensor_tensor(out=out, in0=in0, in1=in1, op=op), reads, writes)

                def act(out, in_, func, reads, writes, **kw):
                    P.op("act", lambda e: e.activation(out=out, in_=in_, func=func, **kw), reads, writes)

                def mm6(bank_i, lk, rk_, lhs_f, rhs_f, extra_reads=()):
                    ov = rbank[bank_i][:, 0:384].rearrange("p (h t) -> p h t", h=6)
                    fns = [(lambda e, h=h: e.matmul(ov[:, h, :], lhs_f(h), rhs_f(h), start=True, stop=True)) for h in range(6)]
                    P.op("pe", fns, reads=list(lk) + list(rk_) + list(extra_reads), writes=[f"rbank{bank_i}"])
                    return ov

                def mmacc(bank_i, terms, reads):
                    ov = rbank[bank_i][:, 0:384].rearrange("p (h t) -> p h t", h=6)
                    fns = []
                    n = len(terms)
                    for h in range(6):
                        for k_, (lf, rf) in enumerate(terms):
                            fns.append(lambda e, h=h, k_=k_, lf=lf, rf=rf: e.matmul(ov[:, h, :], lf(h), rf(h), start=(k_ == 0), stop=(k_ == n - 1)))
                    P.op("pe", fns, reads=reads, writes=[f"rbank{bank_i}"])
                    return ov

                mub = cst["mu"][:, d * 20:(d + 1) * 20].unsqueeze(2).to_broadcast([64, 20, 64])
                mub_s = cst["mu"][:, d * 20 + 18:d * 20 + 20].unsqueeze(2).to_broadcast([64, 2, 64])
                mub_b = cst["mu"][:, d * 20:d * 20 + 18].unsqueeze(2).to_broadcast([64, 18, 64])
                Kw, Kdw = K("mx") + "w", K("df") + "w"
                dfs, dfb = T("df")[:, 18:20, :], T("df")[:, 0:18, :]
                mxs_, mxb = T("mx")[:, 18:20, :], T("mx")[:, 0:18, :]
                tt("dve", dfs, shf[:, 18:20, :], cur[:, 18:20, :], ALU.subtract, [K("X")], [Kdw])
                tt("dve", dfs, dfs, mub_s, ALU.mult, [Kdw, "mu"], [Kdw])
                tt("dve", mxs_, dfs, cur[:, 18:20, :], ALU.add, [Kdw, K("X")], [Kw])
                tt("pool", dfb, shf[:, 0:18, :], cur[:, 0:18, :], ALU.subtract, [K("X")], [K("df")])
                tt("pool", dfb, dfb, mub_b, ALU.mult, [K("df"), "mu"], [K("df")])
                tt("pool", mxb, dfb, cur[:, 0:18, :], ALU.add, [K("df"), K("X")], [K("mx")])
                mx = T("mx")
                r_, k_, v_ = mx[:, 0:6, :], mx[:, 6:12, :], mx[:, 12:18, :]
                tw = twa[pb_]
                act(tw[0:64, 0, :], mx[:, 18, :], AF.Tanh, [Kw], [f"twa{pb_}"])
                P.op("dve", lambda e: e.tensor_copy(out=tw[0:64, 1, :], in_=mx[:, 19, :]), [Kw], [f"twa{pb_}"])
                zb = rq.next()
                zp = mm6(zb, [f"twa{pb_}"], ["w2a"], lambda h: cst["w2a"][:, d * 384 + h * 64:d * 384 + (h + 1) * 64], lambda h: tw[:, 0, :])
                act(T("sg")[:], zp, AF.Sigmoid, [f"rbank{zb}"], [K("sg")])
                ab = rq.next()
                ap_ = mm6(ab, [f"twa{pb_}"], ["a2a"], lambda h: cst["a2a"][:, d * 384 + h * 64:d * 384 + (h + 1) * 64], lambda h: tw[:, 1, :])
                act(T("al")[:], ap_, AF.Sigmoid, [f"rbank{ab}"], [K("al")])
                Lf = T("L")[:].rearrange("p h t -> p (h t)")
                P.op("dve", lambda e: e.tensor_tensor_scan(out=Lf, data0=ones64[:], data1=T("sg")[:].rearrange("p h t -> p (h t)"),
                                                           initial=0.0, op0=ALU.mult, op1=ALU.add), [K("sg"), "ones64"], [K("L")])
                of = offs[pb_]
                P.op("dve", lambda e: e.tensor_copy(out=of[:, 1:6], in_=T("L")[:, 0:5, 63]), [K("L")], [f"offs{pb_}"])
                tt("dve", T("L")[:], T("L")[:], c6(of[:]), ALU.subtract, [K("L"), f"offs{pb_}"], [K("L")])
                tt("dve", T("Lx")[:], T("L")[:], T("sg")[:], ALU.subtract, [K("L"), K("sg")], [K("Lx")])
                P.op("dve", lambda e: e.tensor_copy(out=T("Lend")[:], in_=T("L")[:, :, 63]), [K("L")], [K("Lend")])
                tt("dve", T("Ld")[:], T("L")[:], c6(T("Lend")[:]), ALU.subtract, [K("L"), K("Lend")], [K("Ld")])
                act(T("Pinc")[:], T("L")[:], AF.Exp, [K("L")], [K("Pinc")], scale=-C0)
                act(T("Pinv")[:], T("L")[:], AF.Exp, [K("L")], [K("Pinv")], scale=C0)
                act(T("Pexc")[:], T("Lx")[:], AF.Exp, [K("Lx")], [K("Pexc")], scale=-C0)
                act(T("Pdec")[:], T("Ld")[:], AF.Exp, [K("Ld")], [K("Pdec")], scale=C0)
                act(T("Pend")[:], T("Lend")[:], AF.Exp, [K("Lend")], [K("Pend")], scale=-C0)
                tt("dve", T("kkr")[:], k_, c6(cst["kkp"][:]), ALU.mult, [K("mx"), "kkp"], [K("kkr")])
                tt("dve", T("sq")[:], T("kkr")[:], T("kkr")[:], ALU.mult, [K("kkr")], [K("sq")])
                sb_ = rq.next()
                ssp = mm6(sb_, [K("sq")], ["ones64"], lambda h: ones64[:, 0:64], lambda h: T("sq")[:, h, :])
                act(T("nrm")[:], ssp, AF.Sqrt, [f"rbank{sb_}"], [K("nrm")])
                P.op("dve", lambda e: e.tensor_scalar_max(out=T("nrm")[:], in0=T("nrm")[:], scalar1=1e-12), [K("nrm")], [K("nrm")])
                P.op("dve", lambda e: e.reciprocal(out=T("nrm")[:], in_=T("nrm")[:]), [K("nrm")], [K("nrm")])
                tt("dve", T("kk")[:], T("kkr")[:], T("nrm")[:], ALU.mult, [K("kkr"), K("nrm")], [K("kk")])
                tt("dve", T("t1")[:], T("al")[:], c6(cst["kap"][:]), ALU.mult, [K("al"), "kap"], [K("t1")])
                tt("dve", T("t1")[:], T("t1")[:], c6(oma[:]), ALU.add, [K("t1"), "oma"], [K("t1")])
                tt("dve", T("k2")[:], k_, T("t1")[:], ALU.mult, [K("mx"), K("t1")], [K("k2")])
                tt("dve", T("bv")[:], T("kk")[:], T("al")[:], ALU.mult, [K("kk"), K("al")], [K("bv")])
                P.op("dve", lambda e: e.scalar_tensor_tensor(out=T("at")[:], in0=T("kk")[:], scalar=-1.0, in1=T("Pexc")[:], op0=ALU.mult, op1=ALU.mult),
                     [K("kk"), K("Pexc")], [K("at")])
                tt("dve", T("bt")[:], T("bv")[:], T("Pinv")[:], ALU.mult, [K("bv"), K("Pinv")], [K("bt")])
                tt("dve", T("kt")[:], T("k2")[:], T("Pinv")[:], ALU.mult, [K("k2"), K("Pinv")], [K("kt")])
                tt("dve", T("rt")[:], r_, T("Pinc")[:], ALU.mult, [K("mx"), K("Pinc")], [K("rt")])
                tt("dve", T("bh")[:], T("bv")[:], T("Pdec")[:], ALU.mult, [K("bv"), K("Pdec")], [K("bh")])
                tt("dve", T("kh")[:], T("k2")[:], T("Pdec")[:], ALU.mult, [K("k2"), K("Pdec")], [K("kh")])
                tt("dve", T("rk")[:], r_, T("k2")[:], ALU.mult, [K("mx"), K("k2")], [K("rk")])
                tt("dve", T("rk")[:], T("rk")[:], c6(cst["rkp"][:]), ALU.mult, [K("rk"), "rkp"], [K("rk")])
                bb = rq.next()
                bfn = [(lambda e, h=h: e.matmul(rbank[bb][:, h:h + 1], T("rk")[:, h, :], ones64[:, 0:1], start=True, stop=True)) for h in range(6)]
                P.op("pe", bfn, reads=[K("rk"), "ones64"], writes=[f"rbank{bb}"])
                P.op("act", lambda e: e.copy(out=T("bon")[:], in_=rbank[bb][:, 0:6]), [f"rbank{bb}"], [K("bon")])
                idf = identf[0:64, 0:64]
                for src_nm, dst_nm, srcap, srckey in (("v", "Vt", v_, K("mx")), ("bh", "bhT", T("bh")[:], K("bh")), ("kh", "khT", T("kh")[:], K("kh"))):
                    tb = rq.next()
                    tv = rbank[tb][:, 0:384].rearrange("p (h t) -> p h t", h=6)
                    P.op("pe", [(lambda e, h=h, tv=tv, srcap=srcap: e.transpose(out=tv[:, h, :], in_=srcap[:, h, :], identity=idf)) for h in range(6)],
                         reads=[srckey, "identf"], writes=[f"rbank{tb}"])
                    P.op("act", lambda e, tv=tv, dst_nm=dst_nm: e.copy(out=T(dst_nm)[:], in_=tv), [f"rbank{tb}"], [K(dst_nm)])
                for nm, ln, rn, msk in (("MabT", "bt", "at", mskU), ("Nab", "at", "bt", mskL), ("MakT", "kt", "at", mskU),
                                        ("MrbT", "bt", "rt", mskUi), ("MrkT", "kt", "rt", mskUi)):
                    mb = rq.next()
                    ov = mm6(mb, [K(ln)], [K(rn)], (lambda h, ln=ln: T(ln)[:, h, :]), (lambda h, rn=rn: T(rn)[:, h, :]))
                    tt("dve", T(nm)[:], ov, b6(msk), ALU.mult, [f"rbank{mb}", "msk"], [K(nm)])
                tt("dve", T("X0")[:], T("MabT")[:], b6(idf), ALU.add, [K("MabT"), "identf"], [K("X0")])
                Mc, Nc, Xc = "MabT", "Nab", "X0"
                for j in range(1, 6):
                    Nn = "Na" if j % 2 else "Nb"
                    Mn = "Ma" if j % 2 else "Mb"
                    Xn = "X1" if j % 2 else "X0"
                    nb = rq.next()
                    ov = mm6(nb, [K(Mc)], [K(Nc)], (lambda h, Mc=Mc: T(Mc)[:, h, :]), (lambda h, Nc=Nc: T(Nc)[:, h, :]))
                    P.op("act", lambda e, ov=ov, Nn=Nn: e.copy(out=T(Nn)[:], in_=ov), [f"rbank{nb}"], [K(Nn)])
                    if j < 5:
                        mb = rq.next()
                        ov2 = mm6(mb, [K(Nc)], [K(Mc)], (lambda h, Nc=Nc: T(Nc)[:, h, :]), (lambda h, Mc=Mc: T(Mc)[:, h, :]))
                        P.op("dve", lambda e, ov2=ov2, Mn=Mn: e.tensor_copy(out=T(Mn)[:], in_=ov2), [f"rbank{mb}"], [K(Mn)])
                    xb = rq.next()
                    ov3 = mm6(xb, [K(Nn)], [K(Xc)], (lambda h, Nn=Nn: T(Nn)[:, h, :]), (lambda h, Xc=Xc: T(Xc)[:, h, :]))
                    tt("dve", T(Xn)[:], ov3, T(Xc)[:], ALU.add, [f"rbank{xb}", K(Xc)], [K(Xn)])
                    Mc, Nc, Xc = Mn, Nn, Xn
                XT = Xc
                Hc, Hn = Hs[i % 2], Hs[(i + 1) % 2]
                Hck, Hnk = f"H{i % 2}", f"H{(i + 1) % 2}"
                Hv = lambda Ht: (lambda h: Ht[:, h * 64:(h + 1) * 64])
                tt("dve", T("dgP")[:], b6(idf), c6(T("Pend")[:]), ALU.mult, ["identf", K("Pend")], [K("dgP")])
                wb = rq.next()
                ov = mmacc(wb, [((lambda h: T("at")[:, h, :]), Hv(Hc)), ((lambda h: T("MakT")[:, h, :]), (lambda h: T("Vt")[:, h, :]))],
                           reads=[K("at"), Hck, K("MakT"), K("Vt")])
                P.op("act", lambda e: e.copy(out=T("W")[:], in_=ov), [f"rbank{wb}"], [K("W")])
                ub = rq.next()
                ovu = mm6(ub, [K(XT)], [K("W")], (lambda h: T(XT)[:, h, :]), (lambda h: T("W")[:, h, :]))
                P.op("dve", lambda e: e.tensor_copy(out=T("U")[:], in_=ovu), [f"rbank{ub}"], [K("U")])
                hb_ = rq.next()
                ovh = mmacc(hb_, [((lambda h: T("dgP")[:, h, :]), Hv(Hc)), ((lambda h: T("bhT")[:, h, :]), (lambda h: T("U")[:, h, :])),
                                  ((lambda h: T("khT")[:, h, :]), (lambda h: T("Vt")[:, h, :]))],
                            reads=[K("dgP"), Hck, K("bhT"), K("U"), K("khT"), K("Vt")])
                P.op("act", lambda e: e.copy(out=Hn[:].rearrange("p (h t) -> p h t", h=6), in_=ovh), [f"rbank{hb_}"], [Hnk])
                yb_ = rq.next()
                ovy = mmacc(yb_, [((lambda h: T("rt")[:, h, :]), Hv(Hc)), ((lambda h: T("MrbT")[:, h, :]), (lambda h: T("U")[:, h, :])),
                                  ((lambda h: T("MrkT")[:, h, :]), (lambda h: T("Vt")[:, h, :]))],
                            reads=[K("rt"), Hck, K("MrbT"), K("U"), K("MrkT"), K("Vt")])
                if d == 1:
                    yst = T("yst")
                    P.op("act", lambda e: e.copy(out=yst[:, 0, :].rearrange("p (h t) -> p h t", h=6), in_=ovy), [f"rbank{yb_}"], [K("yst")])
                    tt("dve", yst[:, 1, :].rearrange("p (h t) -> p h t", h=6), T("Vt")[:], c6(T("bon")[:]), ALU.mult, [K("Vt"), K("bon")], [K("yst")])
                    yst2 = T("yld")
                    for c in range(2):
                        jb = rq.next()
                        P.op("pe", lambda e, c=c, jb=jb: e.matmul(rbank[jb][:, 0:384], cst["jrev"][:], yst[:, c, :], start=True, stop=True),
                             reads=[K("yst"), "jrev"], writes=[f"rbank{jb}"])
                        P.op("act" if c == 0 else "dve", (lambda e, c=c, jb=jb: e.copy(out=yst2[:, c, :], in_=rbank[jb][:, 0:384])) if c == 0 else
                             (lambda e, c=c, jb=jb: e.tensor_copy(out=yst2[:, c, :], in_=rbank[jb][:, 0:384])), [f"rbank{jb}"], [K("yld")])
                    P.dma("sp", lambda e: e.dma_start(out=yb_d[t0:t0 + 64], in_=yst2[:]), f"st_y{pb_}", reads=[K("yld")], writes=["yb_d"])
                else:
                    yld = T("yld")
                    P.dma("act", lambda e: e.dma_start(out=yld[:], in_=yb_d[t0:t0 + 64]), f"ld_y{pb_}", writes=[K("yld")])
                    y3 = lambda ap2: ap2.rearrange("p (h t) -> p h t", h=6)
                    tt("dve", T("yo")[:], ovy, y3(yld[:, 0, :]), ALU.add, [f"rbank{yb_}", K("yld")], [K("yo")])
                    tt("dve", T("ybl")[:], T("Vt")[:], c6(T("bon")[:]), ALU.mult, [K("Vt"), K("bon")], [K("ybl")])
                    tt("dve", T("ybl")[:], T("ybl")[:], y3(yld[:, 1, :]), ALU.add, [K("ybl"), K("yld")], [K("ybl")])
                    P.op("dve", lambda e: e.tensor_reduce(out=T("st")[:], in_=T("yo")[:], axis=AX.X, op=ALU.add), [K("yo")], [K("st")])
                    P.op("dve", lambda e: e.tensor_scalar(out=T("st")[:], in0=T("st")[:], scalar1=1.0 / 64, scalar2=None, op0=ALU.mult), [K("st")], [K("st")])
                    tt("dve", T("cen")[:], T("yo")[:], c6(T("st")[:]), ALU.subtract, [K("yo"), K("st")], [K("cen")])
                    tt("dve", T("sqc")[:], T("cen")[:], T("cen")[:], ALU.mult, [K("cen")], [K("sqc")])
                    P.op("dve", lambda e: e.tensor_reduce(out=T("st2")[:], in_=T("sqc")[:], axis=AX.X, op=ALU.add), [K("sqc")], [K("st2")])
                    act(T("st2")[:], T("st2")[:], AF.Sqrt, [K("st2"), "eps12"], [K("st2")], scale=1.0 / 64, bias=eps12[:])
                    P.op("dve", lambda e: e.reciprocal(out=T("st2")[:], in_=T("st2")[:]), [K("st2")], [K("st2")])
                    tt("dve", T("yn")[:], T("cen")[:], c6(T("st2")[:]), ALU.mult, [K("cen"), K("st2")], [K("yn")])
                    tt("dve", T("yn")[:], T("yn")[:], y3(cst["lng"][:]), ALU.mult, [K("yn"), "lng"], [K("yn")])
                    tt("dve", T("yn")[:], T("yn")[:], y3(cst["lnb"][:]), ALU.add, [K("yn"), "lnb"], [K("yn")])
                    tt("dve", T("yn")[:], T("yn")[:], T("ybl")[:], ALU.add, [K("yn"), K("ybl")], [K("yn")])
                    act(T("sgg")[:, 0:2, :], X[:, 20:22, 1:65], AF.Sigmoid, [K("X")], [K("sgg")])
                    gb = rq.next()
                    gfn = [(lambda e, c=c: e.matmul(rbank[gb][:, 0:384], T("sgg")[:, c, :], cst["g2"][:, c * 384:(c + 1) * 384], start=(c == 0), stop=(c == 1)))
                           for c in range(2)]
                    P.op("pe", gfn, reads=[K("sgg"), "g2"], writes=[f"rbank{gb}"])
                    yrwb = T("yrwb")
                    tt("dve", yrwb[:], T("yn")[:].rearrange("p h t -> p (h t)"), rbank[gb][:, 0:384], ALU.mult, [K("yn"), f"rbank{gb}"], [K("yrwb")])
                    P.op("pe", [(lambda e, c=c: e.transpose(out=rbt[:, c * 64:(c + 1) * 64], in_=yrwb[:, c * 128:(c + 1) * 128], identity=identb[0:64, 0:64]))
                                for c in range(3)], reads=[K("yrwb"), "identb"], writes=["rbt"])
                    ob = (i // 8) % 2
                    yT = tl[("yrwT", ob)]
                    P.op("act", lambda e: e.copy(out=yT[:, :, (i % 8) * 64:(i % 8) * 64 + 64], in_=rbt[:, 0:192].rearrange("p (c q) -> p c q", c=3)),
                         reads=["rbt"], writes=[f"yrwT{ob}"])
                    if i % 8 == 7:
                        tb0 = (i // 8) * 512
                        P.dma("sp", lambda e: e.dma_start(out=yrwT_d.rearrange("(c p) t -> p c t", p=128)[:, :, tb0:tb0 + 512], in_=yT[:]),
                              f"st_yrw{ob}", reads=[f"yrwT{ob}"], writes=["yrwT_d"])

            for d in (1, 0):
                P.op("dve", lambda e: e.memset(Hs[0][:], 0.0), writes=["H0"])
                for i in range(RW_CHUNKS):
                    rw_chunk(d, i)
            P.barrier()

        if PHASES >= 4:
          affall = sb("affall", [128, 64, 16], F32)
          with ExitStack() as pd:
            Wg = sb("Wg", [128, 8, 3072], BF16, pd)
            Wb = [sb("Wna", [128, 3, 1024], BF16, pd), sb("Wrw", [128, 3, 1024], BF16, pd), sb("Wmem", [128, 2, 1024], BF16, pd)]
            Wo = sb("Wo", [128, 8, 1024], BF16, pd)
            wr = sb("wr", [128, 8, 16], F32, pd)
            gffn = sb("gffn", [128, 1024], F32, pd)
            tailc = sb("tailc", [128, 64, 4], F32, pd)
            for c in range(8):
                P.dma("pool", lambda e, c=c: e.dma_start(out=Wg[:, c, :], in_=w_in.rearrange("(c p) n -> p c n", p=128)[:, c, 2816:5888]), f"Wg{c}", writes=[f"Wg{c}"])
            P.dma("pool", lambda e: e.dma_start(out=Wb[0][:], in_=wbna_d.rearrange("(c p) n -> p c n", p=128)), "Wb0", writes=["Wb0"])
            P.dma("pool", lambda e: e.dma_start(out=Wb[1][:], in_=wbrw_d.rearrange("(c p) n -> p c n", p=128)), "Wb1", writes=["Wb1"])
            P.dma("pool", lambda e: e.dma_start(out=Wb[2][:], in_=wbmem_d.rearrange("(c p) n -> p c n", p=128)), "Wb2", writes=["Wb2"])
            P.dma("pool", lambda e: e.dma_start(out=Wo[:], in_=wout_d.rearrange("(c p) n -> p c n", p=128)), "Wo", writes=["Wo"])
            P.dma("sp", lambda e: e.dma_start(out=wr[:], in_=wrt_d.rearrange("(c p) n -> p c n", p=128)), "wr", writes=["wr"])
            P.dma("sp", lambda e: e.dma_start(out=gffn[:], in_=gffn_d), "gffn", writes=["gffn"])
            P.dma("sp", lambda e: e.dma_start(out=tailc[:].rearrange("p t k -> p (t k)"), in_=tail_d), "tailc", writes=["tailc"])
            dhT = [sb(f"d_hTb{i}", [128, 8, 512], BF16, pd) for i in range(2)]
            yT = [[sb(f"d_y{b}T{i}", [128, 3 if b < 2 else 2, 512], BF16, pd) for i in range(2)] for b in range(3)]
            sg_t = [sb(f"d_sg{i}", [128, 512], F32, pd) for i in range(2)]
            mg = sb("d_mg", [128, 512], F32, pd)
            tmpm = sb("d_tmpm", [128, 512], F32, pd)
            mT = sb("d_mT", [128, 8, 512], BF16, pd)
            xt_ = [sb(f"d_xt{i}", [128, D], F32, pd) for i in range(2)]
            x1_ = [sb(f"d_x1{i}", [128, D], F32, pd) for i in range(2)]
            h2_ = [sb(f"d_h2{i}", [128, D], F32, pd) for i in range(2)]
            h2row = [sb(f"d_h2row{i}", [128, 1028], BF16, pd) for i in range(2)]
            h2T = sb("d_h2T", [128, 8, 128], F32, pd)
            sqj = sb("d_sqj", [128, D], BF16, pd)
            sst = [sb(f"d_ss{i}", [128, 1], F32, pd) for i in range(2)]
            rst = [sb(f"d_rs{i}", [128, 1], F32, pd) for i in range(2)]
            mxs = [sb(f"d_mx{i}", [128, 1], F32, pd) for i in range(2)]
            sms = [sb(f"d_sm{i}", [128, 1], F32, pd) for i in range(2)]
            lex = [sb(f"d_lex{i}", [128, 16], F32, pd) for i in range(2)]
            epsd = sb("d_eps", [128, 1], F32, pd)
            P.op("dve", lambda e: e.memset(epsd[:], 1e-6), writes=["d_eps"])
            dbank = [ps(f"dbank{i}", [128, 512], F32, pd) for i in range(8)]
            dq = RR(range(8))
            srcs = (ynaT_d, yrwT_d, ymemT_d)
            nck = (3, 3, 2)

            def d_tile(blk, j):
                ti = blk * 4 + j
                b2 = ti % 2
                P.dma("act", lambda e: e.dma_start(out=xt_[b2][:], in_=x_d[ti * 128:(ti + 1) * 128, :]), f"d_ldx{b2}", writes=[f"d_xt{b2}"])
                for half in range(2):
                    ob = dq.next()
                    mm(P, dbank[ob][:], [(mT[:, c, j * 128:(j + 1) * 128], Wo[:, c, half * 512:(half + 1) * 512]) for c in range(8)],
                       reads=["d_mT", "Wo"], writes=[f"dbank{ob}"])
                    P.op("dve", lambda e, ob=ob, half=half: e.tensor_tensor(out=x1_[b2][:, half * 512:(half + 1) * 512], in0=dbank[ob][:],
                                                                          in1=xt_[b2][:, half * 512:(half + 1) * 512], op=ALU.add),
                         reads=[f"dbank{ob}", f"d_xt{b2}"], writes=[f"d_x1{b2}"])
                P.dma("sp", lambda e: e.dma_start(out=acc_d[ti * 128:(ti + 1) * 128, :], in_=x1_[b2][:]), f"d_stx1{b2}", reads=[f"d_x1{b2}"], writes=["acc_d"])
                P.op("act", lambda e: e.activation(out=sqj[:], in_=x1_[b2][:], func=AF.Square, accum_out=sst[b2][:]),
                     reads=[f"d_x1{b2}"], writes=["d_sqj", f"d_ss{b2}"])
                P.op("act", lambda e: e.activation(out=rst[b2][:], in_=sst[b2][:], func=AF.Sqrt, scale=1.0 / D, bias=epsd[:]),
                     reads=[f"d_ss{b2}", "d_eps"], writes=[f"d_rs{b2}"])
                P.op("dve", lambda e: e.reciprocal(out=rst[b2][:], in_=rst[b2][:]), reads=[f"d_rs{b2}"], writes=[f"d_rs{b2}"])
                P.op("act", lambda e: e.activation(out=h2_[b2][:], in_=x1_[b2][:], func=AF.Copy, scale=rst[b2][:]),
                     reads=[f"d_x1{b2}", f"d_rs{b2}"], writes=[f"d_h2{b2}"])
                P.op("dve", lambda e: e.tensor_tensor(out=h2_[b2][:], in0=h2_[b2][:], in1=gffn[:], op=ALU.mult), reads=[f"d_h2{b2}", "gffn"], writes=[f"d_h2{b2}"])
                P.op("act", lambda e: e.copy(out=h2row[b2][:, 0:1024], in_=h2_[b2][:]), reads=[f"d_h2{b2}"], writes=[f"d_h2row{b2}"])
                P.op("dve", lambda e: e.tensor_copy(out=h2row[b2][:, 1024:1028], in_=tailc[:, ti, :]), reads=["tailc"], writes=[f"d_h2row{b2}"])
                P.dma("sp", lambda e: e.dma_start(out=h2_d[ti * 128:(ti + 1) * 128, :], in_=h2row[b2][:]), f"d_sth2{b2}", reads=[f"d_h2row{b2}"], writes=["h2_d"])
                for half in range(2):
                    tb = dq.next()
                    tv = dbank[tb][:].rearrange("p (c t) -> p c t", c=4)
                    P.op("pe", [(lambda e, c=c, tv=tv, half=half: e.transpose(out=tv[:, c, :], in_=h2_[b2][:, (half * 4 + c) * 128:(half * 4 + c + 1) * 128], identity=identf[:]))
                                for c in range(4)], reads=[f"d_h2{b2}", "identf"], writes=[f"dbank{tb}"])
                    if half == 0:
                        P.op("act", lambda e, tv=tv: e.copy(out=h2T[:, 0:4, :], in_=tv), [f"dbank{tb}"], ["d_h2T"])
                    else:
                        P.op("dve", lambda e, tv=tv: e.tensor_copy(out=h2T[:, 4:8, :], in_=tv), [f"dbank{tb}"], ["d_h2T"])
                lb = dq.next()
                mm(P, dbank[lb][:, 0:16], [(h2T[:, c, :], wr[:, c, :]) for c in range(8)], reads=["d_h2T", "wr"], writes=[f"dbank{lb}"])
                P.op("dve", lambda e: e.tensor_reduce(out=mxs[b2][:], in_=dbank[lb][:, 0:16], axis=AX.X, op=ALU.max), [f"dbank{lb}"], [f"d_mx{b2}"])
                P.op("dve", lambda e: e.tensor_scalar(out=mxs[b2][:], in0=mxs[b2][:], scalar1=-1.0, scalar2=None, op0=ALU.mult), [f"d_mx{b2}"], [f"d_mx{b2}"])
                P.op("act", lambda e: e.activation(out=lex[b2][:], in_=dbank[lb][:, 0:16], func=AF.Exp, bias=mxs[b2][:], accum_out=sms[b2][:]),
                     [f"dbank{lb}", f"d_mx{b2}"], [f"d_lex{b2}", f"d_sm{b2}"])
                P.op("dve", lambda e: e.reciprocal(out=sms[b2][:], in_=sms[b2][:]), [f"d_sm{b2}"], [f"d_sm{b2}"])
                P.op("dve", lambda e: e.tensor_scalar(out=affall[:, ti, :], in0=lex[b2][:], scalar1=sms[b2][:], scalar2=None, op0=ALU.mult),
                     [f"d_lex{b2}", f"d_sm{b2}"], ["affall"])

            def d_chunk(blk, hb, dmc):
                for b in range(3):
                    gb = dq.next()
                    c0 = b * 1024 + dmc * 128
                    mm(P, dbank[gb][:], [(Wg[:, c, c0:c0 + 128], dhT[hb][:, c, :]) for c in range(8)], reads=["Wg", f"d_hTb{hb}"], writes=[f"dbank{gb}"])
                    s2 = b % 2
                    P.op("act", lambda e, gb=gb, s2=s2: e.activation(out=sg_t[s2][:], in_=dbank[gb][:], func=AF.Sigmoid), [f"dbank{gb}"], [f"d_sg{s2}"])
                    bb = dq.next()
                    mm(P, dbank[bb][:], [(Wb[b][:, c, dmc * 128:(dmc + 1) * 128], yT[b][hb][:, c, :]) for c in range(nck[b])],
                       reads=[f"Wb{b}", f"d_y{b}T{hb}"], writes=[f"dbank{bb}"])
                    if b == 0:
                        P.op("dve", lambda e, bb=bb, s2=s2: e.tensor_tensor(out=mg[:], in0=dbank[bb][:], in1=sg_t[s2][:], op=ALU.mult),
                             [f"dbank{bb}", f"d_sg{s2}"], ["d_mg"])
                    else:
                        P.op("dve", lambda e, bb=bb, s2=s2: e.tensor_tensor(out=tmpm[:], in0=dbank[bb][:], in1=sg_t[s2][:], op=ALU.mult),
                             [f"dbank{bb}", f"d_sg{s2}"], ["d_tmpm"])
                        if b == 1:
                            P.op("dve", lambda e: e.tensor_tensor(out=mg[:], in0=mg[:], in1=tmpm[:], op=ALU.add), ["d_mg", "d_tmpm"], ["d_mg"])
                        else:
                            P.op("dve", lambda e: e.tensor_tensor(out=mT[:, dmc, :], in0=mg[:], in1=tmpm[:], op=ALU.add), ["d_mg", "d_tmpm"], ["d_mT"])

            def d_block(blk):
                hb = blk % 2
                t0 = blk * 512
                P.dma("sp", lambda e: e.dma_start(out=dhT[hb][:], in_=hT_d.rearrange("(c p) t -> p c t", p=128)[:, :, t0:t0 + 512]), f"d_ldh{hb}", writes=[f"d_hTb{hb}"])
                for b in range(3):
                    P.dma("act", lambda e, b=b: e.dma_start(out=yT[b][hb][:], in_=srcs[b].rearrange("(c p) t -> p c t", p=128)[:, :, t0:t0 + 512]),
                          f"d_ldy{b}{hb}", writes=[f"d_y{b}T{hb}"])
                for dmc in range(8):
                    d_chunk(blk, hb, dmc)
                    if D_BARRIER:
                        P.barrier()
                for j in range(4):
                    d_tile(blk, j)
                    if D_BARRIER:
                        P.barrier()
                    if DBG_D and blk == 0 and j == 0:
                        P.barrier()
                        dv = lambda r0, r1: out_d[r0:r1, :].rearrange("(p a) n -> p (a n)", p=128)
                        P.dma("sp", lambda e: e.dma_start(out=dv(0, 256).bitcast(BF16), in_=dhT[0][:].rearrange("p c t -> p (c t)")), "dbgd", writes=["out"])
                        P.dma("sp", lambda e: e.dma_start(out=dv(256, 512).bitcast(BF16), in_=mT[:].rearrange("p c t -> p (c t)")), "dbgd", writes=["out"])
                        P.dma("sp", lambda e: e.dma_start(out=out_d[512:640, 0:768].bitcast(BF16), in_=yT[1][0][:].rearrange("p c t -> p (c t)")), "dbgd", writes=["out"])
                        P.dma("sp", lambda e: e.dma_start(out=out_d[640:768, :], in_=x1_[0][:]), "dbgd", writes=["out"])
                        P.dma("sp", lambda e: e.dma_start(out=out_d[768:896, 0:512], in_=sg_t[0][:]), "dbgd", writes=["out"])
                        P.dma("sp", lambda e: e.dma_start(out=out_d[768:896, 512:1024], in_=mg[:]), "dbgd", writes=["out"])
                        P.dma("sp", lambda e: e.dma_start(out=out_d[896:1024, :], in_=xt_[0][:]), "dbgd", writes=["out"])
                        P.barrier()

            for blk in range(S // 512):
                d_block(blk)
            P.dma("sp", lambda e: e.dma_start(out=aff_d.rearrange("(t p) e -> p t e", p=128), in_=affall[:]), "st_aff", reads=["affall"], writes=["aff_d"])
            P.barrier()

        if PHASES >= 5:
          with ExitStack() as pe_:
            ones128 = sb("e_ones", [128, 128], F32, pe_)
            ltm = sb("e_ltm", [128, 128], F32, pe_)
            lo = sb("e_lo", [128, 16], F32, pe_)
            hi = sb("e_hi", [128, 16], F32, pe_)
            mid = sb("e_mid", [128, 16], F32, pe_)
            cnt = sb("e_cnt", [128, 16], F32, pe_)
            ge = sb("e_ge", [128, 16], F32, pe_)
            dlt = sb("e_dlt", [128, 16], F32, pe_)
            cmpt = sb("e_cmp", [128, 64, 16], F32, pe_)
            sel = sb("e_sel", [128, 64, 16], F32, pe_)
            cntb = sb("e_cntb", [128, 64, 16], F32, pe_)
            cum = sb("e_cum", [128, 16, 64], F32, pe_)
            rank = sb("e_rank", [128, 64, 16], F32, pe_)
            ranki = sb("e_ranki", [128, 1024], I32, pe_)
            onesr = sb("e_onesr", [128, 64], F32, pe_)
            ebank = [ps(f"ebank{i}", [128, 512], F32, pe_) for i in range(4)]
            P.op("dve", lambda e: e.memset(ones128[:], 1.0), writes=["e_ones"])
            P.op("dve", lambda e: e.memset(onesr[:], 1.0), writes=["e_onesr"])
            P.dma("sp", lambda e: e.dma_start(out=ltm[:], in_=ltm_d), "e_ltm", writes=["e_ltm"])
            P.op("dve", lambda e: e.memset(lo[:], 0.0), writes=["e_lo"])
            P.op("dve", lambda e: e.memset(hi[:], 2.0), writes=["e_hi"])

            def bis_iter(it):
                P.op("dve", lambda e: e.tensor_tensor(out=mid[:], in0=lo[:], in1=hi[:], op=ALU.add), ["e_lo", "e_hi"], ["e_mid"])
                P.op("dve", lambda e: e.tensor_scalar(out=mid[:], in0=mid[:], scalar1=0.5, scalar2=None, op0=ALU.mult), ["e_mid"], ["e_mid"])
                P.op("dve", lambda e: e.tensor_tensor(out=cmpt[:], in0=affall[:], in1=mid[:].unsqueeze(1).to_broadcast([128, 64, 16]), op=ALU.is_ge),
                     ["affall", "e_mid"], ["e_cmp"])
                P.op("dve", lambda e: e.tensor_reduce(out=cnt[:], in_=cmpt[:].rearrange("p t e -> p e t"), axis=AX.X, op=ALU.add), ["e_cmp"], ["e_cnt"])
                b = it % 2
                mm(P, ebank[b][:, 0:16], [(ones128[:], cnt[:])], reads=["e_ones", "e_cnt"], writes=[f"ebank{b}"])
                P.op("dve", lambda e: e.tensor_scalar(out=ge[:], in0=ebank[b][:, 0:16], scalar1=1023.5, scalar2=None, op0=ALU.is_ge), [f"ebank{b}"], ["e_ge"])
                P.op("dve", lambda e: e.tensor_tensor(out=dlt[:], in0=mid[:], in1=lo[:], op=ALU.subtract), ["e_mid", "e_lo"], ["e_dlt"])
                P.op("dve", lambda e: e.tensor_tensor(out=dlt[:], in0=dlt[:], in1=ge[:], op=ALU.mult), ["e_dlt", "e_ge"], ["e_dlt"])
                P.op("dve", lambda e: e.tensor_tensor(out=lo[:], in0=lo[:], in1=dlt[:], op=ALU.add), ["e_lo", "e_dlt"], ["e_lo"])
                P.op("dve", lambda e: e.tensor_tensor(out=dlt[:], in0=hi[:], in1=mid[:], op=ALU.subtract), ["e_hi", "e_mid"], ["e_dlt"])
                P.op("dve", lambda e: e.tensor_tensor(out=dlt[:], in0=dlt[:], in1=ge[:], op=ALU.mult), ["e_dlt", "e_ge"], ["e_dlt"])
                P.op("dve", lambda e: e.tensor_tensor(out=hi[:], in0=mid[:], in1=dlt[:], op=ALU.add), ["e_mid", "e_dlt"], ["e_hi"])

            for it in range(36):
                bis_iter(it)
            P.op("dve", lambda e: e.tensor_tensor(out=sel[:], in0=affall[:], in1=lo[:].unsqueeze(1).to_broadcast([128, 64, 16]), op=ALU.is_ge),
                 ["affall", "e_lo"], ["e_sel"])
            self2 = sel[:].rearrange("p t e -> p (t e)")
            for half in range(2):
                mm(P, ebank[half][:], [(ones128[:], self2[:, half * 512:(half + 1) * 512])], reads=["e_ones", "e_sel"], writes=[f"ebank{half}"])
                P.op("act", lambda e, half=half: e.copy(out=cntb[:].rearrange("p t e -> p (t e)")[:, half * 512:(half + 1) * 512], in_=ebank[half][:]),
                     [f"ebank{half}"], ["e_cntb"])
            for ex in range(16):
                P.op("dve", lambda e, ex=ex: e.tensor_tensor_scan(out=cum[:, ex, :], data0=onesr[:], data1=cntb[:, :, ex], initial=0.0, op0=ALU.mult, op1=ALU.add),
                     ["e_cntb", "e_onesr"], ["e_cum"])
            P.op("dve", lambda e: e.tensor_tensor(out=rank[:], in0=cum[:].rearrange("p e t -> p t e"), in1=cntb[:], op=ALU.subtract), ["e_cum", "e_cntb"], ["e_rank"])
            for half in range(2):
                mm(P, ebank[2 + half][:], [(ltm[:], self2[:, half * 512:(half + 1) * 512])], reads=["e_ltm", "e_sel"], writes=[f"ebank{2 + half}"])
                P.op("dve", lambda e, half=half: e.tensor_tensor(out=rank[:].rearrange("p t e -> p (t e)")[:, half * 512:(half + 1) * 512],
                                                                 in0=rank[:].rearrange("p t e -> p (t e)")[:, half * 512:(half + 1) * 512], in1=ebank[2 + half][:], op=ALU.add),
                     [f"ebank{2 + half}", "e_rank"], ["e_rank"])
            P.op("dve", lambda e: e.tensor_scalar(out=rank[:], in0=rank[:], scalar1=-60000.0, scalar2=None, op0=ALU.add), ["e_rank"], ["e_rank"])
            P.op("dve", lambda e: e.tensor_tensor(out=rank[:], in0=rank[:], in1=sel[:], op=ALU.mult), ["e_rank", "e_sel"], ["e_rank"])
            P.op("dve", lambda e: e.tensor_scalar(out=rank[:], in0=rank[:], scalar1=60000.0, scalar2=None, op0=ALU.add), ["e_rank"], ["e_rank"])
            P.op("dve", lambda e: e.tensor_copy(out=ranki[:], in_=rank[:].rearrange("p t e -> p (t e)")), ["e_rank"], ["e_ranki"])
            if DEBUG:
                P.dma("sp", lambda e: e.dma_start(out=rank_dbg, in_=rank[:].rearrange("p t e -> p (t e)")), "dbg_rank", reads=["e_rank"], writes=["rank_dbg"])
            h2r = [sb(f"e_h2r{i}", [128, 1028], BF16, pe_) for i in range(6)]

            def disp_tile(ti):
                b3 = ti % 6
                P.dma("sp", lambda e: e.dma_start(out=h2r[b3][:], in_=h2_d[ti * 128:(ti + 1) * 128, :]), f"e_ld{b3}", writes=[f"e_h2r{b3}"])
                for ex in range(16):
                    P.dma("pool", lambda e, ex=ex: e.indirect_dma_start(out=xe_d[ex], out_offset=bass.IndirectOffsetOnAxis(ap=ranki[:, ti * 16 + ex:ti * 16 + ex + 1], axis=0),
                                                                        in_=h2r[b3][:], in_offset=None, bounds_check=breg(e, 1023), oob_is_err=False),
                          f"e_sc{b3}", reads=[f"e_h2r{b3}", "e_ranki"], writes=())

            for ti in range(64):
                disp_tile(ti)
            P.barrier()

        if PHASES >= 6:
          with ExitStack() as pf:
            xe = [sb(f"f_xe{i}", [128, 1028], BF16, pf) for i in range(2)]
            xeT = sb("f_xeT", [128, 8, 1024], BF16, pf)
            tailf = sb("f_tailf", [128, 8, 2], F32, pf)
            idf_ = sb("f_idf", [128, 8], F32, pf)
            idi = sb("f_idi", [128, 8], I32, pf)
            affg = sb("f_affg", [128, 8, 16], F32, pf)
            wgt = [sb(f"f_wg{i}", [128, 8, 512], BF16, pf) for i in range(3)]
            wut = [sb(f"f_wu{i}", [128, 8, 512], BF16, pf) for i in range(3)]
            wdt = [sb(f"f_wd{i}", [128, 4, 1024], BF16, pf) for i in range(3)]
            actT = sb("f_actT", [128, 4, 1024], BF16, pf)
            sil = [sb(f"f_sil{i}", [128, 512], F32, pf) for i in range(2)]
            Y = sb("f_Y", [128, 8, 1024], F32, pf)
            fbank = [ps(f"fbank{i}", [128, 512], F32, pf) for i in range(6)]
            fbt = [ps(f"fbt{i}", [128, 1024], BF16, pf) for i in range(2)]
            fq = RR(range(6))
            wctr = [0]

            def load_w(ex, fg):
                wb = wctr[0] % 3
                wctr[0] += 1
                f0 = fg * 512
                P.dma("pool", lambda e: e.dma_start(out=wgt[wb][:], in_=wgate_d[ex].rearrange("(c p) f -> p c f", p=128)[:, :, f0:f0 + 512]), f"f_ldg{wb}", writes=[f"f_wg{wb}"])
                P.dma("pool", lambda e: e.dma_start(out=wut[wb][:], in_=wup_d[ex].rearrange("(c p) f -> p c f", p=128)[:, :, f0:f0 + 512]), f"f_ldu{wb}", writes=[f"f_wu{wb}"])
                P.dma("pool", lambda e: e.dma_start(out=wdt[wb][:], in_=wdown_d[ex, f0:f0 + 512, :].rearrange("(c p) n -> p c n", p=128)), f"f_ldd{wb}", writes=[f"f_wd{wb}"])
                return wb

            def xe_tile(ex, st):
                b2 = st % 2
                P.dma("sp", lambda e: e.dma_start(out=xe[b2][:], in_=xe_d[ex][st * 128:(st + 1) * 128, :]), f"f_ldxe{b2}", writes=[f"f_xe{b2}"])
                P.op("pe", [(lambda e, c=c: e.transpose(out=fbt[b2][:, c * 128:(c + 1) * 128], in_=xe[b2][:, c * 128:(c + 1) * 128], identity=identb[:])) for c in range(8)],
                     reads=[f"f_xe{b2}", "identb"], writes=[f"fbt{b2}"])
                ev = "act" if b2 == 0 else "dve"
                if ev == "act":
                    P.op("act", lambda e: e.copy(out=xeT[:, :, st * 128:(st + 1) * 128], in_=fbt[b2][:].rearrange("p (c t) -> p c t", c=8)), [f"fbt{b2}"], ["f_xeT"])
                else:
                    P.op("dve", lambda e: e.tensor_copy(out=xeT[:, :, st * 128:(st + 1) * 128], in_=fbt[b2][:].rearrange("p (c t) -> p c t", c=8)), [f"fbt{b2}"], ["f_xeT"])
                P.op("dve", lambda e: e.tensor_copy(out=tailf[:, st, :], in_=xe[b2][:, 1024:1026]), [f"f_xe{b2}"], ["f_tailf"])

            def gu(ex, wb, fc, sh):
                gb, ub = fq.next(), fq.next()
                mm(P, fbank[gb][:], [(wgt[wb][:, c, fc * 128:(fc + 1) * 128], xeT[:, c, sh * 512:(sh + 1) * 512]) for c in range(8)],
                   reads=[f"f_wg{wb}", "f_xeT"], writes=[f"fbank{gb}"])
                mm(P, fbank[ub][:], [(wut[wb][:, c, fc * 128:(fc + 1) * 128], xeT[:, c, sh * 512:(sh + 1) * 512]) for c in range(8)],
                   reads=[f"f_wu{wb}", "f_xeT"], writes=[f"fbank{ub}"])
                s2 = (fc * 2 + sh) % 2
                P.op("act", lambda e: e.activation(out=sil[s2][:], in_=fbank[gb][:], func=AF.Silu), [f"fbank{gb}"], [f"f_sil{s2}"])
                P.op("dve", lambda e: e.tensor_tensor(out=actT[:, fc, sh * 512:(sh + 1) * 512], in0=fbank[ub][:], in1=sil[s2][:], op=ALU.mult),
                     [f"fbank{ub}", f"f_sil{s2}"], ["f_actT"])

            def down(ex, wb, fg, st, dh):
                yb = fq.next()
                mm(P, fbank[yb][:], [(actT[:, fc, st * 128:(st + 1) * 128], wdt[wb][:, fc, dh * 512:(dh + 1) * 512]) for fc in range(4)],
                   reads=["f_actT", f"f_wd{wb}"], writes=[f"fbank{yb}"])
                dst = Y[:, st, dh * 512:(dh + 1) * 512]
                if fg == 0:
                    P.op("act", lambda e: e.copy(out=dst, in_=fbank[yb][:]), [f"fbank{yb}"], [f"f_Y{st}"])
                else:
                    P.op("dve", lambda e: e.tensor_tensor(out=dst, in0=fbank[yb][:], in1=dst, op=ALU.add), [f"fbank{yb}", f"f_Y{st}"], [f"f_Y{st}"])

            def expert(ex):
                for st in range(8):
                    xe_tile(ex, st)
                P.op("dve", lambda e: e.scalar_tensor_tensor(out=idf_[:], in0=tailf[:, :, 1], scalar=128.0, in1=tailf[:, :, 0], op0=ALU.mult, op1=ALU.add),
                     ["f_tailf"], ["f_idf"])
                P.op("dve", lambda e: e.tensor_copy(out=idi[:], in_=idf_[:]), ["f_idf"], ["f_idi"])
                for st in range(8):
                    P.dma("pool", lambda e, st=st: e.indirect_dma_start(out=affg[:, st, :], out_offset=None, in_=aff_d,
                                                                        in_offset=bass.IndirectOffsetOnAxis(ap=idi[:, st:st + 1], axis=0)),
                          "f_affg", reads=["f_idi"], writes=["f_affg"])
                for fg in range(4):
                    wb = load_w(ex, fg)
                    for fc in range(4):
                        for sh in range(2):
                            gu(ex, wb, fc, sh)
                    for st in range(8):
                        for dh in range(2):
                            down(ex, wb, fg, st, dh)
                for st in range(8):
                    P.op("dve", lambda e, st=st: e.tensor_scalar(out=Y[:, st, :], in0=Y[:, st, :], scalar1=affg[:, st, ex:ex + 1], scalar2=None, op0=ALU.mult),
                         [f"f_Y{st}", "f_affg"], [f"f_Y{st}"])
                    P.dma("pool", lambda e, st=st: e.indirect_dma_start(out=acc_d, out_offset=bass.IndirectOffsetOnAxis(ap=idi[:, st:st + 1], axis=0),
                                                                        in_=Y[:, st, :], in_offset=None, bounds_check=breg(e, S - 1), oob_is_err=False, compute_op=ALU.add),
                          "f_sca", reads=[f"f_Y{st}", "f_idi"], writes=["acc_d"])

            for ex in range(N_EXP):
                expert(ex)
            P.barrier()

        if PHASES >= 7:
          with ExitStack() as pg:
            gfin = sb("g_gfin", [128, D], F32, pg)
            P.dma("sp", lambda e: e.dma_start(out=gfin[:], in_=gfin_d), "g_gfin", writes=["g_gfin"])
            at_ = [sb(f"g_a{i}", [128, D], F32, pg) for i in range(3)]
            ot_ = [sb(f"g_o{i}", [128, D], F32, pg) for i in range(2)]
            gsq = sb("g_sq", [128, D], BF16, pg)
            gss = [sb(f"g_ss{i}", [128, 1], F32, pg) for i in range(2)]
            geps = sb("g_eps", [128, 1], F32, pg)
            P.op("dve", lambda e: e.memset(geps[:], 1e-6), writes=["g_eps"])

            def fin_tile(ti):
                b3, b2 = ti % 3, ti % 2
                P.dma("sp" if ti % 2 else "act", lambda e: e.dma_start(out=at_[b3][:], in_=acc_d[ti * 128:(ti + 1) * 128, :]), f"g_ld{b3}", writes=[f"g_a{b3}"])
                P.op("act", lambda e: e.activation(out=gsq[:], in_=at_[b3][:], func=AF.Square, accum_out=gss[b2][:]), [f"g_a{b3}"], ["g_sq", f"g_ss{b2}"])
                P.op("act", lambda e: e.activation(out=gss[b2][:], in_=gss[b2][:], func=AF.Sqrt, scale=1.0 / D, bias=geps[:]), [f"g_ss{b2}", "g_eps"], [f"g_ss{b2}"])
                P.op("dve", lambda e: e.reciprocal(out=gss[b2][:], in_=gss[b2][:]), [f"g_ss{b2}"], [f"g_ss{b2}"])
                P.op("dve", lambda e: e.scalar_tensor_tensor(out=ot_[b2][:], in0=at_[b3][:], scalar=gss[b2][:], in1=gfin[:], op0=ALU.mult, op1=ALU.mult),
                     [f"g_a{b3}", f"g_ss{b2}", "g_gfin"], [f"g_o{b2}"])
                if RAW_OUT:
                    P.dma("sp", lambda e: e.dma_start(out=out_d[ti * 128:(ti + 1) * 128, :], in_=at_[b3][:]), f"g_st{b2}", reads=[f"g_a{b3}"], writes=["out"])
                else:
                    P.dma("sp", lambda e: e.dma_start(out=out_d[ti * 128:(ti + 1) * 128, :], in_=ot_[b2][:]), f"g_st{b2}", reads=[f"g_o{b2}"], writes=["out"])

            for ti in range(8 if DBG_D else 0, 64):
                fin_tile(ti)
            P.barrier()

        if PHASES <= 6:
            tmp = sb("tmpo", [128, D], F32)
            P.op("dve", lambda e: e.memset(tmp[:], 0.0), writes=["tmpo"])
            P.dma("sp", lambda e: e.dma_start(out=out_d[0:128, :], in_=tmp[:]), "o", reads=["tmpo"], writes=["out"])
            P.barrier()
        P.emit()
    return nc, outs


def _bf16_eye():
    import ml_dtypes
    return np.eye(128, dtype=np.float32).astype(ml_dtypes.bfloat16)


def make_inputs(inp, b):
    f = lambda a: np.ascontiguousarray(a, dtype=np.float32)
    m = {
        "x": f(inp["x"][b]),
        "mem": f(inp["mem"][b]),
        "w_in": f(inp["w_in"][0]),
        "gmixT": f(inp["norm_mix_g"][0].reshape(8, 128).T),
        "gmemT": f(inp["norm_mem_g"][0].reshape(8, 128).T),
        "w_mem_kv": f(inp["w_mem_kv"][0]),
        "identb_in": _bf16_eye(),
        "identf_in": np.eye(128, dtype=np.float32),
    }
    rpb = f(inp["na_rpb"][0])
    p = np.arange(128)
    q = np.arange(64)
    coff = (p[:, None] % 64) - q[None, :] + 15
    valid = (coff >= 0) & (coff < 31)
    coffc = np.clip(coff, 0, 30)
    aa = np.arange(14)
    ro = (2 * (aa % 7) + aa // 7)[None, :, None] + (p[:, None, None] // 64)
    bias = rpb[:, ro, coffc[:, None, :]]
    bias = np.where(valid[None, :, None, :], bias, 0.0).transpose(1, 0, 2, 3)
    m["biasP_in"] = f(bias.reshape(128, 6 * 14 * 64))
    hp_ = lambda a: f(a.reshape(6, 64).T)
    mu = np.zeros((64, 2, 20), np.float32)
    for d in range(2):
        for j in range(3):
            mu[:, d, j * 6:(j + 1) * 6] = inp["rw_mu_rkv"][0, d, j].reshape(6, 64).T
        mu[:, d, 18] = inp["rw_mu_w"][0, d]
        mu[:, d, 19] = inp["rw_mu_a"][0, d]
    m["mu_in"] = f(mu.reshape(64, 40))
    w2a = np.zeros((65, 2, 384), np.float32)
    a2a = np.zeros((65, 2, 384), np.float32)
    for d in range(2):
        w2a[0:64, d] = inp["rw_w2"][0, d]
        w2a[64, d] = inp["rw_w0"][0, d]
        a2a[0:64, d] = inp["rw_a2"][0, d]
        a2a[64, d] = inp["rw_a0"][0, d]
    m["w2a_in"] = f(w2a.reshape(65, 768))
    m["a2a_in"] = f(a2a.reshape(65, 768))
    m["kkp_in"] = hp_(inp["rw_k_k"][0])
    m["kap_in"] = hp_(inp["rw_k_a"][0])
    m["rkp_in"] = hp_(inp["rw_r_k"][0].reshape(384))
    m["g2_in"] = f(inp["rw_g2"][0].reshape(2, 64, 384).transpose(1, 0, 2).reshape(64, 768))
    m["lng_in"] = f(np.broadcast_to(inp["rw_ln_g"][0][None, :], (64, 384)))
    m["lnb_in"] = f(np.broadcast_to(inp["rw_ln_b"][0][None, :], (64, 384)))
    ii = np.arange(64)
    mU = (ii[:, None] < ii[None, :]).astype(np.float32)
    mUi = (ii[:, None] <= ii[None, :]).astype(np.float32)
    mL = (ii[:, None] > ii[None, :]).astype(np.float32)
    m["msk_in"] = f(np.concatenate([mU, mUi, mL], axis=1))
    m["jrev_in"] = f(np.eye(64)[::-1])
    m["w_branch_na"] = f(inp["w_branch_na"][0])
    m["w_branch_rw"] = f(inp["w_branch_rw"][0])
    m["w_branch_mem"] = f(inp["w_branch_mem"][0])
    m["w_out"] = f(inp["w_out"][0])
    m["w_router"] = f(inp["w_router"][0])
    m["gffn_in"] = f(np.broadcast_to(inp["norm_ffn_g"][0][None, :], (128, D)))
    tail = np.zeros((128, 64, 4), np.float32)
    tail[:, :, 0] = np.arange(128)[:, None]
    tail[:, :, 1] = np.arange(64)[None, :]
    m["tail_in"] = f(tail.reshape(128, 256))
    pp = np.arange(128)
    m["ltm_in"] = f((pp[:, None] < pp[None, :]).astype(np.float32))
    m["gfin_in"] = f(np.broadcast_to(inp["norm_final_g"][None, :], (128, D)))
    m["w_exp_gate"] = f(inp["w_exp_gate"][0])
    m["w_exp_up"] = f(inp["w_exp_up"][0])
    m["w_exp_down"] = f(inp["w_exp_down"][0])
    cs = np.clip(q - 8, 0, 48)
    kc = p % 64
    m["maskP_in"] = f(((kc[:, None] >= cs[None, :]) & (kc[:, None] < cs[None, :] + 16)).astype(np.float32))
    return m


def kernel(**inputs):
    nc, outs = build()
    in_maps = [make_inputs(inputs, c % 4) for c in range(8)]
    res = run_bass_kernel_spmd(nc, in_maps, core_ids=list(range(8)))
    if DEBUG:
        kernel.last = res
    out = np.stack([np.asarray(res.results[b]["out"]) for b in range(4)], 0)
    return out.astype(np.float32)
```

```python
import numpy as np
from contextlib import ExitStack
import concourse.bass as bass
import concourse.mybir as mybir
from concourse.bass_utils import run_bass_kernel_spmd

F32 = mybir.dt.float32
BF16 = mybir.dt.bfloat16
I32 = mybir.dt.int32
AF = mybir.ActivationFunctionType
ALU = mybir.AluOpType
AX = mybir.AxisListType

S = 8192
D = 1024
NT = S // 128
DEBUG = False
PHASES = 99
SEM_ROT = 20000
NA_BARRIER = False
RW_CHUNKS = 128
N_EXP = 16
RAW_OUT = False
D_BARRIER = False
DBG_D = False
ALL_EXT = False
DBG_NAMES = ()


class Prog:
    ENG = ("pe", "dve", "act", "pool", "sp")

    def __init__(self, nc, es):
        self.nc = nc
        self.es = es
        self.streams = {e: [] for e in self.ENG}
        self.cur = {}
        self.res = {}
        self.seen = {e: {} for e in self.ENG}
        self.dmasem = {}
        self.freed = []
        self.retired = []
        self.nsem = 0
        for e in self.ENG:
            self._newsem(e)

    def _mksem(self, name):
        self.nsem += 1
        return self.es.enter_context(self.nc.semaphore(f"{name}_{self.nsem}"))

    def _newsem(self, e):
        self.cur[e] = [self._mksem("s" + e), 0]

    def _need(self, eng, waits, sv):
        if sv is None:
            return
        sem, val = sv
        k = id(sem)
        if self.seen[eng].get(k, (None, 0))[1] >= val:
            return
        if k not in waits or waits[k][1] < val:
            waits[k] = (sem, val)

    def _deps(self, eng, reads, writes):
        waits = {}
        for key in reads:
            r = self.res.get(key)
            if r is not None:
                self._need(eng, waits, r[0])
        for key in writes:
            r = self.res.get(key)
            if r is not None:
                self._need(eng, waits, r[0])
                for sv in r[1].values():
                    self._need(eng, waits, sv)
        for k, sv in waits.items():
            self.seen[eng][k] = sv
        return list(waits.values())

    def _mark(self, tag, sv, reads, writes):
        for key in reads:
            r = self.res.setdefault(key, [None, {}])
            r[1][tag] = sv
        for key in writes:
            self.res[key] = [sv, {}]

    def op(self, eng, fns, reads=(), writes=()):
        if callable(fns):
            fns = [fns]
        waits = self._deps(eng, reads, writes)
        c = self.cur[eng]
        if c[1] >= SEM_ROT:
            self._newsem(eng)
            c = self.cur[eng]
        c[1] += 1
        sv = (c[0], c[1])
        self.streams[eng].append((waits, fns, (c[0], 1)))
        self._mark(eng, sv, reads, writes)
        return sv

    def dma(self, q, fn, key, reads=(), writes=()):
        waits = self._deps(q, reads, writes)
        d = self.dmasem.get(key)
        if d is None or d[1] >= 30000:
            if d is not None:
                self.retired.append(d)
            d = self.dmasem[key] = self._getdsem()
        d[1] += 16
        sv = (d[0], d[1])
        self.streams[q].append((waits, [fn], (d[0], 16)))
        self._mark(("dma", key), sv, reads, writes)
        return sv

    def _getdsem(self):
        while self.freed:
            d = self.freed.pop()
            if d[1] < 20000:
                return d
        return [self._mksem("d"), 0]

    def barrier(self):
        targets = [(d[0], d[1]) for d in self.retired]
        self.retired = []
        for e in self.ENG:
            c = self.cur[e]
            if c[1] > 0:
                targets.append((c[0], c[1]))
        for d in self.dmasem.values():
            targets.append((d[0], d[1]))
        for e in self.ENG:
            ws = [t for t in targets if self.seen[e].get(id(t[0]), (None, 0))[1] < t[1]]
            for t in ws:
                self.seen[e][id(t[0])] = t
            self.streams[e].append((ws, [], None))
        self.res = {}
        self.freed.extend(self.dmasem.values())
        self.dmasem = {}

    def emit(self):
        engmap = {"pe": "tensor", "dve": "vector", "act": "scalar", "pool": "gpsimd", "sp": "sync"}
        with self.nc.Block() as block:
            for e in self.ENG:
                stream = self.streams[e]

                def body(engine, stream=stream):
                    for waits, fns, inc in stream:
                        for sem, val in waits:
                            engine.wait_ge(sem, val)
                        ins = None
                        for f in fns:
                            ins = f(engine)
                        if inc is not None:
                            ins.then_inc(inc[0], inc[1])
                getattr(block, engmap[e])(body)


_BREG = {}


def breg(e, val):
    if val not in _BREG:
        _BREG[val] = e.to_reg(val)
    return _BREG[val]


def mm(P, out, pairs, reads, writes):
    n = len(pairs)
    fns = [(lambda e, l=l, r=r, i=i: e.matmul(out, l, r, start=(i == 0), stop=(i == n - 1)))
           for i, (l, r) in enumerate(pairs)]
    return P.op("pe", fns, reads, writes)


class RR:
    def __init__(self, items):
        self.items = list(items)
        self.i = 0

    def next(self):
        v = self.items[self.i % len(self.items)]
        self.i += 1
        return v


def build():
    _BREG.clear()
    nc = bass.Bass("TRN2", target_bir_lowering=False)
    try:
        nc.allow_low_precision("bf16 matmul operands with fp32 accumulation")
    except Exception:
        pass
    outs = {}

    def din(name, shape, dt=F32):
        return nc.dram_tensor(name, list(shape), dt, kind="ExternalInput").ap()

    def scratch(name, shape, dt):
        kind = "ExternalOutput" if ((DEBUG and name in DBG_NAMES) or ALL_EXT) else "Internal"
        t = nc.dram_tensor(name, list(shape), dt, kind=kind).ap()
        if DEBUG:
            outs[name] = t
        return t

    x_d = din("x", [S, D])
    mem_d = din("mem", [256, D])
    w_in = din("w_in", [D, 5888])
    gmixT = din("gmixT", [128, 8])
    gmemT = din("gmemT", [128, 8])
    w_mem_kv = din("w_mem_kv", [D, 512])
    identb_d = din("identb_in", [128, 128], BF16)
    identf_d = din("identf_in", [128, 128])
    out_d = nc.dram_tensor("out", [S, D], F32, kind="ExternalOutput").ap()

    hT_d = scratch("hT_s", [D, S], BF16)
    qk_d = scratch("qk_s", [12, 64, S], BF16)
    v_d = scratch("v_s", [S, 390], BF16)
    prw_d = scratch("prw_s", [22, 64, S + 2], F32)
    mq_d = scratch("mq_s", [4, 64, S], BF16)
    ynaT_d = scratch("ynaT_s", [384, S], BF16)
    ymemT_d = scratch("ymemT_s", [256, S], BF16)
    biasP_d = din("biasP_in", [128, 6 * 14 * 64])
    maskP_d = din("maskP_in", [128, 64])
    mu_d = din("mu_in", [64, 40])
    w2a_d = din("w2a_in", [65, 768])
    a2a_d = din("a2a_in", [65, 768])
    kkp_d = din("kkp_in", [64, 6])
    kap_d = din("kap_in", [64, 6])
    rkp_d = din("rkp_in", [64, 6])
    g2_d = din("g2_in", [64, 768])
    lng_d = din("lng_in", [64, 384])
    lnb_d = din("lnb_in", [64, 384])
    msk_d = din("msk_in", [64, 192])
    jrev_d = din("jrev_in", [64, 64])
    wbna_d = din("w_branch_na", [384, D])
    wbrw_d = din("w_branch_rw", [384, D])
    wbmem_d = din("w_branch_mem", [256, D])
    wout_d = din("w_out", [D, D])
    wrt_d = din("w_router", [D, 16])
    gffn_d = din("gffn_in", [128, D])
    tail_d = din("tail_in", [128, 256])
    acc_d = scratch("acc_s", [S, D], F32)
    ltm_d = din("ltm_in", [128, 128])
    gfin_d = din("gfin_in", [128, D])
    wgate_d = din("w_exp_gate", [16, D, 2048])
    wup_d = din("w_exp_up", [16, D, 2048])
    wdown_d = din("w_exp_down", [16, 2048, D])
    xe_d = [scratch(f"xe_s{e}", [1024, 1028], BF16) for e in range(16)]
    rank_dbg = scratch("rank_dbg", [128, 1024], F32) if DEBUG else None
    h2_d = scratch("h2_s", [S, 1028], BF16)
    aff_d = scratch("aff_s", [S, 16], F32)
    yb_d = scratch("yb_s", [S, 2, 384], F32)
    yrwT_d = scratch("yrwT_s", [384, S], BF16)

    with ExitStack() as es:
        P = Prog(nc, es)

        def sb(name, shape, dt, stack=es):
            return stack.enter_context(nc.sbuf_tensor("sb_" + name, list(shape), dt))

        def ps(name, shape, dt, stack=es):
            return stack.enter_context(nc.psum_tensor("ps_" + name, list(shape), dt))

        identb = sb("identb", [128, 128], BF16)
        identf = sb("identf", [128, 128], F32)
        gmix = sb("gmix", [128, 8], F32)
        gmem = sb("gmem", [128, 8], F32)
        mkT = sb("mkT", [64, 4, 256], BF16)
        mv = sb("mv", [128, 2, 4, 65], BF16)
        zero = sb("zero", [64, 32], F32)
        P.dma("sp", lambda e: e.dma_start(out=identb[:], in_=identb_d), "c_identb", writes=["identb"])
        P.dma("sp", lambda e: e.dma_start(out=identf[:], in_=identf_d), "c_identf", writes=["identf"])
        P.dma("sp", lambda e: e.dma_start(out=gmix[:], in_=gmixT), "c_gmix", writes=["gmix"])
        P.dma("sp", lambda e: e.dma_start(out=gmem[:], in_=gmemT), "c_gmem", writes=["gmem"])
        P.op("dve", lambda e: e.memset(zero[:], 0.0), writes=["zero"])
        P.op("dve", lambda e: e.memset(mv[:], 1.0), writes=["mv"])

        with ExitStack() as pa:
            NCOL = 2816
            wA = sb("wA", [128, 8, NCOL], BF16, pa)
            wkv = sb("wkv", [128, 8, 512], BF16, pa)
            xt = [sb(f"xt{i}", [128, D], F32, pa) for i in range(3)]
            xn = [sb(f"xn{i}", [128, D], BF16, pa) for i in range(2)]
            sq = sb("sqjunk", [128, D], BF16, pa)
            ss = [sb(f"ss{i}", [128, 1], F32, pa) for i in range(3)]
            rs = [sb(f"rs{i}", [128, 1], F32, pa) for i in range(3)]
            hTb = [sb(f"hTb{i}", [128, 8, 512], BF16, pa) for i in range(2)]
            qk_sb = [sb(f"qk_sb{i}", [64, 12, 512], BF16, pa) for i in range(2)]
            v_sb = [sb(f"v_sb{i}", [128, 4, 390], BF16, pa) for i in range(2)]
            rw_sb = [sb(f"rw_sb{i}", [64, 11, 512], F32, pa) for i in range(2)]
            mq_sb = [sb(f"mq_sb{i}", [64, 4, 512], BF16, pa) for i in range(2)]
            ptr = [ps(f"ptr{i}", [128, 8, 128], BF16, pa) for i in range(2)]
            pacc = [ps(f"pacc{i}", [128, 512], F32, pa) for i in range(5)]

            w_in_v = w_in.rearrange("(c p) n -> p c n", p=128)
            for c in range(8):
                P.dma("pool", lambda e, c=c: e.dma_start(out=wA[:, c, :], in_=w_in_v[:, c, 0:NCOL]),
                      "wA", writes=[f"wA{c}"])
            wkv_v = w_mem_kv.rearrange("(c p) n -> p c n", p=128)
            P.dma("pool", lambda e: e.dma_start(out=wkv[:], in_=wkv_v), "wkv", writes=["wkv"])
            for g0 in (0, 11):
                for col in (0, S + 1):
                    P.dma("sp", lambda e, g0=g0, col=col: e.dma_start(
                        out=prw_d[g0:g0 + 11, :, col:col + 1].rearrange("g p t -> p g t"),
                        in_=zero[:, 0:11].unsqueeze(2), allow_slow_non_contiguous=True), "zpad", reads=["zero"], writes=["prw_pad"])
            for i in range(2):
                P.op("pool", lambda e, i=i: e.memset(v_sb[i][:], 1.0), writes=[f"v_sb{i}"])

            evq = RR(["act", "dve"])
            pq = RR(range(5))
            ldq = RR(["sp", "act"])

            def evac(dst, src, reads, writes, eng=None):
                eng = eng or evq.next()
                if eng == "act":
                    P.op("act", lambda e: e.copy(out=dst, in_=src), reads, writes)
                else:
                    P.op("dve", lambda e: e.tensor_copy(out=dst, in_=src), reads, writes)

            def norm_tile(src_ap, ti, gtile, gkey, dst, dstkey, col0):
                b3 = ti % 3
                b2 = ti % 2
                P.dma(ldq.next(), lambda e: e.dma_start(out=xt[b3][:], in_=src_ap), f"xt{b3}", writes=[f"xt{b3}"])
                P.op("act", lambda e: e.activation(out=sq[:], in_=xt[b3][:], func=AF.Square, accum_out=ss[b3][:]),
                     reads=[f"xt{b3}"], writes=["sq", f"ss{b3}"])
                P.op("act", lambda e: e.activation(out=rs[b3][:], in_=ss[b3][:], func=AF.Sqrt, scale=1.0 / D, bias=eps_t[:]),
                     reads=[f"ss{b3}", "eps"], writes=[f"rs{b3}"])
                P.op("dve", lambda e: e.reciprocal(out=rs[b3][:], in_=rs[b3][:]), reads=[f"rs{b3}"], writes=[f"rs{b3}"])
                P.op("act", lambda e: e.activation(out=xn[b2][:], in_=xt[b3][:], func=AF.Copy, scale=rs[b3][:]),
                     reads=[f"xt{b3}", f"rs{b3}"], writes=[f"xn{b2}"])
                P.op("pe", [(lambda e, c=c: e.transpose(out=ptr[b2][:, c, :], in_=xn[b2][:, c * 128:(c + 1) * 128], identity=identb[:]))
                            for c in range(8)], reads=[f"xn{b2}", "identb"], writes=[f"ptr{b2}"])
                P.op("dve", lambda e: e.tensor_tensor(out=dst[:, :, col0:col0 + 128], in0=ptr[b2][:],
                                                      in1=gtile[:].unsqueeze(2).to_broadcast([128, 8, 128]), op=ALU.mult),
                     reads=[f"ptr{b2}", gkey], writes=[dstkey])

            eps_t = sb("eps_t", [128, 1], F32, pa)
            P.op("dve", lambda e: e.memset(eps_t[:], 1e-6), writes=["eps"])

            mhT = sb("mhT", [128, 8, 256], BF16, pa)
            for t in range(2):
                norm_tile(mem_d[t * 128:(t + 1) * 128, :], t, gmem, "gmem", mhT, "mhT", t * 128)
            for hh in range(4):
                pi = pq.next()
                mm(P, pacc[pi][0:64, 0:256], [(wkv[:, c, hh * 64:(hh + 1) * 64], mhT[:, c, :]) for c in range(8)],
                   reads=["wkv", "mhT"], writes=[f"pacc{pi}"])
                evac(mkT[:, hh, :], pacc[pi][0:64, 0:256], [f"pacc{pi}"], ["mkT"])
            for t in range(2):
                pi = pq.next()
                mm(P, pacc[pi][:, 0:256], [(mhT[:, c, t * 128:(t + 1) * 128], wkv[:, c, 256:512]) for c in range(8)],
                   reads=["wkv", "mhT"], writes=[f"pacc{pi}"])
                evac(mv[:, t, :, 0:64], pacc[pi][:, 0:256].rearrange("p (h d) -> p h d", h=4), [f"pacc{pi}"], ["mv"])

            wAkeys = [f"wA{c}" for c in range(8)]
            for blk in range(S // 512):
                hb = blk % 2
                t0 = blk * 512
                for j in range(4):
                    ti = blk * 4 + j
                    norm_tile(x_d[ti * 128:(ti + 1) * 128, :], ti + 2, gmix, "gmix", hTb[hb], f"hTb{hb}", j * 128)
                P.dma("sp", lambda e, hb=hb, t0=t0: e.dma_start(
                    out=hT_d.rearrange("(c p) t -> p c t", p=128)[:, :, t0:t0 + 512], in_=hTb[hb][:]),
                    f"st_hT{hb}", reads=[f"hTb{hb}"], writes=["hT_d"])
                for g in range(12):
                    pi = pq.next()
                    mm(P, pacc[pi][0:64, :], [(wA[:, c, g * 64:(g + 1) * 64], hTb[hb][:, c, :]) for c in range(8)],
                       reads=wAkeys + [f"hTb{hb}"], writes=[f"pacc{pi}"])
                    evac(qk_sb[hb][:, g, :], pacc[pi][0:64, :], [f"pacc{pi}"], [f"qk_sb{hb}"])
                P.dma("sp", lambda e, hb=hb, t0=t0: e.dma_start(
                    out=qk_d[:, :, t0:t0 + 512].rearrange("g p t -> p g t"), in_=qk_sb[hb][:]),
                    f"st_qk{hb}", reads=[f"qk_sb{hb}"], writes=["qk_d"])
                for j in range(4):
                    pi = pq.next()
                    mm(P, pacc[pi][:, 0:384], [(hTb[hb][:, c, j * 128:(j + 1) * 128], wA[:, c, 768:1152]) for c in range(8)],
                       reads=wAkeys + [f"hTb{hb}"], writes=[f"pacc{pi}"])
                    evac(v_sb[hb][:, j, :].rearrange("p (h d) -> p h d", h=6)[:, :, 0:64],
                         pacc[pi][:, 0:384].rearrange("p (h d) -> p h d", h=6), [f"pacc{pi}"], [f"v_sb{hb}"])
                P.dma("act", lambda e, hb=hb, t0=t0: e.dma_start(
                    out=v_d[t0:t0 + 512, :].rearrange("(n p) c -> p n c", p=128), in_=v_sb[hb][:]),
                    f"st_v{hb}", reads=[f"v_sb{hb}"], writes=["v_d"])
                for half in range(2):
                    for gg in range(11):
                        g = half * 11 + gg
                        pi = pq.next()
                        c0 = 1152 + g * 64
                        mm(P, pacc[pi][0:64, :], [(wA[:, c, c0:c0 + 64], hTb[hb][:, c, :]) for c in range(8)],
                           reads=wAkeys + [f"hTb{hb}"], writes=[f"pacc{pi}"])
                        evac(rw_sb[half][:, gg, :], pacc[pi][0:64, :], [f"pacc{pi}"], [f"rw_sb{half}"])
                    P.dma("sp", lambda e, half=half, t0=t0: e.dma_start(
                        out=prw_d[half * 11:half * 11 + 11, :, 1 + t0:1 + t0 + 512].rearrange("g p t -> p g t"),
                        in_=rw_sb[half][:]), f"st_rw{half}", reads=[f"rw_sb{half}"], writes=["prw_d"])
                for g in range(4):
                    pi = pq.next()
                    c0 = 2560 + g * 64
                    mm(P, pacc[pi][0:64, :], [(wA[:, c, c0:c0 + 64], hTb[hb][:, c, :]) for c in range(8)],
                       reads=wAkeys + [f"hTb{hb}"], writes=[f"pacc{pi}"])
                    evac(mq_sb[hb][:, g, :], pacc[pi][0:64, :], [f"pacc{pi}"], [f"mq_sb{hb}"])
                P.dma("act", lambda e, hb=hb, t0=t0: e.dma_start(
                    out=mq_d[:, :, t0:t0 + 512].rearrange("g p t -> p g t"), in_=mq_sb[hb][:]),
                    f"st_mq{hb}", reads=[f"mq_sb{hb}"], writes=["mq_d"])
            P.barrier()

        if PHASES >= 2:
          with ExitStack() as pb:
            EP = sb("EP", [128, 6, 14, 64], BF16, pb)
            biasP = sb("biasP", [128, 6 * 14 * 64], F32, pb)
            maskP = sb("maskP", [128, 64], F32, pb)
            bank = [ps(f"bank{i}", [128, 512], F32, pb) for i in range(6)]
            pbt = ps("pbt", [128, 1024], BF16, pb)
            P.dma("sp", lambda e: e.dma_start(out=biasP[:], in_=biasP_d), "c_biasP", writes=["biasP"])
            P.dma("sp", lambda e: e.dma_start(out=maskP[:], in_=maskP_d), "c_maskP", writes=["maskP"])
            for h in range(6):
                P.op("act", lambda e, h=h: e.activation(out=biasP[:, h * 896:(h + 1) * 896], in_=biasP[:, h * 896:(h + 1) * 896], func=AF.Exp),
                     reads=["biasP"], writes=["biasP"])
                P.op("dve", lambda e, h=h: e.tensor_tensor(out=EP[:, h, :, :], in0=biasP[:, h * 896:(h + 1) * 896].rearrange("p (a q) -> p a q", q=64),
                                                           in1=maskP[:].unsqueeze(1).to_broadcast([128, 14, 64]), op=ALU.mult),
                     reads=["biasP", "maskP"], writes=["EP"])
            qrow = [sb(f"qrow{i}", [64, 6, 64], BF16, pb) for i in range(2)]
            kwin = [sb(f"kwin{i}", [64, 6, 512], BF16, pb) for i in range(2)]
            vwin = [sb(f"vwin{i}", [128, 4, 390], BF16, pb) for i in range(2)]
            pex = [sb(f"pex{i}", [128, 2, 4, 64], BF16, pb) for i in range(3)]
            rec = [sb(f"rec{i}", [128, 6], F32, pb) for i in range(2)]
            yrow = [sb(f"yrow{i}", [128, 384], BF16, pb) for i in range(2)]
            ynaT_sb = [sb(f"ynaT_sb{i}", [128, 3, 512], BF16, pb) for i in range(2)]
            mq_b = [sb(f"mq_b{i}", [64, 4, 512], BF16, pb) for i in range(2)]
            mpex = [sb(f"mpex{i}", [128, 2, 512], BF16, pb) for i in range(4)]
            ymT_sb = [sb(f"ymT_sb{i}", [128, 2, 512], BF16, pb) for i in range(2)]
            bq = RR(range(6))
            BK = lambda k: [f"bank{k}"] + [f"bank{k}_{h}" for h in range(6)]

            def na_hp(r, b2, ro0, hp, pvb, pv):
                sbk = bq.next()
                sT = bank[sbk][:].rearrange("p (a j q) -> p a j q", a=2, j=4)
                fns = []
                for a in range(2):
                    for j in range(4):
                        fns.append(lambda e, a=a, j=j: e.matmul(sT[:, a, j, :], kwin[b2][:, hp * 2 + a, j * 128:(j + 1) * 128],
                                                                qrow[b2][:, hp * 2 + a, :], start=True, stop=True))
                P.op("pe", fns, reads=[f"kwin{b2}", f"qrow{b2}"], writes=BK(sbk))
                P.op("act", lambda e: e.activation(out=pex[hp][:], in_=sT, func=AF.Exp, scale=0.125),
                     reads=BK(sbk), writes=[f"pex{hp}"])
                a0 = (ro0 % 2) * 7 + ro0 // 2
                P.op("dve", lambda e: e.tensor_tensor(out=pex[hp][:], in0=pex[hp][:], in1=EP[:, hp * 2:hp * 2 + 2, a0:a0 + 4, :], op=ALU.mult),
                     reads=[f"pex{hp}", "EP"], writes=[f"pex{hp}"])
                for a in range(2):
                    h = hp * 2 + a
                    mm(P, pv[:, h, :], [(pex[hp][:, a, j, :], vwin[b2][:, j, h * 65:(h + 1) * 65]) for j in range(4)],
                       reads=[f"pex{hp}", f"vwin{b2}"], writes=[f"bank{pvb}_{h}"])

            def na_row(r):
                b2 = r % 2
                rs_ = min(max(r - 4, 0), 120)
                ro0 = rs_ - r + 7
                blk = r // 8
                P.dma("sp", lambda e: e.dma_start(out=qrow[b2][:], in_=qk_d[0:6, :, r * 64:(r + 1) * 64].rearrange("g p t -> p g t")),
                      f"ld_q{b2}", writes=[f"qrow{b2}"])
                P.dma("sp", lambda e: e.dma_start(out=kwin[b2][:], in_=qk_d[6:12, :, rs_ * 64:rs_ * 64 + 512].rearrange("g p t -> p g t")),
                      f"ld_k{b2}", writes=[f"kwin{b2}"])
                P.dma("act", lambda e: e.dma_start(out=vwin[b2][:], in_=v_d[rs_ * 64:rs_ * 64 + 512, :].rearrange("(n p) c -> p n c", p=128)),
                      f"ld_v{b2}", writes=[f"vwin{b2}"])
                pvb = bq.next()
                pv = bank[pvb][0:64, 0:390].rearrange("p (h d) -> p h d", h=6)
                for hp in range(3):
                    na_hp(r, b2, ro0, hp, pvb, pv)
                pvkeys = [f"bank{pvb}_{h}" for h in range(6)]
                P.op("dve", lambda e: e.reciprocal(out=rec[b2][0:64, :], in_=pv[:, :, 64]), reads=pvkeys, writes=[f"rec{b2}"])
                P.op("dve", lambda e: e.tensor_tensor(out=yrow[b2][0:64, :].rearrange("p (h d) -> p h d", h=6), in0=pv[:, :, 0:64],
                                                      in1=rec[b2][0:64, :].unsqueeze(2).to_broadcast([64, 6, 64]), op=ALU.mult),
                     reads=pvkeys + [f"rec{b2}"], writes=[f"yrow{b2}"] + pvkeys)
                P.op("pe", [(lambda e, c=c: e.transpose(out=pbt[:, c * 64:(c + 1) * 64], in_=yrow[b2][0:64, c * 128:(c + 1) * 128], identity=identb[0:64, 0:64]))
                            for c in range(3)], reads=[f"yrow{b2}", "identb"], writes=["pbt_na"])
                yb = blk % 2
                P.op("act", lambda e: e.copy(out=ynaT_sb[yb][:, :, (r % 8) * 64:(r % 8) * 64 + 64], in_=pbt[:, 0:192].rearrange("p (c q) -> p c q", c=3)),
                     reads=["pbt_na"], writes=[f"ynaT_sb{yb}"])
                if r % 8 == 7:
                    t0 = blk * 512
                    P.dma("sp", lambda e: e.dma_start(out=ynaT_d.rearrange("(c p) t -> p c t", p=128)[:, :, t0:t0 + 512], in_=ynaT_sb[yb][:]),
                          f"st_yna{yb}", reads=[f"ynaT_sb{yb}"], writes=["ynaT_d"])
                    mem_block(blk)

            def mem_s(yb, h, kt):
                sbk = bq.next()
                mm(P, bank[sbk][:], [(mkT[:, h, kt * 128:(kt + 1) * 128], mq_b[yb][:, h, :])], reads=["mkT", f"mq_b{yb}"], writes=BK(sbk))
                P.op("act", lambda e: e.activation(out=mpex[h][:, kt, :], in_=bank[sbk][:], func=AF.Exp, scale=0.125),
                     reads=BK(sbk), writes=[f"mpex{h}"])

            def mem_pv(yb, qt):
                pvb = bq.next()
                pvm = bank[pvb][:, 0:260].rearrange("p (h d) -> p h d", h=4)
                for h in range(4):
                    mm(P, pvm[:, h, :], [(mpex[h][:, kt, qt * 128:(qt + 1) * 128], mv[:, kt, h, :]) for kt in range(2)],
                       reads=[f"mpex{h}", "mv"], writes=[f"bank{pvb}_{h}"])
                pk = [f"bank{pvb}_{h}" for h in range(4)]
                rb = qt % 2
                P.op("dve", lambda e: e.reciprocal(out=rec[rb][:, 0:4], in_=pvm[:, :, 64]), reads=pk, writes=[f"rec{rb}"])
                P.op("dve", lambda e: e.tensor_tensor(out=yrow[rb][:, 0:256].rearrange("p (h d) -> p h d", h=4), in0=pvm[:, :, 0:64],
                                                      in1=rec[rb][:, 0:4].unsqueeze(2).to_broadcast([128, 4, 64]), op=ALU.mult),
                     reads=pk + [f"rec{rb}"], writes=[f"yrow{rb}"] + pk)
                P.op("pe", [(lambda e, c=c: e.transpose(out=pbt[:, 512 + c * 128:512 + (c + 1) * 128], in_=yrow[rb][:, c * 128:(c + 1) * 128], identity=identb[:]))
                            for c in range(2)], reads=[f"yrow{rb}", "identb"], writes=["pbt_m"])
                P.op("act", lambda e: e.copy(out=ymT_sb[yb][:, :, qt * 128:(qt + 1) * 128], in_=pbt[:, 512:768].rearrange("p (c q) -> p c q", c=2)),
                     reads=["pbt_m"], writes=[f"ymT_sb{yb}"])

            def mem_block(blk):
                yb = blk % 2
                t0 = blk * 512
                P.dma("act", lambda e: e.dma_start(out=mq_b[yb][:], in_=mq_d[:, :, t0:t0 + 512].rearrange("g p t -> p g t")),
                      f"ld_mq{yb}", writes=[f"mq_b{yb}"])
                for h in range(4):
                    for kt in range(2):
                        mem_s(yb, h, kt)
                for qt in range(4):
                    mem_pv(yb, qt)
                P.dma("sp", lambda e: e.dma_start(out=ymemT_d.rearrange("(c p) t -> p c t", p=128)[:, :, t0:t0 + 512], in_=ymT_sb[yb][:]),
                      f"st_ym{yb}", reads=[f"ymT_sb{yb}"], writes=["ymemT_d"])

            for r in range(128):
                na_row(r)
            P.barrier()

        if PHASES >= 3:
          with ExitStack() as pc:
            C0 = 0.6065306597126334
            cst = {}
            for nm, shp, src in (("mu", [64, 40], mu_d), ("w2a", [65, 768], w2a_d), ("a2a", [65, 768], a2a_d), ("kkp", [64, 6], kkp_d),
                                 ("kap", [64, 6], kap_d), ("rkp", [64, 6], rkp_d), ("g2", [64, 768], g2_d), ("lng", [64, 384], lng_d),
                                 ("lnb", [64, 384], lnb_d), ("msk", [64, 192], msk_d), ("jrev", [64, 64], jrev_d)):
                cst[nm] = sb("c_" + nm, shp, F32, pc)
                P.dma("sp", (lambda e, t=cst[nm], src=src: e.dma_start(out=t[:], in_=src)), "c_" + nm, writes=[nm])
            oma = sb("oma", [64, 6], F32, pc)
            P.op("dve", lambda e: e.tensor_scalar(out=oma[:], in0=cst["kap"][:], scalar1=-1.0, scalar2=1.0, op0=ALU.mult, op1=ALU.add),
                 reads=["kap"], writes=["oma"])
            ones64 = sb("ones64", [64, 384], F32, pc)
            P.op("dve", lambda e: e.memset(ones64[:], 1.0), writes=["ones64"])
            eps12 = sb("eps12", [64, 1], F32, pc)
            P.op("dve", lambda e: e.memset(eps12[:], 64e-5), writes=["eps12"])
            offs = [sb(f"offs{i}", [64, 6], F32, pc) for i in range(2)]
            twa = [sb(f"twa{i}", [65, 2, 64], F32, pc) for i in range(2)]
            for i in range(2):
                P.op("dve", lambda e, i=i: e.memset(offs[i][:], 0.0), writes=[f"offs{i}"])
                P.op("dve", lambda e, i=i: e.memset(twa[i][:], 1.0), writes=[f"twa{i}"])
            Hs = [sb(f"H{i}", [64, 384], F32, pc) for i in range(2)]
            rbank = [ps(f"rbank{i}", [64, 512], F32, pc) for i in range(7)]
            rbt = ps("rbt", [128, 1024], BF16, pc)
            rq = RR(range(7))
            names3 = ["mx", "df"]
            names = ["sg", "al", "L", "Lx", "Ld", "Pinc", "Pinv", "Pexc", "Pdec", "kkr", "sq", "nrm", "kk", "t1", "k2", "bv",
                     "at", "bt", "kt", "rt", "bh", "kh", "rk", "Vt", "bhT", "khT", "MabT", "Nab", "MakT", "MrbT", "MrkT",
                     "X0", "X1", "Na", "Nb", "Ma", "Mb", "W", "U", "dgP", "yo", "ybl", "cen", "sqc", "sgg", "yn"]
            tl = {}
            for i in range(2):
                tl[("X", i)] = sb(f"rwX{i}", [64, 22, 66], F32, pc)
                tl[("yst", i)] = sb(f"rw_yst{i}", [64, 2, 384], F32, pc)
                tl[("yld", i)] = sb(f"rw_yld{i}", [64, 2, 384], F32, pc)
                tl[("yrwT", i)] = sb(f"rw_yrwT{i}", [128, 3, 512], BF16, pc)
                if i == 1:
                    for nm in ("at", "bt", "kt", "rt", "bh", "kh", "bhT", "khT", "Vt", "dgP", "MabT", "Nab", "MakT", "MrbT", "MrkT"):
                        tl[(nm, i)] = sb(f"rw_{nm}{i}", [64, 6, 64], F32, pc)
                    tl[("bon", i)] = sb(f"rw_bon{i}", [64, 6], F32, pc)
                    tl[("Pend", i)] = sb(f"rw_Pend{i}", [64, 6], F32, pc)
                    continue
                for nm in names3:
                    tl[(nm, i)] = sb(f"rw_{nm}{i}", [64, 20, 64], F32, pc)
                for nm in names:
                    tl[(nm, i)] = sb(f"rw_{nm}{i}", [64, 6, 64], F32, pc)
                tl[("bon", i)] = sb(f"rw_bon{i}", [64, 6], F32, pc)
                tl[("Lend", i)] = sb(f"rw_Lend{i}", [64, 6], F32, pc)
                tl[("Pend", i)] = sb(f"rw_Pend{i}", [64, 6], F32, pc)
                tl[("st", i)] = sb(f"rw_st{i}", [64, 6], F32, pc)
                tl[("st2", i)] = sb(f"rw_st2{i}", [64, 6], F32, pc)
                tl[("yrwb", i)] = sb(f"rw_yrwb{i}", [64, 384], BF16, pc)
            evr = RR(["dve", "pool"])
            mskU = cst["msk"][:, 0:64]
            mskUi = cst["msk"][:, 64:128]
            mskL = cst["msk"][:, 128:192]
            b6 = lambda ap2: ap2.unsqueeze(1).to_broadcast([64, 6, 64])
            c6 = lambda ap2: ap2.unsqueeze(2).to_broadcast([64, 6, 64])

            def rw_chunk(d, i):
                pb_ = i % 2
                DB = ("X", "yst", "yld", "at", "bt", "kt", "rt", "bh", "kh", "bhT", "khT", "Vt", "dgP", "MabT", "Nab", "MakT", "MrbT", "MrkT", "bon", "Pend")
                T = lambda nm: tl[(nm, pb_ if nm in DB else 0)]
                K = lambda nm: f"rw_{nm}{pb_ if nm in DB else 0}"
                ci = i if d == 0 else 127 - i
                t0 = ci * 64
                ng = 22 if d == 0 else 20
                X = T("X")
                P.dma("sp", lambda e: e.dma_start(out=X[:, 0:ng, :], in_=prw_d[0:ng, :, t0:t0 + 66].rearrange("g p t -> p g t")),
                      f"ld_X{pb_}", writes=[K("X")])
                if d == 0:
                    cur, shf = X[:, 0:20, 1:65], X[:, 0:20, 0:64]
                else:
                    cur, shf = X[:, 0:20, 64:0:-1], X[:, 0:20, 65:1:-1]

                def tt(eng, out, in0, in1, op, reads, writes):
                    P.op(eng, lambda e: e.tensor_tensor(out=out, in0=in0, in1=in1, op=op), reads, writes)

                def act(out, in_, func, reads, writes, **kw):
                    P.op("act", lambda e: e.activation(out=out, in_=in_, func=func, **kw), reads, writes)

                def mm6(bank_i, lk, rk_, lhs_f, rhs_f, extra_reads=()):
                    ov = rbank[bank_i][:, 0:384].rearrange("p (h t) -> p h t", h=6)
                    fns = [(lambda e, h=h: e.matmul(ov[:, h, :], lhs_f(h), rhs_f(h), start=True, stop=True)) for h in range(6)]
                    P.op("pe", fns, reads=list(lk) + list(rk_) + list(extra_reads), writes=[f"rbank{bank_i}"])
                    return ov

                def mmacc(bank_i, terms, reads):
                    ov = rbank[bank_i][:, 0:384].rearrange("p (h t) -> p h t", h=6)
                    fns = []
                    n = len(terms)
                    for h in range(6):
                        for k_, (lf, rf) in enumerate(terms):
                            fns.append(lambda e, h=h, k_=k_, lf=lf, rf=rf: e.matmul(ov[:, h, :], lf(h), rf(h), start=(k_ == 0), stop=(k_ == n - 1)))
                    P.op("pe", fns, reads=reads, writes=[f"rbank{bank_i}"])
                    return ov

                mub = cst["mu"][:, d * 20:(d + 1) * 20].unsqueeze(2).to_broadcast([64, 20, 64])
                mub_s = cst["mu"][:, d * 20 + 18:d * 20 + 20].unsqueeze(2).to_broadcast([64, 2, 64])
                mub_b = cst["mu"][:, d * 20:d * 20 + 18].unsqueeze(2).to_broadcast([64, 18, 64])
                Kw, Kdw = K("mx") + "w", K("df") + "w"
                dfs, dfb = T("df")[:, 18:20, :], T("df")[:, 0:18, :]
                mxs_, mxb = T("mx")[:, 18:20, :], T("mx")[:, 0:18, :]
                tt("dve", dfs, shf[:, 18:20, :], cur[:, 18:20, :], ALU.subtract, [K("X")], [Kdw])
                tt("dve", dfs, dfs, mub_s, ALU.mult, [Kdw, "mu"], [Kdw])
                tt("dve", mxs_, dfs, cur[:, 18:20, :], ALU.add, [Kdw, K("X")], [Kw])
                tt("pool", dfb, shf[:, 0:18, :], cur[:, 0:18, :], ALU.subtract, [K("X")], [K("df")])
                tt("pool", dfb, dfb, mub_b, ALU.mult, [K("df"), "mu"], [K("df")])
                tt("pool", mxb, dfb, cur[:, 0:18, :], ALU.add, [K("df"), K("X")], [K("mx")])
                mx = T("mx")
                r_, k_, v_ = mx[:, 0:6, :], mx[:, 6:12, :], mx[:, 12:18, :]
                tw = twa[pb_]
                act(tw[0:64, 0, :], mx[:, 18, :], AF.Tanh, [Kw], [f"twa{pb_}"])
                P.op("dve", lambda e: e.tensor_copy(out=tw[0:64, 1, :], in_=mx[:, 19, :]), [Kw], [f"twa{pb_}"])
                zb = rq.next()
                zp = mm6(zb, [f"twa{pb_}"], ["w2a"], lambda h: cst["w2a"][:, d * 384 + h * 64:d * 384 + (h + 1) * 64], lambda h: tw[:, 0, :])
                act(T("sg")[:], zp, AF.Sigmoid, [f"rbank{zb}"], [K("sg")])
                ab = rq.next()
                ap_ = mm6(ab, [f"twa{pb_}"], ["a2a"], lambda h: cst["a2a"][:, d * 384 + h * 64:d * 384 + (h + 1) * 64], lambda h: tw[:, 1, :])
                act(T("al")[:], ap_, AF.Sigmoid, [f"rbank{ab}"], [K("al")])
                Lf = T("L")[:].rearrange("p h t -> p (h t)")
                P.op("dve", lambda e: e.tensor_tensor_scan(out=Lf, data0=ones64[:], data1=T("sg")[:].rearrange("p h t -> p (h t)"),
                                                           initial=0.0, op0=ALU.mult, op1=ALU.add), [K("sg"), "ones64"], [K("L")])
                of = offs[pb_]
                P.op("dve", lambda e: e.tensor_copy(out=of[:, 1:6], in_=T("L")[:, 0:5, 63]), [K("L")], [f"offs{pb_}"])
                tt("dve", T("L")[:], T("L")[:], c6(of[:]), ALU.subtract, [K("L"), f"offs{pb_}"], [K("L")])
                tt("dve", T("Lx")[:], T("L")[:], T("sg")[:], ALU.subtract, [K("L"), K("sg")], [K("Lx")])
                P.op("dve", lambda e: e.tensor_copy(out=T("Lend")[:], in_=T("L")[:, :, 63]), [K("L")], [K("Lend")])
                tt("dve", T("Ld")[:], T("L")[:], c6(T("Lend")[:]), ALU.subtract, [K("L"), K("Lend")], [K("Ld")])
                act(T("Pinc")[:], T("L")[:], AF.Exp, [K("L")], [K("Pinc")], scale=-C0)
                act(T("Pinv")[:], T("L")[:], AF.Exp, [K("L")], [K("Pinv")], scale=C0)
                act(T("Pexc")[:], T("Lx")[:], AF.Exp, [K("Lx")], [K("Pexc")], scale=-C0)
                act(T("Pdec")[:], T("Ld")[:], AF.Exp, [K("Ld")], [K("Pdec")], scale=C0)
                act(T("Pend")[:], T("Lend")[:], AF.Exp, [K("Lend")], [K("Pend")], scale=-C0)
                tt("dve", T("kkr")[:], k_, c6(cst["kkp"][:]), ALU.mult, [K("mx"), "kkp"], [K("kkr")])
                tt("dve", T("sq")[:], T("kkr")[:], T("kkr")[:], ALU.mult, [K("kkr")], [K("sq")])
                sb_ = rq.next()
                ssp = mm6(sb_, [K("sq")], ["ones64"], lambda h: ones64[:, 0:64], lambda h: T("sq")[:, h, :])
                act(T("nrm")[:], ssp, AF.Sqrt, [f"rbank{sb_}"], [K("nrm")])
                P.op("dve", lambda e: e.tensor_scalar_max(out=T("nrm")[:], in0=T("nrm")[:], scalar1=1e-12), [K("nrm")], [K("nrm")])
                P.op("dve", lambda e: e.reciprocal(out=T("nrm")[:], in_=T("nrm")[:]), [K("nrm")], [K("nrm")])
                tt("dve", T("kk")[:], T("kkr")[:], T("nrm")[:], ALU.mult, [K("kkr"), K("nrm")], [K("kk")])
                tt("dve", T("t1")[:], T("al")[:], c6(cst["kap"][:]), ALU.mult, [K("al"), "kap"], [K("t1")])
                tt("dve", T("t1")[:], T("t1")[:], c6(oma[:]), ALU.add, [K("t1"), "oma"], [K("t1")])
                tt("dve", T("k2")[:], k_, T("t1")[:], ALU.mult, [K("mx"), K("t1")], [K("k2")])
                tt("dve", T("bv")[:], T("kk")[:], T("al")[:], ALU.mult, [K("kk"), K("al")], [K("bv")])
                P.op("dve", lambda e: e.scalar_tensor_tensor(out=T("at")[:], in0=T("kk")[:], scalar=-1.0, in1=T("Pexc")[:], op0=ALU.mult, op1=ALU.mult),
                     [K("kk"), K("Pexc")], [K("at")])
                tt("dve", T("bt")[:], T("bv")[:], T("Pinv")[:], ALU.mult, [K("bv"), K("Pinv")], [K("bt")])
                tt("dve", T("kt")[:], T("k2")[:], T("Pinv")[:], ALU.mult, [K("k2"), K("Pinv")], [K("kt")])
                tt("dve", T("rt")[:], r_, T("Pinc")[:], ALU.mult, [K("mx"), K("Pinc")], [K("rt")])
                tt("dve", T("bh")[:], T("bv")[:], T("Pdec")[:], ALU.mult, [K("bv"), K("Pdec")], [K("bh")])
                tt("dve", T("kh")[:], T("k2")[:], T("Pdec")[:], ALU.mult, [K("k2"), K("Pdec")], [K("kh")])
                tt("dve", T("rk")[:], r_, T("k2")[:], ALU.mult, [K("mx"), K("k2")], [K("rk")])
                tt("dve", T("rk")[:], T("rk")[:], c6(cst["rkp"][:]), ALU.mult, [K("rk"), "rkp"], [K("rk")])
                bb = rq.next()
                bfn = [(lambda e, h=h: e.matmul(rbank[bb][:, h:h + 1], T("rk")[:, h, :], ones64[:, 0:1], start=True, stop=True)) for h in range(6)]
                P.op("pe", bfn, reads=[K("rk"), "ones64"], writes=[f"rbank{bb}"])
                P.op("act", lambda e: e.copy(out=T("bon")[:], in_=rbank[bb][:, 0:6]), [f"rbank{bb}"], [K("bon")])
                idf = identf[0:64, 0:64]
                for src_nm, dst_nm, srcap, srckey in (("v", "Vt", v_, K("mx")), ("bh", "bhT", T("bh")[:], K("bh")), ("kh", "khT", T("kh")[:], K("kh"))):
                    tb = rq.next()
                    tv = rbank[tb][:, 0:384].rearrange("p (h t) -> p h t", h=6)
                    P.op("pe", [(lambda e, h=h, tv=tv, srcap=srcap: e.transpose(out=tv[:, h, :], in_=srcap[:, h, :], identity=idf)) for h in range(6)],
                         reads=[srckey, "identf"], writes=[f"rbank{tb}"])
                    P.op("act", lambda e, tv=tv, dst_nm=dst_nm: e.copy(out=T(dst_nm)[:], in_=tv), [f"rbank{tb}"], [K(dst_nm)])
                for nm, ln, rn, msk in (("MabT", "bt", "at", mskU), ("Nab", "at", "bt", mskL), ("MakT", "kt", "at", mskU),
                                        ("MrbT", "bt", "rt", mskUi), ("MrkT", "kt", "rt", mskUi)):
                    mb = rq.next()
                    ov = mm6(mb, [K(ln)], [K(rn)], (lambda h, ln=ln: T(ln)[:, h, :]), (lambda h, rn=rn: T(rn)[:, h, :]))
                    tt("dve", T(nm)[:], ov, b6(msk), ALU.mult, [f"rbank{mb}", "msk"], [K(nm)])
                tt("dve", T("X0")[:], T("MabT")[:], b6(idf), ALU.add, [K("MabT"), "identf"], [K("X0")])
                Mc, Nc, Xc = "MabT", "Nab", "X0"
                for j in range(1, 6):
                    Nn = "Na" if j % 2 else "Nb"
                    Mn = "Ma" if j % 2 else "Mb"
                    Xn = "X1" if j % 2 else "X0"
                    nb = rq.next()
                    ov = mm6(nb, [K(Mc)], [K(Nc)], (lambda h, Mc=Mc: T(Mc)[:, h, :]), (lambda h, Nc=Nc: T(Nc)[:, h, :]))
                    P.op("act", lambda e, ov=ov, Nn=Nn: e.copy(out=T(Nn)[:], in_=ov), [f"rbank{nb}"], [K(Nn)])
                    if j < 5:
                        mb = rq.next()
                        ov2 = mm6(mb, [K(Nc)], [K(Mc)], (lambda h, Nc=Nc: T(Nc)[:, h, :]), (lambda h, Mc=Mc: T(Mc)[:, h, :]))
                        P.op("dve", lambda e, ov2=ov2, Mn=Mn: e.tensor_copy(out=T(Mn)[:], in_=ov2), [f"rbank{mb}"], [K(Mn)])
                    xb = rq.next()
                    ov3 = mm6(xb, [K(Nn)], [K(Xc)], (lambda h, Nn=Nn: T(Nn)[:, h, :]), (lambda h, Xc=Xc: T(Xc)[:, h, :]))
                    tt("dve", T(Xn)[:], ov3, T(Xc)[:], ALU.add, [f"rbank{xb}", K(Xc)], [K(Xn)])
                    Mc, Nc, Xc = Mn, Nn, Xn
                XT = Xc
                Hc, Hn = Hs[i % 2], Hs[(i + 1) % 2]
                Hck, Hnk = f"H{i % 2}", f"H{(i + 1) % 2}"
                Hv = lambda Ht: (lambda h: Ht[:, h * 64:(h + 1) * 64])
                tt("dve", T("dgP")[:], b6(idf), c6(T("Pend")[:]), ALU.mult, ["identf", K("Pend")], [K("dgP")])
                wb = rq.next()
                ov = mmacc(wb, [((lambda h: T("at")[:, h, :]), Hv(Hc)), ((lambda h: T("MakT")[:, h, :]), (lambda h: T("Vt")[:, h, :]))],
                           reads=[K("at"), Hck, K("MakT"), K("Vt")])
                P.op("act", lambda e: e.copy(out=T("W")[:], in_=ov), [f"rbank{wb}"], [K("W")])
                ub = rq.next()
                ovu = mm6(ub, [K(XT)], [K("W")], (lambda h: T(XT)[:, h, :]), (lambda h: T("W")[:, h, :]))
                P.op("dve", lambda e: e.tensor_copy(out=T("U")[:], in_=ovu), [f"rbank{ub}"], [K("U")])
                hb_ = rq.next()
                ovh = mmacc(hb_, [((lambda h: T("dgP")[:, h, :]), Hv(Hc)), ((lambda h: T("bhT")[:, h, :]), (lambda h: T("U")[:, h, :])),
                                  ((lambda h: T("khT")[:, h, :]), (lambda h: T("Vt")[:, h, :]))],
                            reads=[K("dgP"), Hck, K("bhT"), K("U"), K("khT"), K("Vt")])
                P.op("act", lambda e: e.copy(out=Hn[:].rearrange("p (h t) -> p h t", h=6), in_=ovh), [f"rbank{hb_}"], [Hnk])
                yb_ = rq.next()
                ovy = mmacc(yb_, [((lambda h: T("rt")[:, h, :]), Hv(Hc)), ((lambda h: T("MrbT")[:, h, :]), (lambda h: T("U")[:, h, :])),
                                  ((lambda h: T("MrkT")[:, h, :]), (lambda h: T("Vt")[:, h, :]))],
                            reads=[K("rt"), Hck, K("MrbT"), K("U"), K("MrkT"), K("Vt")])
                if d == 1:
                    yst = T("yst")
                    P.op("act", lambda e: e.copy(out=yst[:, 0, :].rearrange("p (h t) -> p h t", h=6), in_=ovy), [f"rbank{yb_}"], [K("yst")])
                    tt("dve", yst[:, 1, :].rearrange("p (h t) -> p h t", h=6), T("Vt")[:], c6(T("bon")[:]), ALU.mult, [K("Vt"), K("bon")], [K("yst")])
                    yst2 = T("yld")
                    for c in range(2):
                        jb = rq.next()
                        P.op("pe", lambda e, c=c, jb=jb: e.matmul(rbank[jb][:, 0:384], cst["jrev"][:], yst[:, c, :], start=True, stop=True),
                             reads=[K("yst"), "jrev"], writes=[f"rbank{jb}"])
                        P.op("act" if c == 0 else "dve", (lambda e, c=c, jb=jb: e.copy(out=yst2[:, c, :], in_=rbank[jb][:, 0:384])) if c == 0 else
                             (lambda e, c=c, jb=jb: e.tensor_copy(out=yst2[:, c, :], in_=rbank[jb][:, 0:384])), [f"rbank{jb}"], [K("yld")])
                    P.dma("sp", lambda e: e.dma_start(out=yb_d[t0:t0 + 64], in_=yst2[:]), f"st_y{pb_}", reads=[K("yld")], writes=["yb_d"])
                else:
                    yld = T("yld")
                    P.dma("act", lambda e: e.dma_start(out=yld[:], in_=yb_d[t0:t0 + 64]), f"ld_y{pb_}", writes=[K("yld")])
                    y3 = lambda ap2: ap2.rearrange("p (h t) -> p h t", h=6)
                    tt("dve", T("yo")[:], ovy, y3(yld[:, 0, :]), ALU.add, [f"rbank{yb_}", K("yld")], [K("yo")])
                    tt("dve", T("ybl")[:], T("Vt")[:], c6(T("bon")[:]), ALU.mult, [K("Vt"), K("bon")], [K("ybl")])
                    tt("dve", T("ybl")[:], T("ybl")[:], y3(yld[:, 1, :]), ALU.add, [K("ybl"), K("yld")], [K("ybl")])
                    P.op("dve", lambda e: e.tensor_reduce(out=T("st")[:], in_=T("yo")[:], axis=AX.X, op=ALU.add), [K("yo")], [K("st")])
                    P.op("dve", lambda e: e.tensor_scalar(out=T("st")[:], in0=T("st")[:], scalar1=1.0 / 64, scalar2=None, op0=ALU.mult), [K("st")], [K("st")])
                    tt("dve", T("cen")[:], T("yo")[:], c6(T("st")[:]), ALU.subtract, [K("yo"), K("st")], [K("cen")])
                    tt("dve", T("sqc")[:], T("cen")[:], T("cen")[:], ALU.mult, [K("cen")], [K("sqc")])
                    P.op("dve", lambda e: e.tensor_reduce(out=T("st2")[:], in_=T("sqc")[:], axis=AX.X, op=ALU.add), [K("sqc")], [K("st2")])
                    act(T("st2")[:], T("st2")[:], AF.Sqrt, [K("st2"), "eps12"], [K("st2")], scale=1.0 / 64, bias=eps12[:])
                    P.op("dve", lambda e: e.reciprocal(out=T("st2")[:], in_=T("st2")[:]), [K("st2")], [K("st2")])
                    tt("dve", T("yn")[:], T("cen")[:], c6(T("st2")[:]), ALU.mult, [K("cen"), K("st2")], [K("yn")])
                    tt("dve", T("yn")[:], T("yn")[:], y3(cst["lng"][:]), ALU.mult, [K("yn"), "lng"], [K("yn")])
                    tt("dve", T("yn")[:], T("yn")[:], y3(cst["lnb"][:]), ALU.add, [K("yn"), "lnb"], [K("yn")])
                    tt("dve", T("yn")[:], T("yn")[:], T("ybl")[:], ALU.add, [K("yn"), K("ybl")], [K("yn")])
                    act(T("sgg")[:, 0:2, :], X[:, 20:22, 1:65], AF.Sigmoid, [K("X")], [K("sgg")])
                    gb = rq.next()
                    gfn = [(lambda e, c=c: e.matmul(rbank[gb][:, 0:384], T("sgg")[:, c, :], cst["g2"][:, c * 384:(c + 1) * 384], start=(c == 0), stop=(c == 1)))
                           for c in range(2)]
                    P.op("pe", gfn, reads=[K("sgg"), "g2"], writes=[f"rbank{gb}"])
                    yrwb = T("yrwb")
                    tt("dve", yrwb[:], T("yn")[:].rearrange("p h t -> p (h t)"), rbank[gb][:, 0:384], ALU.mult, [K("yn"), f"rbank{gb}"], [K("yrwb")])
                    P.op("pe", [(lambda e, c=c: e.transpose(out=rbt[:, c * 64:(c + 1) * 64], in_=yrwb[:, c * 128:(c + 1) * 128], identity=identb[0:64, 0:64]))
                                for c in range(3)], reads=[K("yrwb"), "identb"], writes=["rbt"])
                    ob = (i // 8) % 2
                    yT = tl[("yrwT", ob)]
                    P.op("act", lambda e: e.copy(out=yT[:, :, (i % 8) * 64:(i % 8) * 64 + 64], in_=rbt[:, 0:192].rearrange("p (c q) -> p c q", c=3)),
                         reads=["rbt"], writes=[f"yrwT{ob}"])
                    if i % 8 == 7:
                        tb0 = (i // 8) * 512
                        P.dma("sp", lambda e: e.dma_start(out=yrwT_d.rearrange("(c p) t -> p c t", p=128)[:, :, tb0:tb0 + 512], in_=yT[:]),
                              f"st_yrw{ob}", reads=[f"yrwT{ob}"], writes=["yrwT_d"])

            for d in (1, 0):
                P.op("dve", lambda e: e.memset(Hs[0][:], 0.0), writes=["H0"])
                for i in range(RW_CHUNKS):
                    rw_chunk(d, i)
            P.barrier()

        if PHASES >= 4:
          affall = sb("affall", [128, 64, 16], F32)
          with ExitStack() as pd:
            Wg = sb("Wg", [128, 8, 3072], BF16, pd)
            Wb = [sb("Wna", [128, 3, 1024], BF16, pd), sb("Wrw", [128, 3, 1024], BF16, pd), sb("Wmem", [128, 2, 1024], BF16, pd)]
            Wo = sb("Wo", [128, 8, 1024], BF16, pd)
            wr = sb("wr", [128, 8, 16], F32, pd)
            gffn = sb("gffn", [128, 1024], F32, pd)
            tailc = sb("tailc", [128, 64, 4], F32, pd)
            for c in range(8):
                P.dma("pool", lambda e, c=c: e.dma_start(out=Wg[:, c, :], in_=w_in.rearrange("(c p) n -> p c n", p=128)[:, c, 2816:5888]), f"Wg{c}", writes=[f"Wg{c}"])
            P.dma("pool", lambda e: e.dma_start(out=Wb[0][:], in_=wbna_d.rearrange("(c p) n -> p c n", p=128)), "Wb0", writes=["Wb0"])
            P.dma("pool", lambda e: e.dma_start(out=Wb[1][:], in_=wbrw_d.rearrange("(c p) n -> p c n", p=128)), "Wb1", writes=["Wb1"])
            P.dma("pool", lambda e: e.dma_start(out=Wb[2][:], in_=wbmem_d.rearrange("(c p) n -> p c n", p=128)), "Wb2", writes=["Wb2"])
            P.dma("pool", lambda e: e.dma_start(out=Wo[:], in_=wout_d.rearrange("(c p) n -> p c n", p=128)), "Wo", writes=["Wo"])
            P.dma("sp", lambda e: e.dma_start(out=wr[:], in_=wrt_d.rearrange("(c p) n -> p c n", p=128)), "wr", writes=["wr"])
            P.dma("sp", lambda e: e.dma_start(out=gffn[:], in_=gffn_d), "gffn", writes=["gffn"])
            P.dma("sp", lambda e: e.dma_start(out=tailc[:].rearrange("p t k -> p (t k)"), in_=tail_d), "tailc", writes=["tailc"])
            dhT = [sb(f"d_hTb{i}", [128, 8, 512], BF16, pd) for i in range(2)]
            yT = [[sb(f"d_y{b}T{i}", [128, 3 if b < 2 else 2, 512], BF16, pd) for i in range(2)] for b in range(3)]
            sg_t = [sb(f"d_sg{i}", [128, 512], F32, pd) for i in range(2)]
            mg = sb("d_mg", [128, 512], F32, pd)
            tmpm = sb("d_tmpm", [128, 512], F32, pd)
            mT = sb("d_mT", [128, 8, 512], BF16, pd)
            xt_ = [sb(f"d_xt{i}", [128, D], F32, pd) for i in range(2)]
            x1_ = [sb(f"d_x1{i}", [128, D], F32, pd) for i in range(2)]
            h2_ = [sb(f"d_h2{i}", [128, D], F32, pd) for i in range(2)]
            h2row = [sb(f"d_h2row{i}", [128, 1028], BF16, pd) for i in range(2)]
            h2T = sb("d_h2T", [128, 8, 128], F32, pd)
            sqj = sb("d_sqj", [128, D], BF16, pd)
            sst = [sb(f"d_ss{i}", [128, 1], F32, pd) for i in range(2)]
            rst = [sb(f"d_rs{i}", [128, 1], F32, pd) for i in range(2)]
            mxs = [sb(f"d_mx{i}", [128, 1], F32, pd) for i in range(2)]
            sms = [sb(f"d_sm{i}", [128, 1], F32, pd) for i in range(2)]
            lex = [sb(f"d_lex{i}", [128, 16], F32, pd) for i in range(2)]
            epsd = sb("d_eps", [128, 1], F32, pd)
            P.op("dve", lambda e: e.memset(epsd[:], 1e-6), writes=["d_eps"])
            dbank = [ps(f"dbank{i}", [128, 512], F32, pd) for i in range(8)]
            dq = RR(range(8))
            srcs = (ynaT_d, yrwT_d, ymemT_d)
            nck = (3, 3, 2)

            def d_tile(blk, j):
                ti = blk * 4 + j
                b2 = ti % 2
                P.dma("act", lambda e: e.dma_start(out=xt_[b2][:], in_=x_d[ti * 128:(ti + 1) * 128, :]), f"d_ldx{b2}", writes=[f"d_xt{b2}"])
                for half in range(2):
                    ob = dq.next()
                    mm(P, dbank[ob][:], [(mT[:, c, j * 128:(j + 1) * 128], Wo[:, c, half * 512:(half + 1) * 512]) for c in range(8)],
                       reads=["d_mT", "Wo"], writes=[f"dbank{ob}"])
                    P.op("dve", lambda e, ob=ob, half=half: e.tensor_tensor(out=x1_[b2][:, half * 512:(half + 1) * 512], in0=dbank[ob][:],
                                                                          in1=xt_[b2][:, half * 512:(half + 1) * 512], op=ALU.add),
                         reads=[f"dbank{ob}", f"d_xt{b2}"], writes=[f"d_x1{b2}"])
                P.dma("sp", lambda e: e.dma_start(out=acc_d[ti * 128:(ti + 1) * 128, :], in_=x1_[b2][:]), f"d_stx1{b2}", reads=[f"d_x1{b2}"], writes=["acc_d"])
                P.op("act", lambda e: e.activation(out=sqj[:], in_=x1_[b2][:], func=AF.Square, accum_out=sst[b2][:]),
                     reads=[f"d_x1{b2}"], writes=["d_sqj", f"d_ss{b2}"])
                P.op("act", lambda e: e.activation(out=rst[b2][:], in_=sst[b2][:], func=AF.Sqrt, scale=1.0 / D, bias=epsd[:]),
                     reads=[f"d_ss{b2}", "d_eps"], writes=[f"d_rs{b2}"])
                P.op("dve", lambda e: e.reciprocal(out=rst[b2][:], in_=rst[b2][:]), reads=[f"d_rs{b2}"], writes=[f"d_rs{b2}"])
                P.op("act", lambda e: e.activation(out=h2_[b2][:], in_=x1_[b2][:], func=AF.Copy, scale=rst[b2][:]),
                     reads=[f"d_x1{b2}", f"d_rs{b2}"], writes=[f"d_h2{b2}"])
                P.op("dve", lambda e: e.tensor_tensor(out=h2_[b2][:], in0=h2_[b2][:], in1=gffn[:], op=ALU.mult), reads=[f"d_h2{b2}", "gffn"], writes=[f"d_h2{b2}"])
                P.op("act", lambda e: e.copy(out=h2row[b2][:, 0:1024], in_=h2_[b2][:]), reads=[f"d_h2{b2}"], writes=[f"d_h2row{b2}"])
                P.op("dve", lambda e: e.tensor_copy(out=h2row[b2][:, 1024:1028], in_=tailc[:, ti, :]), reads=["tailc"], writes=[f"d_h2row{b2}"])
                P.dma("sp", lambda e: e.dma_start(out=h2_d[ti * 128:(ti + 1) * 128, :], in_=h2row[b2][:]), f"d_sth2{b2}", reads=[f"d_h2row{b2}"], writes=["h2_d"])
                for half in range(2):
                    tb = dq.next()
                    tv = dbank[tb][:].rearrange("p (c t) -> p c t", c=4)
                    P.op("pe", [(lambda e, c=c, tv=tv, half=half: e.transpose(out=tv[:, c, :], in_=h2_[b2][:, (half * 4 + c) * 128:(half * 4 + c + 1) * 128], identity=identf[:]))
                                for c in range(4)], reads=[f"d_h2{b2}", "identf"], writes=[f"dbank{tb}"])
                    if half == 0:
                        P.op("act", lambda e, tv=tv: e.copy(out=h2T[:, 0:4, :], in_=tv), [f"dbank{tb}"], ["d_h2T"])
                    else:
                        P.op("dve", lambda e, tv=tv: e.tensor_copy(out=h2T[:, 4:8, :], in_=tv), [f"dbank{tb}"], ["d_h2T"])
                lb = dq.next()
                mm(P, dbank[lb][:, 0:16], [(h2T[:, c, :], wr[:, c, :]) for c in range(8)], reads=["d_h2T", "wr"], writes=[f"dbank{lb}"])
                P.op("dve", lambda e: e.tensor_reduce(out=mxs[b2][:], in_=dbank[lb][:, 0:16], axis=AX.X, op=ALU.max), [f"dbank{lb}"], [f"d_mx{b2}"])
                P.op("dve", lambda e: e.tensor_scalar(out=mxs[b2][:], in0=mxs[b2][:], scalar1=-1.0, scalar2=None, op0=ALU.mult), [f"d_mx{b2}"], [f"d_mx{b2}"])
                P.op("act", lambda e: e.activation(out=lex[b2][:], in_=dbank[lb][:, 0:16], func=AF.Exp, bias=mxs[b2][:], accum_out=sms[b2][:]),
                     [f"dbank{lb}", f"d_mx{b2}"], [f"d_lex{b2}", f"d_sm{b2}"])
                P.op("dve", lambda e: e.reciprocal(out=sms[b2][:], in_=sms[b2][:]), [f"d_sm{b2}"], [f"d_sm{b2}"])
                P.op("dve", lambda e: e.tensor_scalar(out=affall[:, ti, :], in0=lex[b2][:], scalar1=sms[b2][:], scalar2=None, op0=ALU.mult),
                     [f"d_lex{b2}", f"d_sm{b2}"], ["affall"])

            def d_chunk(blk, hb, dmc):
                for b in range(3):
                    gb = dq.next()
                    c0 = b * 1024 + dmc * 128
                    mm(P, dbank[gb][:], [(Wg[:, c, c0:c0 + 128], dhT[hb][:, c, :]) for c in range(8)], reads=[f"Wg{c}" for c in range(8)] + [f"d_hTb{hb}"], writes=[f"dbank{gb}"])
                    s2 = b % 2
                    P.op("act", lambda e, gb=gb, s2=s2: e.activation(out=sg_t[s2][:], in_=dbank[gb][:], func=AF.Sigmoid), [f"dbank{gb}"], [f"d_sg{s2}"])
                    bb = dq.next()
                    mm(P, dbank[bb][:], [(Wb[b][:, c, dmc * 128:(dmc + 1) * 128], yT[b][hb][:, c, :]) for c in range(nck[b])],
                       reads=[f"Wb{b}", f"d_y{b}T{hb}"], writes=[f"dbank{bb}"])
                    if b == 0:
                        P.op("dve", lambda e, bb=bb, s2=s2: e.tensor_tensor(out=mg[:], in0=dbank[bb][:], in1=sg_t[s2][:], op=ALU.mult),
                             [f"dbank{bb}", f"d_sg{s2}"], ["d_mg"])
                    else:
                        P.op("dve", lambda e, bb=bb, s2=s2: e.tensor_tensor(out=tmpm[:], in0=dbank[bb][:], in1=sg_t[s2][:], op=ALU.mult),
                             [f"dbank{bb}", f"d_sg{s2}"], ["d_tmpm"])
                        if b == 1:
                            P.op("dve", lambda e: e.tensor_tensor(out=mg[:], in0=mg[:], in1=tmpm[:], op=ALU.add), ["d_mg", "d_tmpm"], ["d_mg"])
                        else:
                            P.op("dve", lambda e: e.tensor_tensor(out=mT[:, dmc, :], in0=mg[:], in1=tmpm[:], op=ALU.add), ["d_mg", "d_tmpm"], ["d_mT"])

            def d_block(blk):
                hb = blk % 2
                t0 = blk * 512
                P.dma("sp", lambda e: e.dma_start(out=dhT[hb][:], in_=hT_d.rearrange("(c p) t -> p c t", p=128)[:, :, t0:t0 + 512]), f"d_ldh{hb}", writes=[f"d_hTb{hb}"])
                for b in range(3):
                    P.dma("act", lambda e, b=b: e.dma_start(out=yT[b][hb][:], in_=srcs[b].rearrange("(c p) t -> p c t", p=128)[:, :, t0:t0 + 512]),
                          f"d_ldy{b}{hb}", writes=[f"d_y{b}T{hb}"])
                for dmc in range(8):
                    d_chunk(blk, hb, dmc)
                    if D_BARRIER:
                        P.barrier()
                for j in range(4):
                    d_tile(blk, j)
                    if D_BARRIER:
                        P.barrier()
                    if DBG_D and blk == 0 and j == 0:
                        P.barrier()
                        dv = lambda r0, r1: out_d[r0:r1, :].rearrange("(p a) n -> p (a n)", p=128)
                        P.dma("sp", lambda e: e.dma_start(out=dv(0, 256).bitcast(BF16), in_=dhT[0][:].rearrange("p c t -> p (c t)")), "dbgd", writes=["out"])
                        P.dma("sp", lambda e: e.dma_start(out=dv(256, 512).bitcast(BF16), in_=mT[:].rearrange("p c t -> p (c t)")), "dbgd", writes=["out"])
                        P.dma("sp", lambda e: e.dma_start(out=out_d[512:640, 0:768].bitcast(BF16), in_=yT[1][0][:].rearrange("p c t -> p (c t)")), "dbgd", writes=["out"])
                        P.dma("sp", lambda e: e.dma_start(out=out_d[640:768, :], in_=x1_[0][:]), "dbgd", writes=["out"])
                        P.dma("sp", lambda e: e.dma_start(out=out_d[768:896, 0:512], in_=sg_t[0][:]), "dbgd", writes=["out"])
                        P.dma("sp", lambda e: e.dma_start(out=out_d[768:896, 512:1024], in_=mg[:]), "dbgd", writes=["out"])
                        P.dma("sp", lambda e: e.dma_start(out=out_d[896:1024, :], in_=xt_[0][:]), "dbgd", writes=["out"])
                        P.barrier()

            for blk in range(S // 512):
                d_block(blk)
            P.dma("sp", lambda e: e.dma_start(out=aff_d.rearrange("(t p) e -> p t e", p=128), in_=affall[:]), "st_aff", reads=["affall"], writes=["aff_d"])
            P.barrier()

        if PHASES >= 5:
          with ExitStack() as pe_:
            ones128 = sb("e_ones", [128, 128], F32, pe_)
            ltm = sb("e_ltm", [128, 128], F32, pe_)
            lo = sb("e_lo", [128, 16], F32, pe_)
            hi = sb("e_hi", [128, 16], F32, pe_)
            mid = sb("e_mid", [128, 16], F32, pe_)
            cnt = sb("e_cnt", [128, 16], F32, pe_)
            ge = sb("e_ge", [128, 16], F32, pe_)
            dlt = sb("e_dlt", [128, 16], F32, pe_)
            cmpt = sb("e_cmp", [128, 64, 16], F32, pe_)
            sel = sb("e_sel", [128, 64, 16], F32, pe_)
            cntb = sb("e_cntb", [128, 64, 16], F32, pe_)
            cum = sb("e_cum", [128, 16, 64], F32, pe_)
            rank = sb("e_rank", [128, 64, 16], F32, pe_)
            ranki = sb("e_ranki", [128, 1024], I32, pe_)
            onesr = sb("e_onesr", [128, 64], F32, pe_)
            ebank = [ps(f"ebank{i}", [128, 512], F32, pe_) for i in range(4)]
            P.op("dve", lambda e: e.memset(ones128[:], 1.0), writes=["e_ones"])
            P.op("dve", lambda e: e.memset(onesr[:], 1.0), writes=["e_onesr"])
            P.dma("sp", lambda e: e.dma_start(out=ltm[:], in_=ltm_d), "e_ltm", writes=["e_ltm"])
            P.op("dve", lambda e: e.memset(lo[:], 0.0), writes=["e_lo"])
            P.op("dve", lambda e: e.memset(hi[:], 2.0), writes=["e_hi"])

            def bis_iter(it):
                P.op("dve", lambda e: e.tensor_tensor(out=mid[:], in0=lo[:], in1=hi[:], op=ALU.add), ["e_lo", "e_hi"], ["e_mid"])
                P.op("dve", lambda e: e.tensor_scalar(out=mid[:], in0=mid[:], scalar1=0.5, scalar2=None, op0=ALU.mult), ["e_mid"], ["e_mid"])
                P.op("dve", lambda e: e.tensor_tensor(out=cmpt[:], in0=affall[:], in1=mid[:].unsqueeze(1).to_broadcast([128, 64, 16]), op=ALU.is_ge),
                     ["affall", "e_mid"], ["e_cmp"])
                P.op("dve", lambda e: e.tensor_reduce(out=cnt[:], in_=cmpt[:].rearrange("p t e -> p e t"), axis=AX.X, op=ALU.add), ["e_cmp"], ["e_cnt"])
                b = it % 2
                mm(P, ebank[b][:, 0:16], [(ones128[:], cnt[:])], reads=["e_ones", "e_cnt"], writes=[f"ebank{b}"])
                P.op("dve", lambda e: e.tensor_scalar(out=ge[:], in0=ebank[b][:, 0:16], scalar1=1023.5, scalar2=None, op0=ALU.is_ge), [f"ebank{b}"], ["e_ge"])
                P.op("dve", lambda e: e.tensor_tensor(out=dlt[:], in0=mid[:], in1=lo[:], op=ALU.subtract), ["e_mid", "e_lo"], ["e_dlt"])
                P.op("dve", lambda e: e.tensor_tensor(out=dlt[:], in0=dlt[:], in1=ge[:], op=ALU.mult), ["e_dlt", "e_ge"], ["e_dlt"])
                P.op("dve", lambda e: e.tensor_tensor(out=lo[:], in0=lo[:], in1=dlt[:], op=ALU.add), ["e_lo", "e_dlt"], ["e_lo"])
                P.op("dve", lambda e: e.tensor_tensor(out=dlt[:], in0=hi[:], in1=mid[:], op=ALU.subtract), ["e_hi", "e_mid"], ["e_dlt"])
                P.op("dve", lambda e: e.tensor_tensor(out=dlt[:], in0=dlt[:], in1=ge[:], op=ALU.mult), ["e_dlt", "e_ge"], ["e_dlt"])
                P.op("dve", lambda e: e.tensor_tensor(out=hi[:], in0=mid[:], in1=dlt[:], op=ALU.add), ["e_mid", "e_dlt"], ["e_hi"])

            for it in range(36):
                bis_iter(it)
            P.op("dve", lambda e: e.tensor_tensor(out=sel[:], in0=affall[:], in1=lo[:].unsqueeze(1).to_broadcast([128, 64, 16]), op=ALU.is_ge),
                 ["affall", "e_lo"], ["e_sel"])
            self2 = sel[:].rearrange("p t e -> p (t e)")
            for half in range(2):
                mm(P, ebank[half][:], [(ones128[:], self2[:, half * 512:(half + 1) * 512])], reads=["e_ones", "e_sel"], writes=[f"ebank{half}"])
                P.op("act", lambda e, half=half: e.copy(out=cntb[:].rearrange("p t e -> p (t e)")[:, half * 512:(half + 1) * 512], in_=ebank[half][:]),
                     [f"ebank{half}"], ["e_cntb"])
            for ex in range(16):
                P.op("dve", lambda e, ex=ex: e.tensor_tensor_scan(out=cum[:, ex, :], data0=onesr[:], data1=cntb[:, :, ex], initial=0.0, op0=ALU.mult, op1=ALU.add),
                     ["e_cntb", "e_onesr"], ["e_cum"])
            P.op("dve", lambda e: e.tensor_tensor(out=rank[:], in0=cum[:].rearrange("p e t -> p t e"), in1=cntb[:], op=ALU.subtract), ["e_cum", "e_cntb"], ["e_rank"])
            for half in range(2):
                mm(P, ebank[2 + half][:], [(ltm[:], self2[:, half * 512:(half + 1) * 512])], reads=["e_ltm", "e_sel"], writes=[f"ebank{2 + half}"])
                P.op("dve", lambda e, half=half: e.tensor_tensor(out=rank[:].rearrange("p t e -> p (t e)")[:, half * 512:(half + 1) * 512],
                                                                 in0=rank[:].rearrange("p t e -> p (t e)")[:, half * 512:(half + 1) * 512], in1=ebank[2 + half][:], op=ALU.add),
                     [f"ebank{2 + half}", "e_rank"], ["e_rank"])
            P.op("dve", lambda e: e.tensor_scalar(out=rank[:], in0=rank[:], scalar1=-60000.0, scalar2=None, op0=ALU.add), ["e_rank"], ["e_rank"])
            P.op("dve", lambda e: e.tensor_tensor(out=rank[:], in0=rank[:], in1=sel[:], op=ALU.mult), ["e_rank", "e_sel"], ["e_rank"])
            P.op("dve", lambda e: e.tensor_scalar(out=rank[:], in0=rank[:], scalar1=60000.0, scalar2=None, op0=ALU.add), ["e_rank"], ["e_rank"])
            P.op("dve", lambda e: e.tensor_copy(out=ranki[:], in_=rank[:].rearrange("p t e -> p (t e)")), ["e_rank"], ["e_ranki"])
            if DEBUG:
                P.dma("sp", lambda e: e.dma_start(out=rank_dbg, in_=rank[:].rearrange("p t e -> p (t e)")), "dbg_rank", reads=["e_rank"], writes=["rank_dbg"])
            h2r = [sb(f"e_h2r{i}", [128, 1028], BF16, pe_) for i in range(6)]

            def disp_tile(ti):
                b3 = ti % 6
                P.dma("sp", lambda e: e.dma_start(out=h2r[b3][:], in_=h2_d[ti * 128:(ti + 1) * 128, :]), f"e_ld{b3}", writes=[f"e_h2r{b3}"])
                for ex in range(16):
                    P.dma("pool", lambda e, ex=ex: e.indirect_dma_start(out=xe_d[ex], out_offset=bass.IndirectOffsetOnAxis(ap=ranki[:, ti * 16 + ex:ti * 16 + ex + 1], axis=0),
                                                                        in_=h2r[b3][:], in_offset=None, bounds_check=breg(e, 1023), oob_is_err=False),
                          f"e_sc{b3}", reads=[f"e_h2r{b3}", "e_ranki"], writes=())

            for ti in range(64):
                disp_tile(ti)
            P.barrier()

        if PHASES >= 6:
          with ExitStack() as pf:
            xe = [sb(f"f_xe{i}", [128, 1028], BF16, pf) for i in range(2)]
            xeT = sb("f_xeT", [128, 8, 1024], BF16, pf)
            tailf = sb("f_tailf", [128, 8, 2], F32, pf)
            idf_ = sb("f_idf", [128, 8], F32, pf)
            idi = sb("f_idi", [128, 8], I32, pf)
            affg = sb("f_affg", [128, 8, 16], F32, pf)
            wgt = [sb(f"f_wg{i}", [128, 8, 512], BF16, pf) for i in range(3)]
            wut = [sb(f"f_wu{i}", [128, 8, 512], BF16, pf) for i in range(3)]
            wdt = [sb(f"f_wd{i}", [128, 4, 1024], BF16, pf) for i in range(3)]
            actT = sb("f_actT", [128, 4, 1024], BF16, pf)
            sil = [sb(f"f_sil{i}", [128, 512], F32, pf) for i in range(2)]
            Y = sb("f_Y", [128, 8, 1024], F32, pf)
            fbank = [ps(f"fbank{i}", [128, 512], F32, pf) for i in range(6)]
            fbt = [ps(f"fbt{i}", [128, 1024], BF16, pf) for i in range(2)]
            fq = RR(range(6))
            wctr = [0]

            def load_w(ex, fg):
                wb = wctr[0] % 3
                wctr[0] += 1
                f0 = fg * 512
                P.dma("pool", lambda e: e.dma_start(out=wgt[wb][:], in_=wgate_d[ex].rearrange("(c p) f -> p c f", p=128)[:, :, f0:f0 + 512]), f"f_ldg{wb}", writes=[f"f_wg{wb}"])
                P.dma("pool", lambda e: e.dma_start(out=wut[wb][:], in_=wup_d[ex].rearrange("(c p) f -> p c f", p=128)[:, :, f0:f0 + 512]), f"f_ldu{wb}", writes=[f"f_wu{wb}"])
                P.dma("pool", lambda e: e.dma_start(out=wdt[wb][:], in_=wdown_d[ex, f0:f0 + 512, :].rearrange("(c p) n -> p c n", p=128)), f"f_ldd{wb}", writes=[f"f_wd{wb}"])
                return wb

            def xe_tile(ex, st):
                b2 = st % 2
                P.dma("sp", lambda e: e.dma_start(out=xe[b2][:], in_=xe_d[ex][st * 128:(st + 1) * 128, :]), f"f_ldxe{b2}", writes=[f"f_xe{b2}"])
                P.op("pe", [(lambda e, c=c: e.transpose(out=fbt[b2][:, c * 128:(c + 1) * 128], in_=xe[b2][:, c * 128:(c + 1) * 128], identity=identb[:])) for c in range(8)],
                     reads=[f"f_xe{b2}", "identb"], writes=[f"fbt{b2}"])
                ev = "act" if b2 == 0 else "dve"
                if ev == "act":
                    P.op("act", lambda e: e.copy(out=xeT[:, :, st * 128:(st + 1) * 128], in_=fbt[b2][:].rearrange("p (c t) -> p c t", c=8)), [f"fbt{b2}"], ["f_xeT"])
                else:
                    P.op("dve", lambda e: e.tensor_copy(out=xeT[:, :, st * 128:(st + 1) * 128], in_=fbt[b2][:].rearrange("p (c t) -> p c t", c=8)), [f"fbt{b2}"], ["f_xeT"])
                P.op("dve", lambda e: e.tensor_copy(out=tailf[:, st, :], in_=xe[b2][:, 1024:1026]), [f"f_xe{b2}"], ["f_tailf"])

            def gu(ex, wb, fc, sh):
                gb, ub = fq.next(), fq.next()
                mm(P, fbank[gb][:], [(wgt[wb][:, c, fc * 128:(fc + 1) * 128], xeT[:, c, sh * 512:(sh + 1) * 512]) for c in range(8)],
                   reads=[f"f_wg{wb}", "f_xeT"], writes=[f"fbank{gb}"])
                mm(P, fbank[ub][:], [(wut[wb][:, c, fc * 128:(fc + 1) * 128], xeT[:, c, sh * 512:(sh + 1) * 512]) for c in range(8)],
                   reads=[f"f_wu{wb}", "f_xeT"], writes=[f"fbank{ub}"])
                s2 = (fc * 2 + sh) % 2
                P.op("act", lambda e: e.activation(out=sil[s2][:], in_=fbank[gb][:], func=AF.Silu), [f"fbank{gb}"], [f"f_sil{s2}"])
                P.op("dve", lambda e: e.tensor_tensor(out=actT[:, fc, sh * 512:(sh + 1) * 512], in0=fbank[ub][:], in1=sil[s2][:], op=ALU.mult),
                     [f"fbank{ub}", f"f_sil{s2}"], ["f_actT"])

            def down(ex, wb, fg, st, dh):
                yb = fq.next()
                mm(P, fbank[yb][:], [(actT[:, fc, st * 128:(st + 1) * 128], wdt[wb][:, fc, dh * 512:(dh + 1) * 512]) for fc in range(4)],
                   reads=["f_actT", f"f_wd{wb}"], writes=[f"fbank{yb}"])
                dst = Y[:, st, dh * 512:(dh + 1) * 512]
                if fg == 0:
                    P.op("act", lambda e: e.copy(out=dst, in_=fbank[yb][:]), [f"fbank{yb}"], [f"f_Y{st}"])
                else:
                    P.op("dve", lambda e: e.tensor_tensor(out=dst, in0=fbank[yb][:], in1=dst, op=ALU.add), [f"fbank{yb}", f"f_Y{st}"], [f"f_Y{st}"])

            def expert(ex):
                for st in range(8):
                    xe_tile(ex, st)
                P.op("dve", lambda e: e.scalar_tensor_tensor(out=idf_[:], in0=tailf[:, :, 1], scalar=128.0, in1=tailf[:, :, 0], op0=ALU.mult, op1=ALU.add),
                     ["f_tailf"], ["f_idf"])
                P.op("dve", lambda e: e.tensor_copy(out=idi[:], in_=idf_[:]), ["f_idf"], ["f_idi"])
                for st in range(8):
                    P.dma("pool", lambda e, st=st: e.indirect_dma_start(out=affg[:, st, :], out_offset=None, in_=aff_d,
                                                                        in_offset=bass.IndirectOffsetOnAxis(ap=idi[:, st:st + 1], axis=0)),
                          "f_affg", reads=["f_idi"], writes=["f_affg"])
                for fg in range(4):
                    wb = load_w(ex, fg)
                    for fc in range(4):
                        for sh in range(2):
                            gu(ex, wb, fc, sh)
                    for st in range(8):
                        for dh in range(2):
                            down(ex, wb, fg, st, dh)
                for st in range(8):
                    P.op("dve", lambda e, st=st: e.tensor_scalar(out=Y[:, st, :], in0=Y[:, st, :], scalar1=affg[:, st, ex:ex + 1], scalar2=None, op0=ALU.mult),
                         [f"f_Y{st}", "f_affg"], [f"f_Y{st}"])
                    P.dma("pool", lambda e, st=st: e.indirect_dma_start(out=acc_d, out_offset=bass.IndirectOffsetOnAxis(ap=idi[:, st:st + 1], axis=0),
                                                                        in_=Y[:, st, :], in_offset=None, bounds_check=breg(e, S - 1), oob_is_err=False, compute_op=ALU.add),
                          "f_sca", reads=[f"f_Y{st}", "f_idi"], writes=["acc_d"])

            for ex in range(N_EXP):
                expert(ex)
            P.barrier()

        if PHASES >= 7:
          with ExitStack() as pg:
            gfin = sb("g_gfin", [128, D], F32, pg)
            P.dma("sp", lambda e: e.dma_start(out=gfin[:], in_=gfin_d), "g_gfin", writes=["g_gfin"])
            at_ = [sb(f"g_a{i}", [128, D], F32, pg) for i in range(3)]
            ot_ = [sb(f"g_o{i}", [128, D], F32, pg) for i in range(2)]
            gsq = sb("g_sq", [128, D], BF16, pg)
            gss = [sb(f"g_ss{i}", [128, 1], F32, pg) for i in range(2)]
            geps = sb("g_eps", [128, 1], F32, pg)
            P.op("dve", lambda e: e.memset(geps[:], 1e-6), writes=["g_eps"])

            def fin_tile(ti):
                b3, b2 = ti % 3, ti % 2
                P.dma("sp" if ti % 2 else "act", lambda e: e.dma_start(out=at_[b3][:], in_=acc_d[ti * 128:(ti + 1) * 128, :]), f"g_ld{b3}", writes=[f"g_a{b3}"])
                P.op("act", lambda e: e.activation(out=gsq[:], in_=at_[b3][:], func=AF.Square, accum_out=gss[b2][:]), [f"g_a{b3}"], ["g_sq", f"g_ss{b2}"])
                P.op("act", lambda e: e.activation(out=gss[b2][:], in_=gss[b2][:], func=AF.Sqrt, scale=1.0 / D, bias=geps[:]), [f"g_ss{b2}", "g_eps"], [f"g_ss{b2}"])
                P.op("dve", lambda e: e.reciprocal(out=gss[b2][:], in_=gss[b2][:]), [f"g_ss{b2}"], [f"g_ss{b2}"])
                P.op("dve", lambda e: e.scalar_tensor_tensor(out=ot_[b2][:], in0=at_[b3][:], scalar=gss[b2][:], in1=gfin[:], op0=ALU.mult, op1=ALU.mult),
                     [f"g_a{b3}", f"g_ss{b2}", "g_gfin"], [f"g_o{b2}"])
                if RAW_OUT:
                    P.dma("sp", lambda e: e.dma_start(out=out_d[ti * 128:(ti + 1) * 128, :], in_=at_[b3][:]), f"g_st{b2}", reads=[f"g_a{b3}"], writes=["out"])
                else:
                    P.dma("sp", lambda e: e.dma_start(out=out_d[ti * 128:(ti + 1) * 128, :], in_=ot_[b2][:]), f"g_st{b2}", reads=[f"g_o{b2}"], writes=["out"])

            for ti in range(8 if DBG_D else 0, 64):
                fin_tile(ti)
            P.barrier()

        if PHASES <= 6:
            tmp = sb("tmpo", [128, D], F32)
            P.op("dve", lambda e: e.memset(tmp[:], 0.0), writes=["tmpo"])
            P.dma("sp", lambda e: e.dma_start(out=out_d[0:128, :], in_=tmp[:]), "o", reads=["tmpo"], writes=["out"])
            P.barrier()
        P.emit()
    return nc, outs


def _bf16_eye():
    import ml_dtypes
    return np.eye(128, dtype=np.float32).astype(ml_dtypes.bfloat16)


def make_inputs(inp, b):
    f = lambda a: np.ascontiguousarray(a, dtype=np.float32)
    m = {
        "x": f(inp["x"][b]),
        "mem": f(inp["mem"][b]),
        "w_in": f(inp["w_in"][0]),
        "gmixT": f(inp["norm_mix_g"][0].reshape(8, 128).T),
        "gmemT": f(inp["norm_mem_g"][0].reshape(8, 128).T),
        "w_mem_kv": f(inp["w_mem_kv"][0]),
        "identb_in": _bf16_eye(),
        "identf_in": np.eye(128, dtype=np.float32),
    }
    rpb = f(inp["na_rpb"][0])
    p = np.arange(128)
    q = np.arange(64)
    coff = (p[:, None] % 64) - q[None, :] + 15
    valid = (coff >= 0) & (coff < 31)
    coffc = np.clip(coff, 0, 30)
    aa = np.arange(14)
    ro = (2 * (aa % 7) + aa // 7)[None, :, None] + (p[:, None, None] // 64)
    bias = rpb[:, ro, coffc[:, None, :]]
    bias = np.where(valid[None, :, None, :], bias, 0.0).transpose(1, 0, 2, 3)
    m["biasP_in"] = f(bias.reshape(128, 6 * 14 * 64))
    hp_ = lambda a: f(a.reshape(6, 64).T)
    mu = np.zeros((64, 2, 20), np.float32)
    for d in range(2):
        for j in range(3):
            mu[:, d, j * 6:(j + 1) * 6] = inp["rw_mu_rkv"][0, d, j].reshape(6, 64).T
        mu[:, d, 18] = inp["rw_mu_w"][0, d]
        mu[:, d, 19] = inp["rw_mu_a"][0, d]
    m["mu_in"] = f(mu.reshape(64, 40))
    w2a = np.zeros((65, 2, 384), np.float32)
    a2a = np.zeros((65, 2, 384), np.float32)
    for d in range(2):
        w2a[0:64, d] = inp["rw_w2"][0, d]
        w2a[64, d] = inp["rw_w0"][0, d]
        a2a[0:64, d] = inp["rw_a2"][0, d]
        a2a[64, d] = inp["rw_a0"][0, d]
    m["w2a_in"] = f(w2a.reshape(65, 768))
    m["a2a_in"] = f(a2a.reshape(65, 768))
    m["kkp_in"] = hp_(inp["rw_k_k"][0])
    m["kap_in"] = hp_(inp["rw_k_a"][0])
    m["rkp_in"] = hp_(inp["rw_r_k"][0].reshape(384))
    m["g2_in"] = f(inp["rw_g2"][0].reshape(2, 64, 384).transpose(1, 0, 2).reshape(64, 768))
    m["lng_in"] = f(np.broadcast_to(inp["rw_ln_g"][0][None, :], (64, 384)))
    m["lnb_in"] = f(np.broadcast_to(inp["rw_ln_b"][0][None, :], (64, 384)))
    ii = np.arange(64)
    mU = (ii[:, None] < ii[None, :]).astype(np.float32)
    mUi = (ii[:, None] <= ii[None, :]).astype(np.float32)
    mL = (ii[:, None] > ii[None, :]).astype(np.float32)
    m["msk_in"] = f(np.concatenate([mU, mUi, mL], axis=1))
    m["jrev_in"] = f(np.eye(64)[::-1])
    m["w_branch_na"] = f(inp["w_branch_na"][0])
    m["w_branch_rw"] = f(inp["w_branch_rw"][0])
    m["w_branch_mem"] = f(inp["w_branch_mem"][0])
    m["w_out"] = f(inp["w_out"][0])
    m["w_router"] = f(inp["w_router"][0])
    m["gffn_in"] = f(np.broadcast_to(inp["norm_ffn_g"][0][None, :], (128, D)))
    tail = np.zeros((128, 64, 4), np.float32)
    tail[:, :, 0] = np.arange(128)[:, None]
    tail[:, :, 1] = np.arange(64)[None, :]
    m["tail_in"] = f(tail.reshape(128, 256))
    pp = np.arange(128)
    m["ltm_in"] = f((pp[:, None] < pp[None, :]).astype(np.float32))
    m["gfin_in"] = f(np.broadcast_to(inp["norm_final_g"][None, :], (128, D)))
    m["w_exp_gate"] = f(inp["w_exp_gate"][0])
    m["w_exp_up"] = f(inp["w_exp_up"][0])
    m["w_exp_down"] = f(inp["w_exp_down"][0])
    cs = np.clip(q - 8, 0, 48)
    kc = p % 64
    m["maskP_in"] = f(((kc[:, None] >= cs[None, :]) & (kc[:, None] < cs[None, :] + 16)).astype(np.float32))
    return m


def kernel(**inputs):
    nc, outs = build()
    in_maps = [make_inputs(inputs, c % 4) for c in range(8)]
    res = run_bass_kernel_spmd(nc, in_maps, core_ids=list(range(8)))
    if DEBUG:
        kernel.last = res
    out = np.stack([np.asarray(res.results[b]["out"]) for b in range(4)], 0)
    return out.astype(np.float32)
```
